# Optimizing a Trainium2 kernel written in Bass

```python
import jax, jax.numpy as jnp
from jax import lax
import numpy as np

D_MODEL = 2048
BATCH = 4
SEQ = 4096
DEPTH = 2

CTX_LEN = 256
GRID_W = 64
EPS = 1e-6
HEAD_DIM = 128
ROT_AXIS = HEAD_DIM // 2
ROPE_THETA = 10000.0
Q_BLOCK = 128
ATTN_WIDTH = D_MODEL // 2
N_Q_HEADS = ATTN_WIDTH // HEAD_DIM
N_KV_HEADS = N_Q_HEADS // 4
KV_WIDTH = N_KV_HEADS * HEAD_DIM
SCONV_WIDTH = D_MODEL // 2
SCONV_K = 3
SCONV_PAD = 1
EVEN_SIZES = (ATTN_WIDTH, KV_WIDTH, KV_WIDTH, SCONV_WIDTH, SCONV_WIDTH, SCONV_WIDTH)
EVEN_IN = sum(EVEN_SIZES)
EVEN_OUT = ATTN_WIDTH + SCONV_WIDTH
LRU_WIDTH = 5 * D_MODEL // 4
LRU_BLOCK = 256
N_LRU_BLOCKS = LRU_WIDTH // LRU_BLOCK
LRU_CONV_K = 4
LRU_CONV_PAD = 1
LRU_C = 8.0
N_EXPERTS = 16
EXPERT_FF = D_MODEL
EC_FACTOR = 2
N_EVEN = (DEPTH + 1) // 2
N_ODD = DEPTH // 2

kernel_name = "hybrid_attn_conv_rglru_ecmoe_dit"


def rmsnorm(x, g):
    xf = x.astype(jnp.float32)
    y = xf * lax.rsqrt(jnp.mean(xf * xf, axis=-1, keepdims=True) + EPS)
    return (y * g.astype(jnp.float32)).astype(x.dtype)


def adaln(cvec, w, b):
    m = jax.nn.silu(cvec) @ w + b
    return jnp.split(m, 6, axis=-1)


def modulate(h, shift, scale):
    return h * (1 + scale) + shift


def split_cols(p, sizes):
    idx = np.cumsum(np.array(sizes))[:-1].tolist()
    return jnp.split(p, idx, axis=-1)


def heads(t, nh):
    return t.reshape(t.shape[:2] + (nh, HEAD_DIM))


def headnorm(t, g):
    tf = t.astype(jnp.float32)
    y = tf * lax.rsqrt(jnp.mean(tf * tf, axis=-1, keepdims=True) + EPS)
    return (y * g.astype(jnp.float32)).astype(t.dtype)


def rope_tables(pos):
    freqs = ROPE_THETA ** (-jnp.arange(0, ROT_AXIS, 2, dtype=jnp.float32) / ROT_AXIS)
    ang = pos.astype(jnp.float32)[:, None] * freqs[None, :]
    return jnp.cos(ang)[None, :, None, :], jnp.sin(ang)[None, :, None, :]


def rotate(u, cos, sin):
    u1, u2 = jnp.split(u, 2, axis=-1)
    return jnp.concatenate([u1 * cos - u2 * sin, u1 * sin + u2 * cos], axis=-1)


def rope2d(t, rope):
    cos_r, sin_r, cos_c, sin_c = rope
    tf = t.astype(jnp.float32)
    out = jnp.concatenate([rotate(tf[..., :ROT_AXIS], cos_r, sin_r),
                           rotate(tf[..., ROT_AXIS:], cos_c, sin_c)], axis=-1)
    return out.astype(t.dtype)


def block_attention(q, k, v):
    bsz, n = q.shape[0], q.shape[1]
    grp = N_Q_HEADS // N_KV_HEADS
    qb = q.reshape(bsz, n // Q_BLOCK, Q_BLOCK, N_KV_HEADS, grp, HEAD_DIM).transpose(1, 0, 2, 3, 4, 5)
    scale = HEAD_DIM ** -0.5

    def one(qblk):
        s = jnp.einsum('bqkgd,bskd->bkgqs', qblk, k).astype(jnp.float32) * scale
        p = jax.nn.softmax(s, axis=-1).astype(v.dtype)
        return jnp.einsum('bkgqs,bskd->bqkgd', p, v)

    o = lax.map(one, qb)
    return o.transpose(1, 0, 2, 3, 4, 5).reshape(bsz, n, N_Q_HEADS * HEAD_DIM)


def dwconv(u, w, b, pad_left):
    k_w = w.shape[0]
    n = u.shape[1]
    up = jnp.pad(u, ((0, 0), (pad_left, k_w - 1 - pad_left), (0, 0)))
    acc = up[:, 0:n] * w[0]
    for j in range(1, k_w):
        acc = acc + up[:, j:j + n] * w[j]
    return acc + b


def even_mixer(a_lat, a_ctx, w_in, q_g, k_g, conv_w, conv_b, w_out, rope, need_ctx):
    ql, kl, vl, bl, cl, ul = split_cols(a_lat @ w_in, EVEN_SIZES)
    ql = rope2d(headnorm(heads(ql, N_Q_HEADS), q_g), rope)
    kl = rope2d(headnorm(heads(kl, N_KV_HEADS), k_g), rope)
    vl = heads(vl, N_KV_HEADS)
    if need_ctx:
        qc, kc, vc, bc, cc, uc = split_cols(a_ctx @ w_in, EVEN_SIZES)
    else:
        kc, vc = split_cols(a_ctx @ w_in[:, ATTN_WIDTH:ATTN_WIDTH + 2 * KV_WIDTH], (KV_WIDTH, KV_WIDTH))
    kc = headnorm(heads(kc, N_KV_HEADS), k_g)
    vc = heads(vc, N_KV_HEADS)
    k_all = jnp.concatenate([kc, kl], axis=1)
    v_all = jnp.concatenate([vc, vl], axis=1)
    att_lat = block_attention(ql, k_all, v_all)
    conv_lat = bl * dwconv(cl * ul, conv_w, conv_b, SCONV_PAD)
    y_lat = jnp.concatenate([att_lat, conv_lat], axis=-1) @ w_out
    if not need_ctx:
        return y_lat, None
    qc = headnorm(heads(qc, N_Q_HEADS), q_g)
    att_ctx = block_attention(qc, kc, vc)
    conv_ctx = bc * dwconv(cc * uc, conv_w, conv_b, SCONV_PAD)
    y_ctx = jnp.concatenate([att_ctx, conv_ctx], axis=-1) @ w_out
    return y_lat, y_ctx


def blockdiag(u, w):
    bsz, n, width = u.shape
    ub = u.reshape(bsz, n, N_LRU_BLOCKS, LRU_BLOCK)
    return jnp.einsum('bnhi,hij->bnhj', ub, w).reshape(bsz, n, width)


def rglru_coeffs(u, wa, ba, wx, bx, lam):
    r = jax.nn.sigmoid((blockdiag(u, wa) + ba).astype(jnp.float32))
    ig = jax.nn.sigmoid((blockdiag(u, wx) + bx).astype(jnp.float32))
    log_a = -LRU_C * r * jax.nn.softplus(-lam.astype(jnp.float32))
    a = jnp.exp(log_a)
    bterm = jnp.sqrt(-jnp.expm1(2.0 * log_a)) * ig * u.astype(jnp.float32)
    return a, bterm


def linear_scan(a, b, h0, reverse):
    def step(h, ab):
        a_t, b_t = ab
        h = a_t * h + b_t
        return h, h
    h_fin, ys = lax.scan(step, h0, (a.swapaxes(0, 1), b.swapaxes(0, 1)), reverse=reverse)
    return ys.swapaxes(0, 1), h_fin


def odd_mixer(a_lat, a_ctx, w_in, conv_w, conv_b, wa, ba, wx, bx, lam, w_out, need_ctx):
    xl, gl = split_cols(a_lat @ w_in, (LRU_WIDTH, LRU_WIDTH))
    if need_ctx:
        xc, gc = split_cols(a_ctx @ w_in, (LRU_WIDTH, LRU_WIDTH))
    else:
        xc = a_ctx @ w_in[:, :LRU_WIDTH]
    ul = dwconv(xl, conv_w, conv_b, LRU_CONV_PAD)
    uc = dwconv(xc, conv_w, conv_b, LRU_CONV_PAD)
    h0 = jnp.zeros((a_ctx.shape[0], LRU_WIDTH), jnp.float32)
    y_lat = jnp.zeros(ul.shape, jnp.float32)
    y_ctx = jnp.zeros(uc.shape, jnp.float32)
    for d, rev in ((0, False), (1, True)):
        ac, bc = rglru_coeffs(uc, wa[d], ba[d], wx[d], bx[d], lam[d])
        hc, hc_fin = linear_scan(ac, bc, h0, rev)
        al, bl = rglru_coeffs(ul, wa[d], ba[d], wx[d], bx[d], lam[d])
        hl, _ = linear_scan(al, bl, hc_fin, rev)
        y_lat = y_lat + hl
        y_ctx = y_ctx + hc
    out_lat = (y_lat.astype(a_lat.dtype) * jax.nn.gelu(gl)) @ w_out
    if not need_ctx:
        return out_lat, None
    out_ctx = (y_ctx.astype(a_ctx.dtype) * jax.nn.gelu(gc)) @ w_out
    return out_lat, out_ctx


def ec_moe(h, w_router, w_gate, w_up, w_down):
    n = h.shape[1]
    cap = max(1, EC_FACTOR * n // N_EXPERTS)
    logits = jnp.einsum('bnd,de->bne', h, w_router).astype(jnp.float32)
    aff = jax.nn.softmax(logits, axis=-1)
    g, idx = lax.top_k(aff.transpose(0, 2, 1), cap)
    xs = jax.vmap(lambda hb, ib: hb[ib])(h, idx)
    hid = jax.nn.silu(jnp.einsum('becd,edf->becf', xs, w_gate)) * jnp.einsum('becd,edf->becf', xs, w_up)
    out = jnp.einsum('becf,efd->becd', hid, w_down) * g[..., None].astype(h.dtype)
    d = h.shape[-1]
    return jax.vmap(lambda ob, ib: jnp.zeros((n, d), ob.dtype).at[ib.reshape(-1)].add(ob.reshape(-1, d)))(out, idx)


def setup_inputs(seed: int = 0) -> dict:
    key = jax.random.key(seed)
    ks = jax.random.split(key, 32)
    f32 = jnp.float32
    D = D_MODEL

    def nrm(k, shape, scale):
        return jax.random.normal(k, shape, f32) * scale

    u = jax.random.uniform(ks[20], (N_ODD, 2, LRU_WIDTH), f32, 0.9, 0.999)
    a_base = u ** (1.0 / LRU_C)
    od_lam = jnp.log(a_base) - jnp.log1p(-a_base)
    return {
        "x": nrm(ks[0], (BATCH, SEQ, D), 1.0),
        "c": nrm(ks[1], (BATCH, D), 1.0),
        "ctx": nrm(ks[2], (BATCH, CTX_LEN, D), 1.0),
        "c_ctx": nrm(ks[3], (D,), 1.0),
        "mod_w": nrm(ks[4], (DEPTH, D, 6 * D), D ** -0.5),
        "mod_b": nrm(ks[5], (DEPTH, 6 * D), 0.01),
        "norm1_g": 1.0 + nrm(ks[6], (DEPTH, D), 0.02),
        "norm2_g": 1.0 + nrm(ks[7], (DEPTH, D), 0.02),
        "ev_w_in": nrm(ks[8], (N_EVEN, D, EVEN_IN), D ** -0.5),
        "ev_q_norm": 1.0 + nrm(ks[9], (N_EVEN, HEAD_DIM), 0.02),
        "ev_k_norm": 1.0 + nrm(ks[10], (N_EVEN, HEAD_DIM), 0.02),
        "ev_conv_w": nrm(ks[11], (N_EVEN, SCONV_K, SCONV_WIDTH), SCONV_K ** -0.5),
        "ev_conv_b": nrm(ks[12], (N_EVEN, SCONV_WIDTH), 0.01),
        "ev_w_out": nrm(ks[13], (N_EVEN, EVEN_OUT, D), EVEN_OUT ** -0.5),
        "od_w_in": nrm(ks[14], (N_ODD, D, 2 * LRU_WIDTH), D ** -0.5),
        "od_conv_w": nrm(ks[15], (N_ODD, LRU_CONV_K, LRU_WIDTH), LRU_CONV_K ** -0.5),
        "od_conv_b": nrm(ks[16], (N_ODD, LRU_WIDTH), 0.01),
        "od_wa": nrm(ks[17], (N_ODD, 2, N_LRU_BLOCKS, LRU_BLOCK, LRU_BLOCK), LRU_BLOCK ** -0.5),
        "od_ba": nrm(ks[18], (N_ODD, 2, LRU_WIDTH), 0.01),
        "od_wx": nrm(ks[19], (N_ODD, 2, N_LRU_BLOCKS, LRU_BLOCK, LRU_BLOCK), LRU_BLOCK ** -0.5),
        "od_bx": nrm(ks[21], (N_ODD, 2, LRU_WIDTH), 0.01),
        "od_lam": od_lam,
        "od_w_out": nrm(ks[22], (N_ODD, LRU_WIDTH, D), LRU_WIDTH ** -0.5),
        "moe_router": nrm(ks[23], (DEPTH, D, N_EXPERTS), D ** -0.5),
        "moe_w_gate": nrm(ks[24], (DEPTH, N_EXPERTS, D, EXPERT_FF), D ** -0.5),
        "moe_w_up": nrm(ks[25], (DEPTH, N_EXPERTS, D, EXPERT_FF), D ** -0.5),
        "moe_w_down": nrm(ks[26], (DEPTH, N_EXPERTS, EXPERT_FF, D), EXPERT_FF ** -0.5),
        "final_norm_g": 1.0 + nrm(ks[27], (D,), 0.02),
    }


def reference(x, c, ctx, c_ctx, mod_w, mod_b, norm1_g, norm2_g, ev_w_in, ev_q_norm, ev_k_norm,
              ev_conv_w, ev_conv_b, ev_w_out, od_w_in, od_conv_w, od_conv_b, od_wa, od_ba, od_wx,
              od_bx, od_lam, od_w_out, moe_router, moe_w_gate, moe_w_up, moe_w_down, final_norm_g):
    n = x.shape[1]
    rows = n // GRID_W
    row = jnp.repeat(jnp.arange(rows), GRID_W)
    col = jnp.tile(jnp.arange(GRID_W), rows)
    cos_r, sin_r = rope_tables(row)
    cos_c, sin_c = rope_tables(col)
    rope = (cos_r, sin_r, cos_c, sin_c)

    h_lat = x
    h_ctx = ctx
    for i in range(DEPTH):
        need_ctx = i < DEPTH - 1
        ml = [m[:, None, :] for m in adaln(c, mod_w[i], mod_b[i])]
        mc = adaln(c_ctx, mod_w[i], mod_b[i])
        a_lat = modulate(rmsnorm(h_lat, norm1_g[i]), ml[0], ml[1])
        a_ctx = modulate(rmsnorm(h_ctx, norm1_g[i]), mc[0], mc[1])
        if i % 2 == 0:
            j = i // 2
            y_lat, y_ctx = even_mixer(a_lat, a_ctx, ev_w_in[j], ev_q_norm[j], ev_k_norm[j],
                                      ev_conv_w[j], ev_conv_b[j], ev_w_out[j], rope, need_ctx)
        else:
            j = i // 2
            y_lat, y_ctx = odd_mixer(a_lat, a_ctx, od_w_in[j], od_conv_w[j], od_conv_b[j], od_wa[j],
                                     od_ba[j], od_wx[j], od_bx[j], od_lam[j], od_w_out[j], need_ctx)
        h_lat = h_lat + ml[2] * y_lat
        b_lat = modulate(rmsnorm(h_lat, norm2_g[i]), ml[3], ml[4])
        h_lat = h_lat + ml[5] * ec_moe(b_lat, moe_router[i], moe_w_gate[i], moe_w_up[i], moe_w_down[i])
        if need_ctx:
            h_ctx = h_ctx + mc[2] * y_ctx
            b_ctx = modulate(rmsnorm(h_ctx, norm2_g[i]), mc[3], mc[4])
            h_ctx = h_ctx + mc[5] * ec_moe(b_ctx, moe_router[i], moe_w_gate[i], moe_w_up[i], moe_w_down[i])
    return rmsnorm(h_lat, final_norm_g)
```

```python
import time
import concourse.bass_utils as _bu
import contextlib
import numpy as np
import concourse.bass as bass
import concourse.mybir as mybir

F32 = mybir.dt.float32
BF16 = mybir.dt.bfloat16
I32 = mybir.dt.int32
U32 = mybir.dt.uint32
ALU = mybir.AluOpType
AF = mybir.ActivationFunctionType
AX = mybir.AxisListType

ENGS = ("tensor", "vector", "scalar", "gpsimd", "sync")


class Buf:
    def __init__(self, P, name, ap_fn):
        self.P = P
        self.name = name
        self._ap = ap_fn
        self.last_w = None
        self.reads = []
        self.sem = None
        self.cnt = 0

    def __getitem__(self, idx):
        return self._ap()[idx]

    @property
    def ap(self):
        return self._ap()


class Prog:
    def __init__(self):
        self.nc = bass.Bass("TRN2", target_bir_lowering=False)
        self.es = contextlib.ExitStack()
        self.q = {e: [] for e in ENGS}
        self.esem = {}
        self.ecnt = {e: 0 for e in ENGS}
        for e in ENGS:
            self.esem[e] = self.es.enter_context(self.nc.semaphore("S_" + e))
        self.waited = {e: {} for e in ENGS}
        self.semobjs = {}
        self.nbuf = 0
        self.dma_bufs = []

    def sb(self, name, shape, dt, es=None):
        t = (es or self.es).enter_context(self.nc.sbuf_tensor(name, list(shape), dt))
        return Buf(self, name, lambda: t)

    def ps(self, name, shape, dt, es=None):
        t = (es or self.es).enter_context(self.nc.psum_tensor(name, list(shape), dt))
        return Buf(self, name, lambda: t)

    def dram(self, name, shape, dt, kind=None):
        if kind is None:
            t = self.nc.dram_tensor(name, list(shape), dt)
        else:
            t = self.nc.dram_tensor(name, list(shape), dt, kind=kind)
        return Buf(self, name, lambda: t.ap())

    def _bufsem(self, b):
        if b.sem is None:
            b.sem = self.es.enter_context(self.nc.semaphore("D_%d" % self.nbuf))
            self.nbuf += 1
            self.dma_bufs.append(b)
        return b.sem

    def _deps(self, eng, reads, writes):
        evs = []
        for b in reads:
            if b.last_w is not None:
                evs.append(b.last_w)
        for b in writes:
            if b.last_w is not None:
                evs.append(b.last_w)
            evs.extend(b.reads)
        need = {}
        for (sem, val) in evs:
            if eng == "tensor" and sem is self.esem["tensor"]:
                continue
            k = id(sem)
            if self.waited[eng].get(k, 0) >= val:
                continue
            if k not in need or need[k][1] < val:
                need[k] = (sem, val)
        for k, (sem, val) in need.items():
            self.waited[eng][k] = val
        return list(need.values())

    def _mark(self, ev, reads, writes):
        for b in reads:
            b.reads.append(ev)
            if len(b.reads) > 64:
                best = {}
                for (s, v) in b.reads:
                    if id(s) not in best or best[id(s)][1] < v:
                        best[id(s)] = (s, v)
                b.reads = list(best.values())
        for b in writes:
            b.last_w = ev
            b.reads = []

    def op(self, eng, fn, reads=(), writes=()):
        waits = self._deps(eng, reads, writes)
        self.ecnt[eng] += 1
        ev = (self.esem[eng], self.ecnt[eng])
        self.q[eng].append((waits, fn, ev[0], 1))
        self._mark(ev, reads, writes)
        return ev

    def dma(self, eng, fn, dst, reads=(), extra_writes=(), disjoint=False):
        sem = self._bufsem(dst)
        waits = self._deps(eng, reads, ([] if disjoint else [dst]) + list(extra_writes))
        dst.cnt += 16
        ev = (sem, dst.cnt)
        self.q[eng].append((waits, fn, sem, 16))
        self._mark(ev, reads, [dst] + list(extra_writes))
        return ev

    def cc(self, fn, dst, reads=()):
        sem = self._bufsem(dst)
        waits = self._deps("gpsimd", reads, [dst])
        dst.cnt += 1
        ev = (sem, dst.cnt)
        self.q["gpsimd"].append((waits, fn, sem, None))
        self._mark(ev, reads, [dst])
        return ev

    def barrier(self):
        evs = [(self.esem[e], self.ecnt[e]) for e in ENGS if self.ecnt[e] > 0]
        evs += [(b.sem, b.cnt) for b in self.dma_bufs if b.cnt > 0]
        for eng in ENGS:
            waits = []
            for (sem, val) in evs:
                if sem is self.esem[eng]:
                    continue
                k = id(sem)
                if self.waited[eng].get(k, 0) >= val:
                    continue
                self.waited[eng][k] = val
                waits.append((sem, val))
            if waits:
                self.q[eng].append((waits, None, None, 0))

    def wait_all(self, eng, bufs):
        waits = []
        for b in bufs:
            if b.last_w is not None:
                waits.append(b.last_w)
        self.q[eng].append((waits, None, None, 0))

    def build(self):
        nc = self.nc
        with nc.Block() as block:
            def mk(engname):
                items = self.q[engname]

                def body(e):
                    for (waits, fn, sem, inc) in items:
                        for (s, v) in waits:
                            e.wait_ge(s, v)
                        if fn is None:
                            continue
                        ins = fn(e)
                        if inc is None:
                            ins.then_inc(sem)
                        else:
                            ins.then_inc(sem, inc)
                return body
            block.tensor(mk("tensor"))
            block.vector(mk("vector"))
            block.scalar(mk("scalar"))
            block.gpsimd(mk("gpsimd"))
            block.sync(mk("sync"))
        self.es.close()
        return nc


import contextlib, math
import numpy as np

EPS = 1e-6


class Cfg:
    def __init__(self, D=2048, S=4096, C=256):
        self.D, self.S, self.C = D, S, C
        self.B, self.NE, self.L = 4, 16, 2
        self.DC = D // 128
        self.AW = D // 2; self.HQ = self.AW // 128; self.HKV = self.HQ // 4; self.KVW = self.HKV * 128
        self.SW = D // 2; self.SC = self.SW // 128
        self.EIN = self.AW + 2 * self.KVW + 3 * self.SW
        self.LW = 5 * D // 4; self.LC = self.LW // 128; self.NLB = self.LW // 256
        self.FF = D; self.FC = self.FF // 128
        self.SH, self.CH = S // 2, C // 2
        self.NK = S + C
        self.cap = 2 * S // 16; self.capc = 2 * C // 16
        self.D8 = D // 8
        self.NOWN = self.SH + self.CH


def build_mod(cfg):
    c = cfg; D, DC = c.D, c.DC
    P = Prog()
    din = lambda n, s, dt=F32: P.dram(n, s, dt, kind="ExternalInput")
    c5 = din("c5", [5, D]); modw = din("modw", [2, D, 6 * c.D8]); modb = din("modb", [2, 6 * c.D8])
    ident_in = din("ident", [128, 128])
    mout = P.dram("mloc_out", [5, 2 * 6 * c.D8], F32, kind="ExternalOutput")
    PS = [P.ps("ps%d" % i, [128, 512], F32) for i in range(4)]
    ident = P.sb("identf", [128, 128], F32)
    P.dma("sync", lambda e, o=ident[:], i=ident_in[:]: e.dma_start(out=o, in_=i), ident)
    c5t = P.sb("c5t", [5, D], F32); cT = P.sb("cT", [128, DC, 5], F32)
    P.dma("sync", lambda e, o=c5t[:], i=c5[:]: e.dma_start(out=o, in_=i), c5t)
    P.op("scalar", lambda e, o=c5t[:], i=c5t[:]: e.activation(out=o, in_=i, func=AF.Silu), [c5t], [c5t])
    for k in range(DC):
        pt = PS[k % 2]
        P.op("tensor", lambda e, o=pt[:, 0:5], i=c5t[:, k * 128:(k + 1) * 128], idn=ident[0:5, 0:5]:
             e.transpose(out=o, in_=i, identity=idn), [c5t, ident], [pt])
        P.op("vector", lambda e, o=cT[:, k, :], i=pt[:, 0:5]: e.tensor_copy(out=o, in_=i), [pt], [cT])
    ncol = 6 * c.D8
    mloc = P.sb("mloc", [5, 2 * ncol], F32); mbt = P.sb("mbt", [5, 2 * ncol], F32)
    P.dma("sync", lambda e, o=mbt[:], i=modb.ap.rearrange("l n -> (l n)").partition_broadcast(5): e.dma_start(out=o, in_=i), mbt)
    wst = [P.sb("mw%d" % i, [128, DC, 256], F32) for i in range(2)]
    ci = 0
    for l in range(2):
        for n0 in range(0, ncol, 256):
            w = wst[ci % 2]; pm = PS[2 + ci % 2]; ci += 1
            P.dma("sync", lambda e, o=w[:], i=modw.ap[l, :, n0:n0 + 256].rearrange("(k p) n -> p k n", p=128): e.dma_start(out=o, in_=i), w)
            for k in range(DC):
                P.op("tensor", lambda e, o=pm[0:5, 0:256], a=cT[:, k, :], b=w[:, k, :], k=k:
                     e.matmul(o, lhsT=a, rhs=b, start=(k == 0), stop=(k == DC - 1)), [cT, w], [pm])
            P.op("vector", lambda e, o=mloc[:, l * ncol + n0:l * ncol + n0 + 256], a=pm[0:5, 0:256],
                 b=mbt[:, l * ncol + n0:l * ncol + n0 + 256]: e.tensor_tensor(out=o, in0=a, in1=b, op=ALU.add), [pm, mbt], [mloc])
    P.dma("sync", lambda e, o=mout.ap, i=mloc[:]: e.dma_start(out=o, in_=i), mout, [mloc])
    P.wait_all("sync", [mout])
    return P.build(), ["mloc_out"]


def build(cfg, which, layer=0, dbg=False, upto=99):
    c = cfg
    D, S, C, DC, SH, CH, NK = c.D, c.S, c.C, c.DC, c.SH, c.CH, c.NK
    P = Prog(); nc = P.nc
    din = lambda n, s, dt=F32: P.dram(n, s, dt, kind="ExternalInput")
    dout = lambda n, s, dt=F32: P.dram(n, s, dt, kind="ExternalOutput")
    outs = []
    modrows = din("modrows", [2, 2 * 6 * D])
    n1g = din("n1g", [2, D]); n2g = din("n2g", [2, D])
    ident_in = din("ident", [128, 128])
    PS = [P.ps("ps%d" % i, [128, 512], F32) for i in range(8)]
    rr = {}

    def psn(group, banks):
        i = rr.get(group, 0); rr[group] = i + 1
        return PS[banks[i % len(banks)]]

    ident = P.sb("identf", [128, 128], F32)
    P.dma("sync", lambda e, o=ident[:], i=ident_in[:]: e.dma_start(out=o, in_=i), ident)
    identb = P.sb("identb", [128, 128], BF16)
    P.op("vector", lambda e, o=identb[:], i=ident[:]: e.tensor_copy(out=o, in_=i), [ident], [identb])
    selt = P.sb("selt", [2, 256], F32)
    P.op("vector", lambda e, o=selt[:]: e.memset(o, 0.0), [], [selt])
    selin = din("selin", [2, 256])
    P.dma("sync", lambda e, o=selt[:], i=selin.ap: e.dma_start(out=o, in_=i), selt)
    bvn = [0]

    def bcast_vec(dst, l, j, which):
        tes = contextlib.ExitStack()
        bvn[0] += 1
        mv = P.sb("mv%d" % bvn[0], [2, D], F32, tes)
        P.dma("sync", lambda e, o=mv[:], i=modrows.ap[:, (l * 6 + j) * D:(l * 6 + j + 1) * D]: e.dma_start(out=o, in_=i), mv)
        if j in (1, 4):
            gv = P.sb("gv%d" % bvn[0], [2, D], F32, tes)
            gsrc = n1g if j == 1 else n2g
            P.dma("sync", lambda e, o=gv[:], i=gsrc.ap[l:l + 1, :].partition_broadcast(2): e.dma_start(out=o, in_=i), gv)
            P.op("vector", lambda e, o=mv[:], a=mv[:], b=gv[:]:
                 e.scalar_tensor_tensor(out=o, in0=a, scalar=1.0, in1=b, op0=ALU.add, op1=ALU.mult), [mv, gv], [mv])
        for n0 in range(0, D, 512):
            pb = psn("m", [2, 3])
            P.op("tensor", lambda e, o=pb[:, :], a=selt[:, which * 128:(which + 1) * 128], b=mv[:, n0:n0 + 512]:
                 e.matmul(o, lhsT=a, rhs=b, start=True, stop=True), [selt, mv], [pb])
            P.op("scalar", lambda e, o=dst[:, n0:n0 + 512], i=pb[:, :]: e.copy(out=o, in_=i), [pb], [dst])
        P.barrier(); tes.close()

    def norm_tile(xt, gs, sh, at, small):
        P.op("scalar", lambda e, o=at[:], i=xt[:], a=small[:, 0:1]: e.activation(out=o, in_=i, func=AF.Square, accum_out=a),
             [xt], [at, small])
        P.op("scalar", lambda e, o=small[:, 1:2], i=small[:, 0:1]: e.activation(out=o, in_=i, func=AF.Sqrt, scale=1.0 / D, bias=EPS),
             [small], [small])
        P.op("vector", lambda e, o=small[:, 2:3], i=small[:, 1:2]: e.reciprocal(out=o, in_=i), [small], [small])
        P.op("vector", lambda e, o=at[:], a=xt[:], s=small[:, 2:3], b=gs[:]:
             e.scalar_tensor_tensor(out=o, in0=a, scalar=s, in1=b, op0=ALU.mult, op1=ALU.mult), [xt, small, gs], [at])
        if sh is not None:
            P.op("gpsimd", lambda e, o=at[:], a=at[:], b=sh[:]: e.tensor_tensor(out=o, in0=a, in1=b, op=ALU.add), [at, sh], [at])

    def transpose_to(at, dstT, col0, ncols_part=128, dt_ident=None):
        for k4 in range(0, DC, 4):
            pt = psn("t", [0, 1])
            for kk in range(4):
                k = k4 + kk
                P.op("tensor", lambda e, o=pt[:, kk * 128:(kk + 1) * 128], i=at[:, k * 128:(k + 1) * 128], idn=ident[:]:
                     e.transpose(out=o, in_=i, identity=idn), [at, ident], [pt])
            P.op("scalar", lambda e, o=dstT[:, k4:k4 + 4, col0:col0 + 128], i=pt[:].rearrange("p (k t) -> p k t", k=4):
                 e.copy(out=o, in_=i), [pt], [dstT])


    def wload(dst, src_ap):
        import os
        if os.environ.get("NOWL"):
            P.op("vector", lambda e, o=dst[:]: e.memset(o, 0.01), [], [dst]); return
        P.dma("gpsimd", lambda e, o=dst[:], i=src_ap.rearrange("(k p) n -> p k n", p=128): e.dma_start(out=o, in_=i), dst)

    def pass_out(l, srcT, KC, w_all, hsrc, hrow0, nrows, cbound, hdst, bdst, affdst, dbgname=None):
        es = contextlib.ExitStack()
        Wo = P.sb("Wo", [128, KC, D], BF16, es)
        wload(Wo, w_all.ap)
        nw = 2 if cbound > 0 else 1
        g1 = [P.sb("g1_%d" % w, [128, D], F32, es) for w in range(nw)]
        gs2 = [P.sb("gs2_%d" % w, [128, D], F32, es) for w in range(nw)]
        sh2 = [P.sb("sh2_%d" % w, [128, D], F32, es) for w in range(nw)]
        for w in range(nw):
            bcast_vec(g1[w], l, 2, w); bcast_vec(gs2[w], l, 4, w); bcast_vec(sh2[w], l, 3, w)
        wr = P.sb("wr", [128, DC, 16], F32, es)
        P.dma("sync", lambda e, o=wr[:], i=router.ap.rearrange("(k p) n -> p k n", p=128): e.dma_start(out=o, in_=i), wr)
        NBF = 1 if D > 1024 else 2
        cts = [P.sb("ct%d" % i, [128, KC, 512], BF16, es) for i in range(NBF)]
        xts = [P.sb("oxt%d" % i, [128, D], F32, es) for i in range(NBF)]
        hts = [P.sb("oht%d" % i, [128, D], F32, es) for i in range(NBF)]
        bts = [P.sb("obt%d" % i, [128, D], F32, es) for i in range(NBF)]
        bbs = [P.sb("obb%d" % i, [128, D], BF16, es) for i in range(NBF)]
        bTs = [P.sb("obT%d" % i, [128, DC, 128], F32, es) for i in range(NBF)]
        sms = [P.sb("osm%d" % i, [128, 4], F32, es) for i in range(2)]
        exs = [P.sb("oex%d" % i, [128, 16], F32, es) for i in range(2)]
        affTt = P.sb("affTt", [16, nrows], F32, es)
        ti = 0; ni = 0
        for r0 in range(0, nrows, 512):
            nrow = min(512, nrows - r0)
            T = cts[ti % NBF]; ti += 1
            P.dma("sync", lambda e, o=T[:, :, 0:nrow], i=srcT.ap[:, :, r0:r0 + nrow]: e.dma_start(out=o, in_=i), T, [srcT])
            for sub in range(0, nrow, 128):
                rr0 = r0 + sub
                w = 1 if rr0 < cbound else 0
                xt, ht, bt, bb, bT, sm, ex = xts[ni % NBF], hts[ni % NBF], bts[ni % NBF], bbs[ni % NBF], bTs[ni % NBF], sms[ni % 2], exs[ni % 2]; ni += 1
                P.dma("sync", lambda e, o=xt[:], i=hsrc.ap[hrow0 + rr0:hrow0 + rr0 + 128, :]: e.dma_start(out=o, in_=i), xt, [hsrc])
                for n0 in range(0, D, 512):
                    po = psn("o", [2, 3, 4])
                    for k in range(KC):
                        P.op("tensor", lambda e, o=po[:, :], a=T[:, k, sub:sub + 128], b=Wo[:, k, n0:n0 + 512], k=k:
                             e.matmul(o, lhsT=a, rhs=b, start=(k == 0), stop=(k == KC - 1)), [T, Wo], [po])
                    P.op("vector", lambda e, o=ht[:, n0:n0 + 512], a=po[:, :], b=g1[w][:, n0:n0 + 512]:
                         e.tensor_tensor(out=o, in0=a, in1=b, op=ALU.mult), [po, g1[w]], [ht])
                P.op("gpsimd", lambda e, o=ht[:], a=ht[:], b=xt[:]: e.tensor_tensor(out=o, in0=a, in1=b, op=ALU.add), [ht, xt], [ht])
                P.dma("sync", lambda e, o=hdst.ap[rr0:rr0 + 128, :], i=ht[:]: e.dma_start(out=o, in_=i), hdst, [ht])
                norm_tile(ht, gs2[w], sh2[w], bt, sm)
                P.op("scalar", lambda e, o=bb[:], a=bt[:]: e.copy(out=o, in_=a), [bt], [bb])
                P.dma("sync", lambda e, o=bdst.ap[rr0:rr0 + 128, :], i=bb[:]: e.dma_start(out=o, in_=i), bdst, [bb])
                transpose_to(bt, bT, 0)
                pr = psn("r", [5, 6])
                for k in range(DC):
                    P.op("tensor", lambda e, o=pr[:, 0:16], a=bT[:, k, :], b=wr[:, k, :], k=k:
                         e.matmul(o, lhsT=a, rhs=b, start=(k == 0), stop=(k == DC - 1)), [bT, wr], [pr])
                P.op("vector", lambda e, o=sm[:, 0:1], a=pr[:, 0:16]: e.tensor_reduce(out=o, in_=a, axis=AX.X, op=ALU.max, negate=True), [pr], [sm])
                P.op("scalar", lambda e, o=ex[:], a=pr[:, 0:16], b=sm[:, 0:1], s=sm[:, 1:2]:
                     e.activation(out=o, in_=a, func=AF.Exp, bias=b, accum_out=s), [pr, sm], [ex, sm])
                P.op("vector", lambda e, o=sm[:, 2:3], a=sm[:, 1:2]: e.reciprocal(out=o, in_=a), [sm], [sm])
                P.op("vector", lambda e, o=ex[:], a=ex[:], s=sm[:, 2:3]: e.tensor_scalar(out=o, in0=a, scalar1=s, scalar2=None, op0=ALU.mult), [ex, sm], [ex])
                pt = psn("t", [0, 1])
                P.op("tensor", lambda e, o=pt[0:16, 0:128], a=ex[:], idn=ident[:]: e.transpose(out=o, in_=a, identity=idn), [ex, ident], [pt])
                P.op("scalar", lambda e, o=affTt[:, rr0:rr0 + 128], a=pt[0:16, 0:128]: e.copy(out=o, in_=a), [pt], [affTt])
        P.dma("sync", lambda e, o=affdst.ap, i=affTt[:]: e.dma_start(out=o, in_=i), affdst, [affTt])
        P.barrier(); es.close()


    def combine(l, hsrc, nrows, cbound, slots_in, ridx_in, slotsc_in, ridxc_in, acc, hdst, post=None):
        es = contextlib.ExitStack()
        nw = 2 if cbound > 0 else 1
        g2 = [P.sb("cg2_%d" % w, [128, D], F32, es) for w in range(nw)]
        for w in range(nw):
            bcast_vec(g2[w], l, 5, w)
        zt = P.sb("czt", [128, D], F32, es)
        P.op("vector", lambda e, o=zt[:]: e.memset(o, 0.0), [], [zt])
        for r0 in range(0, nrows, 128):
            P.dma("sync", lambda e, o=acc.ap[r0:r0 + 128, :], i=zt[:]: e.dma_start(out=o, in_=i), acc, [zt], disjoint=True)
        sts = [P.sb("cst%d" % i, [128, D], F32, es) for i in range(2)]
        its = [P.sb("cit%d" % i, [128, 1], I32, es) for i in range(2)]
        n = 0
        NT_ = c.cap // 128
        for e_ in range(16):
            tl = [(slots_in, ridx_in, j * 128, 128) for j in range(NT_)]
            if slotsc_in is not None:
                tl.append((slotsc_in, ridxc_in, 0, c.capc))
            for (sl, ri, j0, ns) in tl:
                st = sts[n % 2]; it = its[n % 2]; n += 1
                P.dma("sync", lambda e, o=st[0:ns, :], i=sl.ap[e_, j0:j0 + ns, :]: e.dma_start(out=o, in_=i), st)
                P.dma("sync", lambda e, o=it[0:ns, :], i=ri.ap[e_:e_ + 1, j0:j0 + ns].rearrange("o n -> n o"): e.dma_start(out=o, in_=i), it)
                P.dma("gpsimd", lambda e, ix=it[0:ns, 0:1], i=st[0:ns, :]: e.indirect_dma_start(
                    out=acc.ap, out_offset=bass.IndirectOffsetOnAxis(ap=ix, axis=0), in_=i, in_offset=None, compute_op=ALU.add),
                    acc, [st, it])
        hts = [P.sb("cht%d" % i, [128, D], F32, es) for i in range(2)]
        ats_ = [P.sb("cat%d" % i, [128, D], F32, es) for i in range(2)]
        n = 0
        for r0 in range(0, nrows, 128):
            w = 1 if r0 < cbound else 0
            ht = hts[n % 2]; at = ats_[n % 2]; n += 1
            P.dma("sync", lambda e, o=ht[:], i=hsrc.ap[r0:r0 + 128, :]: e.dma_start(out=o, in_=i), ht, [hsrc])
            P.dma("sync", lambda e, o=at[:], i=acc.ap[r0:r0 + 128, :]: e.dma_start(out=o, in_=i), at, [acc])
            P.op("vector", lambda e, o=at[:], a=at[:], b=g2[w][:]: e.tensor_tensor(out=o, in0=a, in1=b, op=ALU.mult), [at, g2[w]], [at])
            P.op("gpsimd", lambda e, o=ht[:], a=ht[:], b=at[:]: e.tensor_tensor(out=o, in0=a, in1=b, op=ALU.add), [ht, at], [ht])
            if post is None:
                P.dma("sync", lambda e, o=hdst.ap[r0:r0 + 128, :], i=ht[:]: e.dma_start(out=o, in_=i), hdst, [ht], disjoint=True)
            else:
                post(ht, r0, at)
        P.barrier(); es.close()

    if which == "final":
        h2 = din("h2", [S, D]); slots_in = din("slots_b", [16, c.cap, D]); ridx_in = din("ridx_b", [16, c.cap], I32)
        fng = din("fng", [1, D])
        out = dout("out", [S, D]); outs.append("out")
        acc = P.dram("acc", [S, D], F32)
        fgb = P.sb("fgb", [128, D], F32)
        P.dma("sync", lambda e, o=fgb[:], i=fng.ap[0:1, :].partition_broadcast(128): e.dma_start(out=o, in_=i), fgb)
        smf = [P.sb("smf%d" % i, [128, 4], F32) for i in range(2)]
        cnt = [0]

        def post(ht, r0, scratch):
            sm = smf[cnt[0] % 2]; cnt[0] += 1
            norm_tile(ht, fgb, None, scratch, sm)
            P.dma("sync", lambda e, o=out.ap[r0:r0 + 128, :], i=scratch[:]: e.dma_start(out=o, in_=i), out, [scratch], disjoint=True)
        combine(1, h2, S, 0, slots_in, ridx_in, None, None, acc, None, post)
        P.wait_all("sync", [out])
        return P.build(), outs

    if which == "mix1":
        LW, LC, NLB = c.LW, c.LC, c.NLB
        h1 = din("h1", [NK, D]); slots_in = din("slots_b", [16, c.cap, D]); ridx_in = din("ridx_b", [16, c.cap], I32)
        slotsc_in = din("slotsc_b", [16, c.capc, D]); ridxc_in = din("ridxc_b", [16, c.capc], I32)
        od_w_in = din("od_w_in", [D, 2 * LW]); od_w_out = din("od_w_out", [LW, D])
        od_cw = din("od_cw", [4, LW]); od_cb = din("od_cb", [1, LW])
        od_wa = din("od_wa", [2, NLB, 256, 256]); od_wx = din("od_wx", [2, NLB, 256, 256])
        od_vec = din("od_vec", [6, LW])
        router = din("router", [D, 16])
        h2 = dout("h2", [S, D]); blat1 = dout("blat1", [S, D], BF16); affT1 = dout("affT1", [16, S]); outs += ["h2", "blat1", "affT1"]
        acc = P.dram("acc", [NK, D], F32); hl0 = P.dram("hl0", [NK, D], F32)
        aT1 = P.dram("aT1", [128, DC, NK], BF16)
        xT_d = P.dram("xT_d", [LC, 128, NK], F32); gT_d = P.dram("gT_d", [LC, 128, S], BF16)
        ygT = P.dram("ygT", [128, LC, S], BF16)
        combine(0, h1, NK, C, slots_in, ridx_in, slotsc_in, ridxc_in, acc, hl0)
        es = contextlib.ExitStack()
        gs1 = [P.sb("gs1_%d" % w, [128, D], F32, es) for w in range(2)]
        sh1 = [P.sb("sh1_%d" % w, [128, D], F32, es) for w in range(2)]
        for w in range(2):
            bcast_vec(gs1[w], 1, 1, w); bcast_vec(sh1[w], 1, 0, w)
        xts = [P.sb("xt%d" % i, [128, D], F32, es) for i in range(2)]
        ats = [P.sb("at%d" % i, [128, D], F32, es) for i in range(2)]
        smalls = [P.sb("sm%d" % i, [128, 4], F32, es) for i in range(2)]
        aTt = [P.sb("aTt%d" % i, [128, DC, 512], BF16, es) for i in range(2)]
        nt = 0; ti = 0
        for r0 in range(0, NK, 512):
            nrow = min(512, NK - r0)
            T = aTt[ti % 2]; ti += 1
            for sub in range(0, nrow, 128):
                xt = xts[nt % 2]; at = ats[nt % 2]; sm = smalls[nt % 2]; nt += 1
                rr0 = r0 + sub
                P.dma("sync", lambda e, o=xt[:], i=hl0.ap[rr0:rr0 + 128, :]: e.dma_start(out=o, in_=i), xt, [hl0])
                w = 1 if rr0 < C else 0
                norm_tile(xt, gs1[w], sh1[w], at, sm)
                transpose_to(at, T, sub)
            P.dma("sync", lambda e, o=aT1.ap[:, :, r0:r0 + nrow], i=T[:, :, 0:nrow]: e.dma_start(out=o, in_=i), aT1, [T], disjoint=True)
        P.barrier(); es.close()
        es = contextlib.ExitStack()
        Wx_ = [P.sb("pWx%d" % i, [128, DC, 256], BF16, es) for i in range(2)]
        Wg_ = [P.sb("pWg%d" % i, [128, DC, 256], BF16, es) for i in range(2)]
        aTt = [P.sb("aTp%d" % i, [128, DC, 512], BF16, es) for i in range(2)]
        xfull = [P.sb("xfull%d" % i, [128, NK], F32, es) for i in range(2)]
        gfull = [P.sb("gfull%d" % i, [128, NK], BF16, es) for i in range(2)]
        ti = 0
        for cc in range(LC // 2):
            Wx = Wx_[cc % 2]; Wg = Wg_[cc % 2]
            wload(Wx, od_w_in.ap[:, cc * 256:(cc + 1) * 256]); wload(Wg, od_w_in.ap[:, LW + cc * 256:LW + (cc + 1) * 256])
            for r0 in range(0, NK, 512):
                nrow = min(512, NK - r0)
                T = aTt[ti % 2]; ti += 1
                P.dma("sync", lambda e, o=T[:, :, 0:nrow], i=aT1.ap[:, :, r0:r0 + nrow]: e.dma_start(out=o, in_=i), T, [aT1])
                for half in range(2):
                    px = psn("x", [2, 3]); pg = psn("g", [4, 5])
                    for k in range(DC):
                        P.op("tensor", lambda e, o=px[:, 0:nrow], a=Wx[:, k, half * 128:(half + 1) * 128], b=T[:, k, 0:nrow], k=k:
                             e.matmul(o, lhsT=a, rhs=b, start=(k == 0), stop=(k == DC - 1)), [Wx, T], [px])
                    for k in range(DC):
                        P.op("tensor", lambda e, o=pg[:, 0:nrow], a=Wg[:, k, half * 128:(half + 1) * 128], b=T[:, k, 0:nrow], k=k:
                             e.matmul(o, lhsT=a, rhs=b, start=(k == 0), stop=(k == DC - 1)), [Wg, T], [pg])
                    P.op("vector", lambda e, o=xfull[half][:, r0:r0 + nrow], a=px[:, 0:nrow]: e.tensor_copy(out=o, in_=a), [px], [xfull[half]])
                    P.op("scalar", lambda e, o=gfull[half][:, r0:r0 + nrow], a=pg[:, 0:nrow]: e.activation(out=o, in_=a, func=AF.Gelu_apprx_tanh),
                         [pg], [gfull[half]])
            for half in range(2):
                ch = cc * 2 + half
                P.dma("sync", lambda e, o=xT_d.ap[ch], i=xfull[half][:]: e.dma_start(out=o, in_=i), xT_d, [xfull[half]], disjoint=True)
                P.dma("sync", lambda e, o=gT_d.ap[ch], i=gfull[half][:, C:NK]: e.dma_start(out=o, in_=i), gT_d, [gfull[half]], disjoint=True)
        P.barrier(); es.close()
        es = contextlib.ExitStack()
        vr = P.sb("vr", [11, LW], F32, es); colv = P.sb("colv", [128, LC, 11], F32, es)
        P.dma("sync", lambda e, o=vr[0:4, :], i=od_cw.ap: e.dma_start(out=o, in_=i), vr)
        P.dma("sync", lambda e, o=vr[4:5, :], i=od_cb.ap: e.dma_start(out=o, in_=i), vr)
        P.dma("sync", lambda e, o=vr[5:11, :], i=od_vec.ap: e.dma_start(out=o, in_=i), vr)
        for k in range(LC):
            pt = psn("t", [0, 1])
            P.op("tensor", lambda e, o=pt[:, 0:11], a=vr[:, k * 128:(k + 1) * 128], idn=ident[0:11, 0:11]:
                 e.transpose(out=o, in_=a, identity=idn), [vr, ident], [pt])
            P.op("vector", lambda e, o=colv[:, k, :], a=pt[:, 0:11]: e.tensor_copy(out=o, in_=a), [pt], [colv])
        sc8 = P.sb("sc8", [128, LC, 2], F32, es)
        P.op("scalar", lambda e, o=sc8[:], a=colv[:, :, 9:11]: e.activation(out=o, in_=a, func=AF.Exp, scale=-1.0), [colv], [sc8])
        P.op("scalar", lambda e, o=sc8[:], a=sc8[:]: e.activation(out=o, in_=a, func=AF.Ln, bias=1.0), [sc8], [sc8])
        P.op("vector", lambda e, o=sc8[:], a=sc8[:]: e.tensor_scalar(out=o, in0=a, scalar1=-8.0, scalar2=None, op0=ALU.mult), [sc8], [sc8])
        xb = P.sb("xb", [128, NK], F32, es)
        ub = [P.sb("ub%d" % i, [128, NK], F32, es) for i in range(2)]
        ubf = P.sb("ubf", [128, 2, NK], BF16, es)
        afull = P.sb("afull", [128, NK], F32, es); bfull = P.sb("bfull", [128, NK], F32, es); ysc = P.sb("ysc", [128, NK], F32, es)
        ysum = P.sb("ysum", [128, S], F32, es); gt = P.sb("gt", [128, S], BF16, es); ygb = P.sb("ygb", [128, S], BF16, es)
        Wa_t = [P.sb("Wa%d" % i, [128, 2, 256], BF16, es) for i in range(2)]
        Wx_t = [P.sb("Wxx%d" % i, [128, 2, 256], BF16, es) for i in range(2)]
        rt_ = [P.sb("rt_%d" % i, [128, 512], F32, es) for i in range(2)]
        it_ = [P.sb("it_%d" % i, [128, 512], F32, es) for i in range(2)]
        t2_ = [P.sb("t2_%d" % i, [128, 512], F32, es) for i in range(2)]
        wi = 0; ri_ = 0
        SCH = 1024

        def scan_cols(dst, cols_fwd, init_ap, init_buf):
            prev = init_ap; pb = init_buf
            for (a0, n, rev) in cols_fwd:
                if rev:
                    sl = slice(a0 + n - 1, (a0 - 1) if a0 > 0 else None, -1)
                    last = slice(a0, a0 + 1)
                else:
                    sl = slice(a0, a0 + n); last = slice(a0 + n - 1, a0 + n)
                rd = [afull, bfull] + ([pb] if pb is not None else [])
                P.op("vector", lambda e, o=dst[:, sl], d0=afull[:, sl], d1=bfull[:, sl], ini=prev:
                     e.tensor_tensor_scan(out=o, data0=d0, data1=d1, initial=ini, op0=ALU.mult, op1=ALU.add), rd, [dst])
                prev = dst[:, last]; pb = dst
        for hb in range(NLB):
            for jc in range(2):
                ch = 2 * hb + jc
                P.dma("sync", lambda e, o=xb[:], i=xT_d.ap[ch]: e.dma_start(out=o, in_=i), xb, [xT_d])
                u = ub[jc]
                P.op("vector", lambda e, o=u[:], a=xb[:], s1=colv[:, ch, 1:2], s2=colv[:, ch, 4:5]:
                     e.tensor_scalar(out=o, in0=a, scalar1=s1, scalar2=s2, op0=ALU.mult, op1=ALU.add), [xb, colv], [u])
                for (a0, a1) in ((0, C), (C, NK)):
                    for (tap, do, di, ln) in ((0, a0 + 1, a0, a1 - a0 - 1), (2, a0, a0 + 1, a1 - a0 - 1), (3, a0, a0 + 2, a1 - a0 - 2)):
                        P.op("vector", lambda e, o=u[:, do:do + ln], a=xb[:, di:di + ln], s=colv[:, ch, tap:tap + 1], b=u[:, do:do + ln]:
                             e.scalar_tensor_tensor(out=o, in0=a, scalar=s, in1=b, op0=ALU.mult, op1=ALU.add), [xb, colv, u], [u])
                P.op("gpsimd", lambda e, o=ubf[:, jc, :], a=u[:]: e.tensor_copy(out=o, in_=a), [u], [ubf])
            for jc in range(2):
                ch = 2 * hb + jc
                for d in range(2):
                    Wa = Wa_t[wi % 2]; Wx = Wx_t[wi % 2]; wi += 1
                    wload(Wa, od_wa.ap[d, hb]); wload(Wx, od_wx.ap[d, hb])
                    for r0 in range(0, NK, 512):
                        nrow = min(512, NK - r0)
                        pa = psn("x", [2, 3]); px = psn("g", [4, 5])
                        for ic in range(2):
                            P.op("tensor", lambda e, o=pa[:, 0:nrow], a=Wa[:, ic, jc * 128:(jc + 1) * 128], b=ubf[:, ic, r0:r0 + nrow], ic=ic:
                                 e.matmul(o, lhsT=a, rhs=b, start=(ic == 0), stop=(ic == 1)), [Wa, ubf], [pa])
                        for ic in range(2):
                            P.op("tensor", lambda e, o=px[:, 0:nrow], a=Wx[:, ic, jc * 128:(jc + 1) * 128], b=ubf[:, ic, r0:r0 + nrow], ic=ic:
                                 e.matmul(o, lhsT=a, rhs=b, start=(ic == 0), stop=(ic == 1)), [Wx, ubf], [px])
                        rt = rt_[ri_ % 2]; itt = it_[ri_ % 2]; t2 = t2_[ri_ % 2]; ri_ += 1
                        P.op("scalar", lambda e, o=rt[:, 0:nrow], a=pa[:, 0:nrow], b=colv[:, ch, 5 + d:6 + d]:
                             e.activation(out=o, in_=a, func=AF.Sigmoid, bias=b), [pa, colv], [rt])
                        P.op("scalar", lambda e, o=afull[:, r0:r0 + nrow], a=rt[:, 0:nrow], s=sc8[:, ch, d:d + 1]:
                             e.activation(out=o, in_=a, func=AF.Exp, scale=s), [rt, sc8], [afull])
                        P.op("scalar", lambda e, o=itt[:, 0:nrow], a=px[:, 0:nrow], b=colv[:, ch, 7 + d:8 + d]:
                             e.activation(out=o, in_=a, func=AF.Sigmoid, bias=b), [px, colv], [itt])
                        P.op("gpsimd", lambda e, o=t2[:, 0:nrow], a=afull[:, r0:r0 + nrow]: e.tensor_tensor(out=o, in0=a, in1=a, op=ALU.mult), [afull], [t2])
                        P.op("scalar", lambda e, o=t2[:, 0:nrow], a=t2[:, 0:nrow]: e.activation(out=o, in_=a, func=AF.Sqrt, scale=-1.0, bias=1.0), [t2], [t2])
                        P.op("gpsimd", lambda e, o=t2[:, 0:nrow], a=t2[:, 0:nrow], b=itt[:, 0:nrow]: e.tensor_tensor(out=o, in0=a, in1=b, op=ALU.mult), [t2, itt], [t2])
                        P.op("vector", lambda e, o=bfull[:, r0:r0 + nrow], a=t2[:, 0:nrow], b=ub[jc][:, r0:r0 + nrow]:
                             e.tensor_tensor(out=o, in0=a, in1=b, op=ALU.mult), [t2, ub[jc]], [bfull])
                    if d == 0:
                        pieces = [(a0, min(SCH, NK - a0), False) for a0 in range(0, NK, SCH)]
                        scan_cols(ysc, pieces, 0.0, None)
                        P.op("gpsimd", lambda e, o=ysum[:], a=ysc[:, C:NK]: e.tensor_copy(out=o, in_=a), [ysc], [ysum])
                    else:
                        pieces = [(0, C, True)] + [(a0, min(SCH, NK - a0), True) for a0 in range(C + ((NK - C - 1) // SCH) * SCH, C - 1, -SCH)]
                        scan_cols(ysc, pieces, 0.0, None)
                        P.op("gpsimd", lambda e, o=ysum[:], a=ysum[:], b=ysc[:, C:NK]: e.tensor_tensor(out=o, in0=a, in1=b, op=ALU.add), [ysum, ysc], [ysum])
                P.dma("sync", lambda e, o=gt[:], i=gT_d.ap[ch]: e.dma_start(out=o, in_=i), gt, [gT_d])
                P.op("vector", lambda e, o=ygb[:], a=ysum[:], b=gt[:]: e.tensor_tensor(out=o, in0=a, in1=b, op=ALU.mult), [ysum, gt], [ygb])
                P.dma("sync", lambda e, o=ygT.ap[:, ch, :], i=ygb[:]: e.dma_start(out=o, in_=i), ygT, [ygb], disjoint=True)
        P.barrier(); es.close()
        pass_out(1, ygT, LC, od_w_out, hl0, C, S, 0, h2, blat1, affT1)
        P.wait_all("sync", [h2, blat1, affT1])
        return P.build(), outs

    if which == "mix0":
        xin = din("xin", [NK, D])
        ropet = din("ropet", [NK, 128])
        w_in_all = din("ev_w_in", [D, c.EIN]); w_out_all = din("ev_w_out", [D, D])
        evq = din("evq", [1, 128]); evk = din("evk", [1, 128])
        ev_cw = din("ev_cw", [3, c.SW]); ev_cb = din("ev_cb", [1, c.SW])
        router = din("router", [D, 16])
        aT0 = P.dram("aT0", [128, DC, NK], BF16); catT = P.dram("catT", [128, DC, NK], BF16)
        h1 = dout("h1", [NK, D]); blat = dout("blat", [NK, D], BF16); affT = dout("affT", [16, NK])
        outs += ["h1", "blat", "affT"]
        def wload(dst, src_ap):
            P.dma("gpsimd", lambda e, o=dst[:], i=src_ap.rearrange("(k p) n -> p k n", p=128): e.dma_start(out=o, in_=i), dst)

        def bc4(ap2, H):
            return bass.AP(ap2.tensor, ap2.offset, [list(ap2.ap[0]), [0, H], list(ap2.ap[1])])

        es = contextlib.ExitStack()
        gs1 = [P.sb("gs1_%d" % w, [128, D], F32, es) for w in range(2)]
        sh1 = [P.sb("sh1_%d" % w, [128, D], F32, es) for w in range(2)]
        for w in range(2):
            bcast_vec(gs1[w], 0, 1, w); bcast_vec(sh1[w], 0, 0, w)
        xts = [P.sb("xt%d" % i, [128, D], F32, es) for i in range(2)]
        ats = [P.sb("at%d" % i, [128, D], F32, es) for i in range(2)]
        smalls = [P.sb("sm%d" % i, [128, 4], F32, es) for i in range(2)]
        aTt = [P.sb("aTt%d" % i, [128, DC, 512], BF16, es) for i in range(2)]

        def pass_a(src, gsl, shl, dstT, nrows, cbound):
            nt = 0; ti = 0
            for r0 in range(0, nrows, 512):
                nrow = min(512, nrows - r0)
                T = aTt[ti % 2]; ti += 1
                for sub in range(0, nrow, 128):
                    xt = xts[nt % 2]; at = ats[nt % 2]; sm = smalls[nt % 2]; nt += 1
                    rr0 = r0 + sub
                    P.dma("sync", lambda e, o=xt[:], i=src.ap[rr0:rr0 + 128, :]: e.dma_start(out=o, in_=i), xt)
                    w = 1 if rr0 < cbound else 0
                    norm_tile(xt, gsl[w], shl[w], at, sm)
                    transpose_to(at, T, sub)
                P.dma("sync", lambda e, o=dstT.ap[:, :, r0:r0 + nrow], i=T[:, :, 0:nrow]: e.dma_start(out=o, in_=i), dstT, [T])
        if upto <= -3:
            d0 = dout("d0", [128, D]); outs.append("d0")
            P.dma("sync", lambda e, o=d0.ap, i=gs1[0][:]: e.dma_start(out=o, in_=i), d0, [gs1[0]])
            P.wait_all("sync", [d0]); es.close(); return P.build(), outs
        pass_a(xin, gs1, sh1, aT0, NK, C)
        P.barrier(); es.close()
        if upto <= -2:
            d0 = dout("d0", [128, DC, NK], BF16); outs.append("d0")
            P.dma("gpsimd", lambda e, o=d0.ap, i=aT0.ap: e.dma_start(out=o, in_=i), d0, [aT0])
            P.wait_all("sync", [d0]); return P.build(), outs

        es = contextlib.ExitStack()
        HQ, HKV, AW, KVW = c.HQ, c.HKV, c.AW, c.KVW
        NKT = NK // 128
        kT = P.sb("kT", [128, HKV, NK], BF16, es)
        qT = P.sb("qT", [128, HQ, NK], BF16, es)
        Vaug = P.sb("Vaug", [128, NKT, HKV, 132], BF16, es)
        import os
        if not os.environ.get("SKV"):
            onesf = P.sb("onesf", [128, 132], F32, es)
            P.op("vector", lambda e, o=onesf[:]: e.memset(o, 1.0), [], [onesf])
            oa = onesf[:]
            P.op("vector", lambda e, o=Vaug[:].rearrange("p a b c -> p (a b) c"),
                 i=bass.AP(oa.tensor, oa.offset, [list(oa.ap[0]), [0, NKT * HKV], [1, 132]]): e.tensor_copy(out=o, in_=i), [onesf], [Vaug])
        gkb = P.sb("gkb", [128, 128], F32, es); gqb = P.sb("gqb", [128, 128], F32, es)
        if not os.environ.get("SKG"):
            P.dma("sync", lambda e, o=gkb[:], i=evk.ap[0:1, :].partition_broadcast(128): e.dma_start(out=o, in_=i), gkb)
            P.dma("sync", lambda e, o=gqb[:], i=evq.ap[0:1, :].partition_broadcast(128): e.dma_start(out=o, in_=i), gqb)
        es2 = contextlib.ExitStack()
        Wkv = P.sb("Wkv", [128, DC, 2 * KVW], BF16, es2); Wq = P.sb("Wq", [128, DC, AW], BF16, es2)
        wload(Wkv, w_in_all.ap[:, AW:AW + 2 * KVW]); wload(Wq, w_in_all.ap[:, 0:AW])
        NBQ = 1 if D > 1024 else 2
        aTt = [P.sb("aTq%d" % i, [128, DC, 512], BF16, es2) for i in range(NBQ)]
        kf = [P.sb("kf%d" % i, [128, 4 * 128], F32, es2) for i in range(2)]
        sq = [P.sb("sq%d" % i, [128, 4 * 128], F32, es2) for i in range(2)]
        kr = [P.sb("kr%d" % i, [128, 4 * 128], F32, es2) for i in range(2)]
        hs = [P.sb("hs%d" % i, [128, 8], F32, es2) for i in range(2)]
        rts = [P.sb("rt%d" % i, [128, 128], F32, es2) for i in range(2)]
        hn = [0]

        import os
        SKF = os.environ.get("SKF", "")
        _realop = P.op
        def headnorm_rope(ps_ap, H, gb, rt, dstT, h0, tok0):
            i = hn[0] % 2; hn[0] += 1
            cnt = [0]
            class PX:
                def op(self, eng, fn, r=(), w=()):
                    cnt[0] += 1
                    if ("%02d" % cnt[0]) in SKF.split(","):
                        return
                    return _realop(eng, fn, r, w)
            P_ = PX()
            f, s_, r_, h_ = kf[i], sq[i], kr[i], hs[i]
            n = H * 128
            P_.op("vector", lambda e, o=f[:, 0:n], a=ps_ap: e.tensor_copy(out=o, in_=a), [ps_ap_buf[0]], [f])
            P_.op("vector", lambda e, o=s_[:, 0:n], a=f[:, 0:n]: e.tensor_tensor(out=o, in0=a, in1=a, op=ALU.mult), [f], [s_])
            P_.op("vector", lambda e, o=h_[:, 0:H], a=s_[:, 0:n].rearrange("p (h d) -> p h d", h=H):
                 e.tensor_reduce(out=o, in_=a, axis=AX.X, op=ALU.add), [s_], [h_])
            P_.op("scalar", lambda e, o=h_[:, 0:H], a=h_[:, 0:H]: e.activation(out=o, in_=a, func=AF.Sqrt, scale=1.0 / 128, bias=EPS), [h_], [h_])
            P_.op("vector", lambda e, o=h_[:, 0:H], a=h_[:, 0:H]: e.reciprocal(out=o, in_=a), [h_], [h_])
            f3 = f[:, 0:n].rearrange("p (h d) -> p h d", h=H)
            hb_ = h_[:, 0:H]
            rb = bass.AP(hb_.tensor, hb_.offset, [list(hb_.ap[0]), list(hb_.ap[1]), [0, 128]])
            P_.op("vector", lambda e, o=f3, a=f3, b=rb: e.tensor_tensor(out=o, in0=a, in1=b, op=ALU.mult), [f, h_], [f])
            P_.op("vector", lambda e, o=f3, a=f3, b=bc4(gb[:], H): e.tensor_tensor(out=o, in0=a, in1=b, op=ALU.mult), [f, gb], [f])
            f5 = f[:, 0:n].rearrange("p (g t j) -> p g t j", t=2, j=32)
            r5 = r_[:, 0:n].rearrange("p (g t j) -> p g t j", t=2, j=32)
            s5 = s_[:, 0:n].rearrange("p (g t j) -> p g t j", t=2, j=32)
            u1, u2 = f5[:, :, 0, :], f5[:, :, 1, :]
            cs = rt[:, 0:64]; sn = rt[:, 64:128]
            def tb(a2):
                return bass.AP(a2.tensor, a2.offset, [list(a2.ap[0]), [0, H], [32, 2], [1, 32]])
            u1v = bass.AP(u1.tensor, u1.offset, [list(u1.ap[0]), [128, H], [64, 2], [1, 32]])
            u2v = bass.AP(u2.tensor, u2.offset, [list(u2.ap[0]), [128, H], [64, 2], [1, 32]])
            o1 = r5[:, :, 0, :]; o2 = r5[:, :, 1, :]
            o1v = bass.AP(o1.tensor, o1.offset, [list(o1.ap[0]), [128, H], [64, 2], [1, 32]])
            o2v = bass.AP(o2.tensor, o2.offset, [list(o2.ap[0]), [128, H], [64, 2], [1, 32]])
            t1 = s5[:, :, 0, :]; t2 = s5[:, :, 1, :]
            t1v = bass.AP(t1.tensor, t1.offset, [list(t1.ap[0]), [128, H], [64, 2], [1, 32]])
            t2v = bass.AP(t2.tensor, t2.offset, [list(t2.ap[0]), [128, H], [64, 2], [1, 32]])
            P_.op("vector", lambda e, o=o1v, a=u1v, b=tb(cs): e.tensor_tensor(out=o, in0=a, in1=b, op=ALU.mult), [f, rt], [r_])
            P_.op("vector", lambda e, o=t1v, a=u2v, b=tb(sn): e.tensor_tensor(out=o, in0=a, in1=b, op=ALU.mult), [f, rt], [s_])
            P_.op("vector", lambda e, o=o1v, a=o1v, b=t1v: e.tensor_tensor(out=o, in0=a, in1=b, op=ALU.subtract), [r_, s_], [r_])
            P_.op("vector", lambda e, o=o2v, a=u1v, b=tb(sn): e.tensor_tensor(out=o, in0=a, in1=b, op=ALU.mult), [f, rt], [r_])
            P_.op("vector", lambda e, o=t2v, a=u2v, b=tb(cs): e.tensor_tensor(out=o, in0=a, in1=b, op=ALU.mult), [f, rt], [s_])
            P_.op("vector", lambda e, o=o2v, a=o2v, b=t2v: e.tensor_tensor(out=o, in0=a, in1=b, op=ALU.add), [r_, s_], [r_])
            pt = psn("t", [0, 1])
            for h in range(H):
                P_.op("tensor", lambda e, o=pt[:, h * 128:(h + 1) * 128], a=r_[:, h * 128:(h + 1) * 128], idn=ident[:]:
                     e.transpose(out=o, in_=a, identity=idn), [r_, ident], [pt])
            P_.op("scalar", lambda e, o=dstT[:, h0:h0 + H, tok0:tok0 + 128], a=pt[:, 0:n].rearrange("p (h t) -> p h t", h=H):
                 e.copy(out=o, in_=a), [pt], [dstT])

        ps_ap_buf = [None]
        ti = 0; ri = 0
        for r0 in range(0, NK, 512):
            nrow = min(512, NK - r0)
            T = aTt[ti % NBQ]; ti += 1
            P.dma("sync", lambda e, o=T[:, :, 0:nrow], i=aT0.ap[:, :, r0:r0 + nrow]: e.dma_start(out=o, in_=i), T, [aT0])
            for sub in range(0, nrow, 128):
                tok0 = r0 + sub; kt = tok0 // 128
                rt = rts[ri % 2]; ri += 1
                P.dma("sync", lambda e, o=rt[:], i=ropet.ap[tok0:tok0 + 128, :]: e.dma_start(out=o, in_=i), rt)
                pk = psn("p", [2, 3])
                for k in range(DC):
                    P.op("tensor", lambda e, o=pk[:, 0:2 * KVW], a=T[:, k, sub:sub + 128], b=Wkv[:, k, :], k=k:
                         e.matmul(o, lhsT=a, rhs=b, start=(k == 0), stop=(k == DC - 1)), [T, Wkv], [pk])
                for hh in range(HKV):
                    P.op("vector", lambda e, o=Vaug[:, kt, hh, 0:128], a=pk[:, KVW + hh * 128:KVW + (hh + 1) * 128]:
                         e.tensor_copy(out=o, in_=a), [pk], [Vaug])
                ps_ap_buf[0] = pk
                headnorm_rope(pk[:, 0:KVW], HKV, gkb, rt, kT, 0, tok0)
                for q0 in range(0, AW, 512):
                    nq = min(512, AW - q0); Hh = nq // 128
                    pq = psn("p", [2, 3])
                    for k in range(DC):
                        P.op("tensor", lambda e, o=pq[:, 0:nq], a=T[:, k, sub:sub + 128], b=Wq[:, k, q0:q0 + nq], k=k:
                             e.matmul(o, lhsT=a, rhs=b, start=(k == 0), stop=(k == DC - 1)), [T, Wq], [pq])
                    ps_ap_buf[0] = pq
                    headnorm_rope(pq[:, 0:nq], Hh, gqb, rt, qT, q0 // 128, tok0)
        P.barrier(); es2.close()
        if upto <= -1:
            d0 = dout("d0", [128, HQ, NK], BF16); outs.append("d0")
            P.dma("sync", lambda e, o=d0.ap, i=qT[:]: e.dma_start(out=o, in_=i), d0, [qT])
            P.wait_all("sync", [d0]); es.close(); return P.build(), outs

        es2 = contextlib.ExitStack()
        ptb = [P.sb("ptb%d" % i, [128, 512], BF16, es2) for i in range(3)]
        catg = [P.sb("catg%d" % i, [128, HQ, 512], BF16, es2) for i in range(2)]
        ofs = [P.sb("of%d" % i, [128, 128], F32, es2) for i in range(2)]
        rsm = [P.sb("rsm%d" % i, [128, 1], F32, es2) for i in range(2)]
        groups = [(0, C, C)] + [(C + g * 512, 512, NK) for g in range(S // 512)]
        sc = 128 ** -0.5
        pi = 0; oi = 0
        for gi, (q0, nq, nkeys) in enumerate(groups):
            cg = catg[gi % 2]
            nsub = nq // 128
            for hq in range(HQ):
                hk = hq // (HQ // HKV)
                nkt = nkeys // 128
                for kt in range(nkt):
                    sp = psn("s", [2, 3])
                    P.op("tensor", lambda e, o=sp[:, 0:nq], a=kT[:, hk, kt * 128:(kt + 1) * 128], b=qT[:, hq, q0:q0 + nq]:
                         e.matmul(o, lhsT=a, rhs=b, start=True, stop=True), [kT, qT], [sp])
                    pb = ptb[pi % 3]; pi += 1
                    P.op("scalar", lambda e, o=pb[:, 0:nq], a=sp[:, 0:nq]: e.activation(out=o, in_=a, func=AF.Exp, scale=sc), [sp], [pb])
                    for qs in range(nsub):
                        P.op("tensor", lambda e, o=PS[4 + qs][:, 0:129], a=pb[:, qs * 128:(qs + 1) * 128], b=Vaug[:, kt, hk, 0:129], kt=kt:
                             e.matmul(o, lhsT=a, rhs=b, start=(kt == 0), stop=(kt == nkt - 1)), [pb, Vaug], [PS[4 + qs]])
                pt = psn("t", [0, 1])
                for qs in range(nsub):
                    of = ofs[oi % 2]; rs = rsm[oi % 2]; oi += 1
                    P.op("vector", lambda e, o=rs[:], a=PS[4 + qs][:, 128:129]: e.reciprocal(out=o, in_=a), [PS[4 + qs]], [rs])
                    P.op("vector", lambda e, o=of[:], a=PS[4 + qs][:, 0:128], s=rs[:, 0:1]:
                         e.tensor_scalar(out=o, in0=a, scalar1=s, scalar2=None, op0=ALU.mult), [PS[4 + qs], rs], [of])
                    P.op("tensor", lambda e, o=pt[:, qs * 128:(qs + 1) * 128], a=of[:], idn=ident[:]:
                         e.transpose(out=o, in_=a, identity=idn), [of, ident], [pt])
                P.op("scalar", lambda e, o=cg[:, hq, 0:nq], a=pt[:, 0:nq]: e.copy(out=o, in_=a), [pt], [cg])
            P.dma("sync", lambda e, o=catT.ap[:, 0:HQ, q0:q0 + nq], i=cg[:, :, 0:nq]: e.dma_start(out=o, in_=i), catT, [cg])
        P.barrier(); es2.close(); es.close()
        if upto <= 0:
            d0 = dout("d0", [128, DC, NK], BF16); outs.append("d0")
            P.dma("gpsimd", lambda e, o=d0.ap, i=catT.ap: e.dma_start(out=o, in_=i), d0, [catT])
            P.wait_all("sync", [d0]); return P.build(), outs

        es = contextlib.ExitStack()
        SC, SW = c.SC, c.SW
        cwr = P.sb("cwr", [4, SW], F32, es); cwt = P.sb("cwt", [128, SC, 4], F32, es)
        P.dma("sync", lambda e, o=cwr[0:3, :], i=ev_cw.ap: e.dma_start(out=o, in_=i), cwr)
        P.dma("sync", lambda e, o=cwr[3:4, :], i=ev_cb.ap: e.dma_start(out=o, in_=i), cwr)
        for k in range(SC):
            pt = psn("t", [0, 1])
            P.op("tensor", lambda e, o=pt[:, 0:4], a=cwr[:, k * 128:(k + 1) * 128], idn=ident[0:4, 0:4]:
                 e.transpose(out=o, in_=a, identity=idn), [cwr, ident], [pt])
            P.op("vector", lambda e, o=cwt[:, k, :], a=pt[:, 0:4]: e.tensor_copy(out=o, in_=a), [pt], [cwt])
        Wb3 = [[P.sb("Wb%d_%d" % (i, j), [128, DC, 128], BF16, es) for j in range(3)] for i in range(2)]
        aTt = [P.sb("aTc%d" % i, [128, DC, 512], BF16, es) for i in range(2)]
        cuf = [P.sb("cuf%d" % i, [128, NK], F32, es) for i in range(2)]
        Bf = [P.sb("Bf%d" % i, [128, NK], F32, es) for i in range(2)]
        accf = [P.sb("accf%d" % i, [128, NK], F32, es) for i in range(2)]
        outb = [P.sb("outb%d" % i, [128, NK], BF16, es) for i in range(2)]
        tmpc = [P.sb("tmpc%d" % i, [128, 512], F32, es) for i in range(2)]
        base = AW + 2 * KVW
        ti = 0; tci = 0
        for ch in range(SC):
            W3 = Wb3[ch % 2]
            for j in range(3):
                wload(W3[j], w_in_all.ap[:, base + j * SW + ch * 128: base + j * SW + (ch + 1) * 128])
            cu, Bt, ac, ob = cuf[ch % 2], Bf[ch % 2], accf[ch % 2], outb[ch % 2]
            for r0 in range(0, NK, 512):
                nrow = min(512, NK - r0)
                T = aTt[ti % 2]; ti += 1
                P.dma("sync", lambda e, o=T[:, :, 0:nrow], i=aT0.ap[:, :, r0:r0 + nrow]: e.dma_start(out=o, in_=i), T, [aT0])
                pp = []
                for j in range(3):
                    pj = psn("c", [2, 3, 4, 5, 6, 7])
                    for k in range(DC):
                        P.op("tensor", lambda e, o=pj[:, 0:nrow], a=W3[j][:, k, :], b=T[:, k, 0:nrow], k=k:
                             e.matmul(o, lhsT=a, rhs=b, start=(k == 0), stop=(k == DC - 1)), [W3[j], T], [pj])
                    pp.append(pj)
                tc_ = tmpc[tci % 2]; tci += 1
                P.op("scalar", lambda e, o=Bt[:, r0:r0 + nrow], a=pp[0][:, 0:nrow]: e.copy(out=o, in_=a), [pp[0]], [Bt])
                P.op("scalar", lambda e, o=tc_[:, 0:nrow], a=pp[1][:, 0:nrow]: e.copy(out=o, in_=a), [pp[1]], [tc_])
                P.op("vector", lambda e, o=cu[:, r0:r0 + nrow], a=tc_[:, 0:nrow], b=pp[2][:, 0:nrow]:
                     e.tensor_tensor(out=o, in0=a, in1=b, op=ALU.mult), [tc_, pp[2]], [cu])
            P.op("vector", lambda e, o=ac[:], a=cu[:], s1=cwt[:, ch, 1:2], s2=cwt[:, ch, 3:4]:
                 e.tensor_scalar(out=o, in0=a, scalar1=s1, scalar2=s2, op0=ALU.mult, op1=ALU.add), [cu, cwt], [ac])
            for (a0, a1) in ((0, C), (C, NK)):
                P.op("vector", lambda e, o=ac[:, a0 + 1:a1], a=cu[:, a0:a1 - 1], s=cwt[:, ch, 0:1], b=ac[:, a0 + 1:a1]:
                     e.scalar_tensor_tensor(out=o, in0=a, scalar=s, in1=b, op0=ALU.mult, op1=ALU.add), [cu, cwt, ac], [ac])
                P.op("vector", lambda e, o=ac[:, a0:a1 - 1], a=cu[:, a0 + 1:a1], s=cwt[:, ch, 2:3], b=ac[:, a0:a1 - 1]:
                     e.scalar_tensor_tensor(out=o, in0=a, scalar=s, in1=b, op0=ALU.mult, op1=ALU.add), [cu, cwt, ac], [ac])
            P.op("gpsimd", lambda e, o=ob[:], a=ac[:], b=Bt[:]: e.tensor_tensor(out=o, in0=a, in1=b, op=ALU.mult), [ac, Bt], [ob])
            P.dma("sync", lambda e, o=catT.ap[:, HQ + ch, :], i=ob[:]: e.dma_start(out=o, in_=i), catT, [ob])
        P.barrier(); es.close()

        pass_out(0, catT, DC, w_out_all, xin, 0, NK, C, h1, blat, affT)
        P.wait_all("sync", [h1, blat, affT])
        return P.build(), outs


def build_moe(cfg, has_ctx):
    c = cfg
    D, S, C, DC, FF, FC = c.D, c.S, c.C, c.DC, c.FF, c.FC
    C0 = C if has_ctx else 0
    NKl = S + C0
    cap, capc = c.cap, c.capc
    NT = cap // 128
    P = Prog()
    din = lambda n, s, dt=F32: P.dram(n, s, dt, kind="ExternalInput")
    dout = lambda n, s, dt=F32: P.dram(n, s, dt, kind="ExternalOutput")
    affrows = din("affrows", [8, NKl]); blat_all = din("blat_all", [4 * NKl, D], BF16)
    wg = din("wg", [2, D, FF]); wu = din("wu", [2, D, FF]); wd = din("wd", [2, FF, D])
    rowoff = din("rowoff", [8, 1]); ident_in = din("ident", [128, 128])
    slots = dout("slots", [8, cap, D]); ridx = dout("ridx", [8, cap], I32)
    outs = ["slots", "ridx"]
    if has_ctx:
        slotsc = dout("slotsc", [8, capc, D]); ridxc = dout("ridxc", [8, capc], I32); outs += ["slotsc", "ridxc"]
    PS = [P.ps("ps%d" % i, [128, 512], F32) for i in range(8)]
    rr = {}

    def psn(group, banks):
        i = rr.get(group, 0); rr[group] = i + 1
        return PS[banks[i % len(banks)]]
    ident = P.sb("identf", [128, 128], F32)
    P.dma("sync", lambda e, o=ident[:], i=ident_in[:]: e.dma_start(out=o, in_=i), ident)
    identb = P.sb("identb", [128, 128], BF16)
    P.op("vector", lambda e, o=identb[:], i=ident[:]: e.tensor_copy(out=o, in_=i), [ident], [identb])
    rof = P.sb("rof", [8, 1], F32)
    P.dma("sync", lambda e, o=rof[:], i=rowoff.ap: e.dma_start(out=o, in_=i), rof)
    gT = P.sb("gT", [128, NT, 8], F32); gixT = P.sb("gixT", [128, NT, 8], I32)
    gcT = P.sb("gcT", [32, 8], F32); gixcT = P.sb("gixcT", [32, 8], I32)
    es = contextlib.ExitStack()
    at = P.sb("at", [8, NKl], F32, es); wk = P.sb("wk", [8, S], F32, es)
    P.dma("sync", lambda e, o=at[:], i=affrows.ap: e.dma_start(out=o, in_=i), at)

    def topk(src_ap, srcbuf, n, k, work, off, tagv):
        vals = P.sb("vals" + tagv, [8, k], F32, es); idxu = P.sb("idxu" + tagv, [8, k], U32, es)
        idxf = P.sb("idxf" + tagv, [8, k], F32, es); gi = P.sb("gi" + tagv, [8, k], F32, es); ri = P.sb("ri" + tagv, [8, k], I32, es)
        cur = src_ap; curb = srcbuf
        for it in range(k // 8):
            v8 = vals[:, it * 8:(it + 1) * 8]
            P.op("vector", lambda e, o=v8, a=cur: e.max(out=o, in_=a), [curb], [vals])
            P.op("vector", lambda e, o=idxu[:, it * 8:(it + 1) * 8], m=v8, a=cur: e.max_index(out=o, in_max=m, in_values=a), [curb, vals], [idxu])
            if it < k // 8 - 1:
                P.op("vector", lambda e, o=work[:, 0:n], m=v8, a=cur: e.match_replace(out=o, in_to_replace=m, in_values=a, imm_value=-1.0),
                     [curb, vals], [work])
                cur = work[:, 0:n]; curb = work
        P.op("vector", lambda e, o=idxf[:], a=idxu[:]: e.tensor_copy(out=o, in_=a), [idxu], [idxf])
        P.op("vector", lambda e, o=gi[:], a=idxf[:], s=rof[:, 0:1]: e.tensor_scalar(out=o, in0=a, scalar1=s, scalar2=float(off), op0=ALU.add, op1=ALU.add),
             [idxf, rof], [gi])
        P.op("vector", lambda e, o=idxf[:], a=idxf[:]: e.tensor_scalar(out=o, in0=a, scalar1=float(off), scalar2=None, op0=ALU.add), [idxf], [idxf])
        P.op("vector", lambda e, o=ri[:], a=idxf[:]: e.tensor_copy(out=o, in_=a), [idxf], [ri])
        return vals, gi, ri
    vals, gi, ri = topk(at[:, C0:NKl], at, S, cap, wk, C0, "l")
    P.dma("sync", lambda e, o=ridx.ap, i=ri[:]: e.dma_start(out=o, in_=i), ridx, [ri])
    for j in range(NT):
        pt = psn("t", [0, 1])
        P.op("tensor", lambda e, o=pt[:, 0:8], a=gi[:, j * 128:(j + 1) * 128], idn=ident[0:8, 0:8]: e.transpose(out=o, in_=a, identity=idn), [gi, ident], [pt])
        P.op("tensor", lambda e, o=pt[:, 8:16], a=vals[:, j * 128:(j + 1) * 128], idn=ident[0:8, 0:8]: e.transpose(out=o, in_=a, identity=idn), [vals, ident], [pt])
        P.op("vector", lambda e, o=gixT[:, j, :], a=pt[:, 0:8]: e.tensor_copy(out=o, in_=a), [pt], [gixT])
        P.op("vector", lambda e, o=gT[:, j, :], a=pt[:, 8:16]: e.tensor_copy(out=o, in_=a), [pt], [gT])
    if has_ctx:
        wkc = P.sb("wkc", [8, C], F32, es)
        valsc, gic, ric = topk(at[:, 0:C], at, C, capc, wkc, 0, "c")
        P.dma("sync", lambda e, o=ridxc.ap, i=ric[:]: e.dma_start(out=o, in_=i), ridxc, [ric])
        pt = psn("t", [0, 1])
        P.op("tensor", lambda e, o=pt[0:capc, 0:8], a=gic[:, :], idn=ident[0:8, 0:8]: e.transpose(out=o, in_=a, identity=idn), [gic, ident], [pt])
        P.op("tensor", lambda e, o=pt[0:capc, 8:16], a=valsc[:, :], idn=ident[0:8, 0:8]: e.transpose(out=o, in_=a, identity=idn), [valsc, ident], [pt])
        P.op("vector", lambda e, o=gixcT[0:capc, :], a=pt[0:capc, 0:8]: e.tensor_copy(out=o, in_=a), [pt], [gixcT])
        P.op("vector", lambda e, o=gcT[0:capc, :], a=pt[0:capc, 8:16]: e.tensor_copy(out=o, in_=a), [pt], [gcT])
    P.barrier(); es.close()
    NSL = 2 * cap + (2 * capc if has_ctx else 0)
    xsT = P.sb("xsT", [128, DC, NSL], BF16); hidT = P.sb("hidT", [128, FC, NSL], BF16)
    xg = [P.sb("xg%d" % i, [128, D], BF16) for i in range(2)]
    Wt = [[P.sb("W%d_%d" % (m, i), [128, DC, 256], BF16) for i in range(2)] for m in range(3)]
    sg = [P.sb("sg%d" % i, [128, 512], F32) for i in range(2)]
    ot = [P.sb("ot%d" % i, [128, 256], F32) for i in range(4)]
    wi = [0, 0, 0]; xi = 0; si = 0; oi = 0

    def wl(m, src_ap):
        t = Wt[m][wi[m] % 2]; wi[m] += 1
        P.dma("gpsimd", lambda e, o=t[:], i=src_ap.rearrange("(k p) n -> p k n", p=128): e.dma_start(out=o, in_=i), t)
        return t
    for el in range(2):
        for bg in range(2):
            tiles = []
            s0 = 0
            for b in (2 * bg, 2 * bg + 1):
                r8 = el * 4 + b
                for j in range(NT):
                    tiles.append((s0, 128, r8, 0, j)); s0 += 128
                if has_ctx:
                    tiles.append((s0, capc, r8, 1, 0)); s0 += capc
            nsl = s0
            for (t0, ns, r8, kind, j) in tiles:
                g = xg[xi % 2]; xi += 1
                ixap = gixcT[0:ns, r8:r8 + 1] if kind else gixT[:, j, r8:r8 + 1]
                ixb = gixcT if kind else gixT
                P.dma("gpsimd", lambda e, o=g[0:ns, :], ix=ixap: e.indirect_dma_start(
                    out=o, out_offset=None, in_=blat_all.ap, in_offset=bass.IndirectOffsetOnAxis(ap=ix, axis=0)), g, [ixb, blat_all])
                for k4 in range(0, DC, 4):
                    pt = psn("t", [0, 1])
                    ptb = pt[:].bitcast(BF16)
                    for kk in range(4):
                        k = k4 + kk
                        P.op("tensor", lambda e, o=ptb[:, kk * 128:kk * 128 + ns], a=g[0:ns, k * 128:(k + 1) * 128], idn=identb[0:ns, 0:ns]:
                             e.transpose(out=o, in_=a, identity=idn), [g, identb], [pt])
                    P.op("scalar", lambda e, o=xsT[:, k4:k4 + 4, t0:t0 + ns], a=ptb[:, 0:512].rearrange("p (k t) -> p k t", k=4)[:, :, 0:ns]:
                         e.copy(out=o, in_=a), [pt], [xsT])
            for f2 in range(FF // 256):
                Wg_ = wl(0, wg.ap[el, :, f2 * 256:(f2 + 1) * 256]); Wu_ = wl(1, wu.ap[el, :, f2 * 256:(f2 + 1) * 256])
                for half in range(2):
                    fc = f2 * 2 + half
                    for c0 in range(0, nsl, 512):
                        ns = min(512, nsl - c0)
                        pg = psn("g", [2, 3]); pu = psn("u", [4, 5])
                        for k in range(DC):
                            P.op("tensor", lambda e, o=pg[:, 0:ns], a=Wg_[:, k, half * 128:(half + 1) * 128], b=xsT[:, k, c0:c0 + ns], k=k:
                                 e.matmul(o, lhsT=a, rhs=b, start=(k == 0), stop=(k == DC - 1)), [Wg_, xsT], [pg])
                        for k in range(DC):
                            P.op("tensor", lambda e, o=pu[:, 0:ns], a=Wu_[:, k, half * 128:(half + 1) * 128], b=xsT[:, k, c0:c0 + ns], k=k:
                                 e.matmul(o, lhsT=a, rhs=b, start=(k == 0), stop=(k == DC - 1)), [Wu_, xsT], [pu])
                        s_ = sg[si % 2]; si += 1
                        P.op("scalar", lambda e, o=s_[:, 0:ns], a=pg[:, 0:ns]: e.activation(out=o, in_=a, func=AF.Silu), [pg], [s_])
                        P.op("vector", lambda e, o=hidT[:, fc, c0:c0 + ns], a=s_[:, 0:ns], b=pu[:, 0:ns]:
                             e.tensor_tensor(out=o, in0=a, in1=b, op=ALU.mult), [s_, pu], [hidT])
            for n0 in range(0, D, 256):
                Wd_ = wl(2, wd.ap[el, :, n0:n0 + 256])
                for (t0, ns, r8, kind, j) in tiles:
                    po = psn("o", [6, 7])
                    for f in range(FC):
                        P.op("tensor", lambda e, o=po[0:ns, 0:256], a=hidT[:, f, t0:t0 + ns], b=Wd_[:, f, :], f=f:
                             e.matmul(o, lhsT=a, rhs=b, start=(f == 0), stop=(f == FC - 1)), [hidT, Wd_], [po])
                    o_ = ot[oi % 4]; oi += 1
                    gap = gcT[0:ns, r8:r8 + 1] if kind else gT[:, j, r8:r8 + 1]
                    P.op("vector", lambda e, o=o_[0:ns, :], a=po[0:ns, 0:256], s=gap: e.tensor_scalar(out=o, in0=a, scalar1=s, scalar2=None, op0=ALU.mult),
                         [po, gcT if kind else gT], [o_])
                    if kind:
                        P.dma("sync", lambda e, o=slotsc.ap[r8, 0:ns, n0:n0 + 256], i=o_[0:ns, :]: e.dma_start(out=o, in_=i), slotsc, [o_], disjoint=True)
                    else:
                        P.dma("sync", lambda e, o=slots.ap[r8, j * 128:(j + 1) * 128, n0:n0 + 256], i=o_[0:ns, :]: e.dma_start(out=o, in_=i), slots, [o_], disjoint=True)
    P.wait_all("sync", [slots, ridx] + ([slotsc, ridxc] if has_ctx else []))
    return P.build(), outs


import time


def rope_table(cfg):
    S, C = cfg.S, cfg.C
    t = np.arange(S); row = t // 64; col = t % 64
    freqs = (10000.0 ** (-np.arange(0, 64, 2, dtype=np.float32) / 64)).astype(np.float32)
    ar = row[:, None].astype(np.float32) * freqs[None]; ac = col[:, None].astype(np.float32) * freqs[None]
    tab = np.zeros((cfg.NK, 128), np.float32)
    tab[:C, 0:64] = 1.0
    tab[C:, 0:32] = np.cos(ar); tab[C:, 32:64] = np.cos(ac); tab[C:, 64:96] = np.sin(ar); tab[C:, 96:128] = np.sin(ac)
    return tab


def _run(nc, maps, tag):
    t0 = time.time()
    maps = [{k: np.ascontiguousarray(v) for k, v in m.items()} for m in maps]
    res = _bu.run_bass_kernel_spmd(nc, maps, core_ids=list(range(len(maps))))
    print("launch", tag, "%.1fs" % (time.time() - t0), flush=True)
    return res.results


def pipeline(kb, cfg, I, progs=None):
    kb = _KB
    D, S, C, NK, D8 = cfg.D, cfg.S, cfg.C, cfg.NK, cfg.D8
    ident = np.eye(128, dtype=np.float32)
    sel = np.zeros((2, 256), np.float32); sel[0, :128] = 1; sel[1, 128:] = 1
    nc, _ = kb.build_mod(cfg)
    maps = []
    for r in range(8):
        maps.append(dict(c5=np.concatenate([I["c"], I["c_ctx"][None]], 0),
                         modw=I["mod_w"].reshape(2, D, 6, 8, D8)[:, :, :, r, :].reshape(2, D, 6 * D8),
                         modb=I["mod_b"].reshape(2, 6, 8, D8)[:, :, r, :].reshape(2, 6 * D8), ident=ident))
    res = _run(nc, maps, "mod")
    mod = np.stack([res[r]["mloc_out"].reshape(5, 2, 6, D8) for r in range(8)], 3).reshape(5, 2, 6, D)

    def common(b):
        return dict(modrows=np.stack([mod[b].reshape(-1), mod[4].reshape(-1)], 0), n1g=I["norm1_g"], n2g=I["norm2_g"],
                    ident=ident, selin=sel)
    nc, _ = kb.build(cfg, "mix0")
    rt = rope_table(cfg)
    maps = []
    for b in range(4):
        m = common(b)
        m.update(xin=np.concatenate([I["ctx"][b], I["x"][b]], 0), ropet=rt, ev_w_in=I["ev_w_in"][0], ev_w_out=I["ev_w_out"][0],
                 evq=I["ev_q_norm"], evk=I["ev_k_norm"], ev_cw=I["ev_conv_w"][0], ev_cb=I["ev_conv_b"], router=I["moe_router"][0])
        maps.append(m)
    r0 = _run(nc, maps, "mix0")

    def moe(layer, blat_list, aff_list, has_ctx):
        NKl = NK if has_ctx else S
        nc, _ = kb.build_moe(cfg, has_ctx)
        blat_all = np.concatenate(blat_list, 0)
        maps = []
        for k in range(8):
            maps.append(dict(blat_all=blat_all, ident=ident,
                             affrows=np.stack([aff_list[b][2 * k + el] for el in range(2) for b in range(4)], 0),
                             rowoff=np.array([[b * NKl] for el in range(2) for b in range(4)], np.float32),
                             wg=I["moe_w_gate"][layer, 2 * k:2 * k + 2], wu=I["moe_w_up"][layer, 2 * k:2 * k + 2],
                             wd=I["moe_w_down"][layer, 2 * k:2 * k + 2]))
        rs = _run(nc, maps, "moe%d" % layer)
        outb = []
        for b in range(4):
            d = dict(slots_b=np.stack([rs[e // 2]["slots"][(e % 2) * 4 + b] for e in range(16)], 0),
                     ridx_b=np.stack([rs[e // 2]["ridx"][(e % 2) * 4 + b] for e in range(16)], 0))
            if has_ctx:
                d.update(slotsc_b=np.stack([rs[e // 2]["slotsc"][(e % 2) * 4 + b] for e in range(16)], 0),
                         ridxc_b=np.stack([rs[e // 2]["ridxc"][(e % 2) * 4 + b] for e in range(16)], 0))
            outb.append(d)
        return outb
    m0 = moe(0, [r0[b]["blat"] for b in range(4)], [r0[b]["affT"] for b in range(4)], True)
    nc, _ = kb.build(cfg, "mix1")
    maps = []
    od_vec = np.concatenate([I["od_ba"][0], I["od_bx"][0], I["od_lam"][0]], 0)
    for b in range(4):
        m = common(b)
        m.update(h1=r0[b]["h1"], od_w_in=I["od_w_in"][0], od_w_out=I["od_w_out"][0], od_cw=I["od_conv_w"][0], od_cb=I["od_conv_b"],
                 od_wa=I["od_wa"][0], od_wx=I["od_wx"][0], od_vec=od_vec, router=I["moe_router"][1])
        m.update(m0[b])
        maps.append(m)
    r1 = _run(nc, maps, "mix1")
    m1 = moe(1, [r1[b]["blat1"] for b in range(4)], [r1[b]["affT1"] for b in range(4)], False)
    nc, _ = kb.build(cfg, "final")
    maps = []
    for b in range(4):
        m = common(b)
        m.update(h2=r1[b]["h2"], fng=I["final_norm_g"][None])
        m.update(m1[b])
        maps.append(m)
    r2 = _run(nc, maps, "final")
    dbg = dict(r0=r0, r1=r1)
    return np.stack([r2[b]["out"] for b in range(4)], 0), dbg


class _KBNS:
    pass


_KB = _KBNS()
_KB.build_mod = build_mod
_KB.build = build
_KB.build_moe = build_moe


def kernel(**inputs):
    I = {k: np.asarray(v) for k, v in inputs.items()}
    cfg = Cfg()
    out, _ = pipeline(None, cfg, I)
    return np.ascontiguousarray(out.astype(np.float32))
```

```python
import time
import concourse.bass_utils as _bu
import contextlib
import numpy as np
import concourse.bass as bass
import concourse.mybir as mybir

F32 = mybir.dt.float32
BF16 = mybir.dt.bfloat16
I32 = mybir.dt.int32
U32 = mybir.dt.uint32
ALU = mybir.AluOpType
AF = mybir.ActivationFunctionType
AX = mybir.AxisListType

ENGS = ("tensor", "vector", "scalar", "gpsimd", "sync")


class Buf:
    def __init__(self, P, name, ap_fn):
        self.P = P
        self.name = name
        self._ap = ap_fn
        self.last_w = None
        self.reads = []
        self.sem = None
        self.cnt = 0

    def __getitem__(self, idx):
        return self._ap()[idx]

    @property
    def ap(self):
        return self._ap()


class Prog:
    def __init__(self):
        self.nc = bass.Bass("TRN2", target_bir_lowering=False)
        self.es = contextlib.ExitStack()
        self.q = {e: [] for e in ENGS}
        self.esem = {}
        self.ecnt = {e: 0 for e in ENGS}
        for e in ENGS:
            self.esem[e] = self.es.enter_context(self.nc.semaphore("S_" + e))
        self.waited = {e: {} for e in ENGS}
        self.semobjs = {}
        self.nbuf = 0
        self.dma_bufs = []

    def sb(self, name, shape, dt, es=None):
        t = (es or self.es).enter_context(self.nc.sbuf_tensor(name, list(shape), dt))
        return Buf(self, name, lambda: t)

    def ps(self, name, shape, dt, es=None):
        t = (es or self.es).enter_context(self.nc.psum_tensor(name, list(shape), dt))
        return Buf(self, name, lambda: t)

    def dram(self, name, shape, dt, kind=None):
        if kind is None:
            t = self.nc.dram_tensor(name, list(shape), dt)
        else:
            t = self.nc.dram_tensor(name, list(shape), dt, kind=kind)
        return Buf(self, name, lambda: t.ap())

    def _bufsem(self, b):
        if b.sem is None:
            b.sem = self.es.enter_context(self.nc.semaphore("D_%d" % self.nbuf))
            self.nbuf += 1
            self.dma_bufs.append(b)
        return b.sem

    def _deps(self, eng, reads, writes):
        evs = []
        for b in reads:
            if b.last_w is not None:
                evs.append(b.last_w)
        for b in writes:
            if b.last_w is not None:
                evs.append(b.last_w)
            evs.extend(b.reads)
        need = {}
        for (sem, val) in evs:
            if eng == "tensor" and sem is self.esem["tensor"]:
                continue
            k = id(sem)
            if self.waited[eng].get(k, 0) >= val:
                continue
            if k not in need or need[k][1] < val:
                need[k] = (sem, val)
        for k, (sem, val) in need.items():
            self.waited[eng][k] = val
        return list(need.values())

    def _mark(self, ev, reads, writes):
        for b in reads:
            b.reads.append(ev)
            if len(b.reads) > 64:
                best = {}
                for (s, v) in b.reads:
                    if id(s) not in best or best[id(s)][1] < v:
                        best[id(s)] = (s, v)
                b.reads = list(best.values())
        for b in writes:
            b.last_w = ev
            b.reads = []

    def op(self, eng, fn, reads=(), writes=()):
        waits = self._deps(eng, reads, writes)
        self.ecnt[eng] += 1
        ev = (self.esem[eng], self.ecnt[eng])
        self.q[eng].append((waits, fn, ev[0], 1))
        self._mark(ev, reads, writes)
        return ev

    def dma(self, eng, fn, dst, reads=(), extra_writes=(), disjoint=False):
        sem = self._bufsem(dst)
        waits = self._deps(eng, reads, ([] if disjoint else [dst]) + list(extra_writes))
        dst.cnt += 16
        ev = (sem, dst.cnt)
        self.q[eng].append((waits, fn, sem, 16))
        self._mark(ev, reads, [dst] + list(extra_writes))
        return ev

    def cc(self, fn, dst, reads=()):
        sem = self._bufsem(dst)
        waits = self._deps("gpsimd", reads, [dst])
        dst.cnt += 1
        ev = (sem, dst.cnt)
        self.q["gpsimd"].append((waits, fn, sem, None))
        self._mark(ev, reads, [dst])
        return ev

    def barrier(self):
        evs = [(self.esem[e], self.ecnt[e]) for e in ENGS if self.ecnt[e] > 0]
        evs += [(b.sem, b.cnt) for b in self.dma_bufs if b.cnt > 0]
        for eng in ENGS:
            waits = []
            for (sem, val) in evs:
                if sem is self.esem[eng]:
                    continue
                k = id(sem)
                if self.waited[eng].get(k, 0) >= val:
                    continue
                self.waited[eng][k] = val
                waits.append((sem, val))
            if waits:
                self.q[eng].append((waits, None, None, 0))

    def wait_all(self, eng, bufs):
        waits = []
        for b in bufs:
            if b.last_w is not None:
                waits.append(b.last_w)
        self.q[eng].append((waits, None, None, 0))

    def build(self):
        nc = self.nc
        with nc.Block() as block:
            def mk(engname):
                items = self.q[engname]

                def body(e):
                    for (waits, fn, sem, inc) in items:
                        for (s, v) in waits:
                            e.wait_ge(s, v)
                        if fn is None:
                            continue
                        ins = fn(e)
                        if inc is None:
                            ins.then_inc(sem)
                        else:
                            ins.then_inc(sem, inc)
                return body
            block.tensor(mk("tensor"))
            block.vector(mk("vector"))
            block.scalar(mk("scalar"))
            block.gpsimd(mk("gpsimd"))
            block.sync(mk("sync"))
        self.es.close()
        return nc


import contextlib, math
import numpy as np

EPS = 1e-6


class Cfg:
    def __init__(self, D=2048, S=4096, C=256):
        self.D, self.S, self.C = D, S, C
        self.B, self.NE, self.L = 4, 16, 2
        self.DC = D // 128
        self.AW = D // 2; self.HQ = self.AW // 128; self.HKV = self.HQ // 4; self.KVW = self.HKV * 128
        self.SW = D // 2; self.SC = self.SW // 128
        self.EIN = self.AW + 2 * self.KVW + 3 * self.SW
        self.LW = 5 * D // 4; self.LC = self.LW // 128; self.NLB = self.LW // 256
        self.FF = D; self.FC = self.FF // 128
        self.SH, self.CH = S // 2, C // 2
        self.NK = S + C
        self.cap = 2 * S // 16; self.capc = 2 * C // 16
        self.D8 = D // 8
        self.NOWN = self.SH + self.CH


def half_cfg(c):
    import copy
    h = copy.copy(c)
    h.AW = c.AW // 2; h.HQ = c.HQ // 2; h.HKV = max(1, c.HKV // 2); h.KVW = h.HKV * 128
    h.SW = c.SW // 2; h.SC = c.SC // 2; h.EIN = h.AW + 2 * h.KVW + 3 * h.SW
    h.LW = c.LW // 2; h.LC = c.LC // 2; h.NLB = c.NLB // 2
    return h


def build_mod(cfg):
    c = cfg; D, DC = c.D, c.DC
    P = Prog()
    din = lambda n, s, dt=F32: P.dram(n, s, dt, kind="ExternalInput")
    c5 = din("c5", [5, D]); modw = din("modw", [2, D, 6 * c.D8]); modb = din("modb", [2, 6 * c.D8])
    ident_in = din("ident", [128, 128])
    mout = P.dram("mloc_out", [5, 2 * 6 * c.D8], F32, kind="ExternalOutput")
    PS = [P.ps("ps%d" % i, [128, 512], F32) for i in range(4)]
    ident = P.sb("identf", [128, 128], F32)
    P.dma("sync", lambda e, o=ident[:], i=ident_in[:]: e.dma_start(out=o, in_=i), ident)
    c5t = P.sb("c5t", [5, D], F32); cT = P.sb("cT", [128, DC, 5], F32)
    P.dma("sync", lambda e, o=c5t[:], i=c5[:]: e.dma_start(out=o, in_=i), c5t)
    P.op("scalar", lambda e, o=c5t[:], i=c5t[:]: e.activation(out=o, in_=i, func=AF.Silu), [c5t], [c5t])
    for k in range(DC):
        pt = PS[k % 2]
        P.op("tensor", lambda e, o=pt[:, 0:5], i=c5t[:, k * 128:(k + 1) * 128], idn=ident[0:5, 0:5]:
             e.transpose(out=o, in_=i, identity=idn), [c5t, ident], [pt])
        P.op("vector", lambda e, o=cT[:, k, :], i=pt[:, 0:5]: e.tensor_copy(out=o, in_=i), [pt], [cT])
    ncol = 6 * c.D8
    mloc = P.sb("mloc", [5, 2 * ncol], F32); mbt = P.sb("mbt", [5, 2 * ncol], F32)
    P.dma("sync", lambda e, o=mbt[:], i=modb.ap.rearrange("l n -> (l n)").partition_broadcast(5): e.dma_start(out=o, in_=i), mbt)
    wst = [P.sb("mw%d" % i, [128, DC, 256], F32) for i in range(2)]
    ci = 0
    for l in range(2):
        for n0 in range(0, ncol, 256):
            w = wst[ci % 2]; pm = PS[2 + ci % 2]; ci += 1
            P.dma("sync", lambda e, o=w[:], i=modw.ap[l, :, n0:n0 + 256].rearrange("(k p) n -> p k n", p=128): e.dma_start(out=o, in_=i), w)
            for k in range(DC):
                P.op("tensor", lambda e, o=pm[0:5, 0:256], a=cT[:, k, :], b=w[:, k, :], k=k:
                     e.matmul(o, lhsT=a, rhs=b, start=(k == 0), stop=(k == DC - 1)), [cT, w], [pm])
            P.op("vector", lambda e, o=mloc[:, l * ncol + n0:l * ncol + n0 + 256], a=pm[0:5, 0:256],
                 b=mbt[:, l * ncol + n0:l * ncol + n0 + 256]: e.tensor_tensor(out=o, in0=a, in1=b, op=ALU.add), [pm, mbt], [mloc])
    P.dma("sync", lambda e, o=mout.ap, i=mloc[:]: e.dma_start(out=o, in_=i), mout, [mloc])
    P.wait_all("sync", [mout])
    return P.build(), ["mloc_out"]


def build(cfg, which, layer=0, dbg=False, upto=99):
    c = cfg
    D, S, C, DC, SH, CH, NK = c.D, c.S, c.C, c.DC, c.SH, c.CH, c.NK
    P = Prog(); nc = P.nc
    din = lambda n, s, dt=F32: P.dram(n, s, dt, kind="ExternalInput")
    dout = lambda n, s, dt=F32: P.dram(n, s, dt, kind="ExternalOutput")
    outs = []
    modrows = din("modrows", [2, 2 * 6 * D])
    n1g = din("n1g", [2, D]); n2g = din("n2g", [2, D])
    ident_in = din("ident", [128, 128])
    PS = [P.ps("ps%d" % i, [128, 512], F32) for i in range(8)]
    rr = {}

    def psn(group, banks):
        i = rr.get(group, 0); rr[group] = i + 1
        return PS[banks[i % len(banks)]]

    ident = P.sb("identf", [128, 128], F32)
    P.dma("sync", lambda e, o=ident[:], i=ident_in[:]: e.dma_start(out=o, in_=i), ident)
    identb = P.sb("identb", [128, 128], BF16)
    P.op("vector", lambda e, o=identb[:], i=ident[:]: e.tensor_copy(out=o, in_=i), [ident], [identb])
    selt = P.sb("selt", [2, 256], F32)
    P.op("vector", lambda e, o=selt[:]: e.memset(o, 0.0), [], [selt])
    selin = din("selin", [2, 256])
    P.dma("sync", lambda e, o=selt[:], i=selin.ap: e.dma_start(out=o, in_=i), selt)
    bvn = [0]

    def bcast_vec(dst, l, j, which):
        tes = contextlib.ExitStack()
        bvn[0] += 1
        mv = P.sb("mv%d" % bvn[0], [2, D], F32, tes)
        P.dma("sync", lambda e, o=mv[:], i=modrows.ap[:, (l * 6 + j) * D:(l * 6 + j + 1) * D]: e.dma_start(out=o, in_=i), mv)
        if j in (1, 4):
            gv = P.sb("gv%d" % bvn[0], [2, D], F32, tes)
            gsrc = n1g if j == 1 else n2g
            P.dma("sync", lambda e, o=gv[:], i=gsrc.ap[l:l + 1, :].partition_broadcast(2): e.dma_start(out=o, in_=i), gv)
            P.op("vector", lambda e, o=mv[:], a=mv[:], b=gv[:]:
                 e.scalar_tensor_tensor(out=o, in0=a, scalar=1.0, in1=b, op0=ALU.add, op1=ALU.mult), [mv, gv], [mv])
        for n0 in range(0, D, 512):
            pb = psn("m", [2, 3])
            P.op("tensor", lambda e, o=pb[:, :], a=selt[:, which * 128:(which + 1) * 128], b=mv[:, n0:n0 + 512]:
                 e.matmul(o, lhsT=a, rhs=b, start=True, stop=True), [selt, mv], [pb])
            P.op("scalar", lambda e, o=dst[:, n0:n0 + 512], i=pb[:, :]: e.copy(out=o, in_=i), [pb], [dst])
        P.barrier(); tes.close()

    def norm_tile(xt, gs, sh, at, small):
        P.op("scalar", lambda e, o=at[:], i=xt[:], a=small[:, 0:1]: e.activation(out=o, in_=i, func=AF.Square, accum_out=a),
             [xt], [at, small])
        P.op("scalar", lambda e, o=small[:, 1:2], i=small[:, 0:1]: e.activation(out=o, in_=i, func=AF.Sqrt, scale=1.0 / D, bias=EPS),
             [small], [small])
        P.op("vector", lambda e, o=small[:, 2:3], i=small[:, 1:2]: e.reciprocal(out=o, in_=i), [small], [small])
        P.op("vector", lambda e, o=at[:], a=xt[:], s=small[:, 2:3], b=gs[:]:
             e.scalar_tensor_tensor(out=o, in0=a, scalar=s, in1=b, op0=ALU.mult, op1=ALU.mult), [xt, small, gs], [at])
        if sh is not None:
            P.op("gpsimd", lambda e, o=at[:], a=at[:], b=sh[:]: e.tensor_tensor(out=o, in0=a, in1=b, op=ALU.add), [at, sh], [at])

    def transpose_to(at, dstT, col0, ncols_part=128, dt_ident=None):
        for k4 in range(0, DC, 4):
            pt = psn("t", [0, 1])
            for kk in range(4):
                k = k4 + kk
                P.op("tensor", lambda e, o=pt[:, kk * 128:(kk + 1) * 128], i=at[:, k * 128:(k + 1) * 128], idn=ident[:]:
                     e.transpose(out=o, in_=i, identity=idn), [at, ident], [pt])
            P.op("scalar", lambda e, o=dstT[:, k4:k4 + 4, col0:col0 + 128], i=pt[:].rearrange("p (k t) -> p k t", k=4):
                 e.copy(out=o, in_=i), [pt], [dstT])


    def wload(dst, src_ap):
        import os
        if os.environ.get("NOWL"):
            P.op("vector", lambda e, o=dst[:]: e.memset(o, 0.01), [], [dst]); return
        P.dma("gpsimd", lambda e, o=dst[:], i=src_ap.rearrange("(k p) n -> p k n", p=128): e.dma_start(out=o, in_=i), dst)

    def pass_out(l, srcT, KC, w_all, hsrc, hrow0, nrows, cbound, hdst, bdst, affdst, dbgname=None):
        es = contextlib.ExitStack()
        Wo = P.sb("Wo", [128, KC, D], BF16, es)
        wload(Wo, w_all.ap)
        nw = 2 if cbound > 0 else 1
        g1 = [P.sb("g1_%d" % w, [128, D], F32, es) for w in range(nw)]
        gs2 = [P.sb("gs2_%d" % w, [128, D], F32, es) for w in range(nw)]
        sh2 = [P.sb("sh2_%d" % w, [128, D], F32, es) for w in range(nw)]
        for w in range(nw):
            bcast_vec(g1[w], l, 2, w); bcast_vec(gs2[w], l, 4, w); bcast_vec(sh2[w], l, 3, w)
        wr = P.sb("wr", [128, DC, 16], F32, es)
        P.dma("sync", lambda e, o=wr[:], i=router.ap.rearrange("(k p) n -> p k n", p=128): e.dma_start(out=o, in_=i), wr)
        NBF = 1 if D > 1024 else 2
        cts = [P.sb("ct%d" % i, [128, KC, 512], BF16, es) for i in range(NBF)]
        xts = [P.sb("oxt%d" % i, [128, D], F32, es) for i in range(NBF)]
        hts = [P.sb("oht%d" % i, [128, D], F32, es) for i in range(NBF)]
        bts = [P.sb("obt%d" % i, [128, D], F32, es) for i in range(NBF)]
        bbs = [P.sb("obb%d" % i, [128, D], BF16, es) for i in range(NBF)]
        bTs = [P.sb("obT%d" % i, [128, DC, 128], F32, es) for i in range(NBF)]
        sms = [P.sb("osm%d" % i, [128, 4], F32, es) for i in range(2)]
        exs = [P.sb("oex%d" % i, [128, 16], F32, es) for i in range(2)]
        affTt = P.sb("affTt", [16, nrows], F32, es)
        ti = 0; ni = 0
        for r0 in range(0, nrows, 512):
            nrow = min(512, nrows - r0)
            T = cts[ti % NBF]; ti += 1
            P.dma("sync", lambda e, o=T[:, :, 0:nrow], i=srcT.ap[:, :, r0:r0 + nrow]: e.dma_start(out=o, in_=i), T, [srcT])
            for sub in range(0, nrow, 128):
                rr0 = r0 + sub
                w = 1 if rr0 < cbound else 0
                xt, ht, bt, bb, bT, sm, ex = xts[ni % NBF], hts[ni % NBF], bts[ni % NBF], bbs[ni % NBF], bTs[ni % NBF], sms[ni % 2], exs[ni % 2]; ni += 1
                P.dma("sync", lambda e, o=xt[:], i=hsrc.ap[hrow0 + rr0:hrow0 + rr0 + 128, :]: e.dma_start(out=o, in_=i), xt, [hsrc])
                for n0 in range(0, D, 512):
                    po = psn("o", [2, 3, 4])
                    for k in range(KC):
                        P.op("tensor", lambda e, o=po[:, :], a=T[:, k, sub:sub + 128], b=Wo[:, k, n0:n0 + 512], k=k:
                             e.matmul(o, lhsT=a, rhs=b, start=(k == 0), stop=(k == KC - 1)), [T, Wo], [po])
                    P.op("vector", lambda e, o=ht[:, n0:n0 + 512], a=po[:, :], b=g1[w][:, n0:n0 + 512]:
                         e.tensor_tensor(out=o, in0=a, in1=b, op=ALU.mult), [po, g1[w]], [ht])
                P.op("gpsimd", lambda e, o=ht[:], a=ht[:], b=xt[:]: e.tensor_tensor(out=o, in0=a, in1=b, op=ALU.add), [ht, xt], [ht])
                P.dma("sync", lambda e, o=hdst.ap[rr0:rr0 + 128, :], i=ht[:]: e.dma_start(out=o, in_=i), hdst, [ht])
                norm_tile(ht, gs2[w], sh2[w], bt, sm)
                P.op("scalar", lambda e, o=bb[:], a=bt[:]: e.copy(out=o, in_=a), [bt], [bb])
                P.dma("sync", lambda e, o=bdst.ap[rr0:rr0 + 128, :], i=bb[:]: e.dma_start(out=o, in_=i), bdst, [bb])
                transpose_to(bt, bT, 0)
                pr = psn("r", [5, 6])
                for k in range(DC):
                    P.op("tensor", lambda e, o=pr[:, 0:16], a=bT[:, k, :], b=wr[:, k, :], k=k:
                         e.matmul(o, lhsT=a, rhs=b, start=(k == 0), stop=(k == DC - 1)), [bT, wr], [pr])
                P.op("vector", lambda e, o=sm[:, 0:1], a=pr[:, 0:16]: e.tensor_reduce(out=o, in_=a, axis=AX.X, op=ALU.max, negate=True), [pr], [sm])
                P.op("scalar", lambda e, o=ex[:], a=pr[:, 0:16], b=sm[:, 0:1], s=sm[:, 1:2]:
                     e.activation(out=o, in_=a, func=AF.Exp, bias=b, accum_out=s), [pr, sm], [ex, sm])
                P.op("vector", lambda e, o=sm[:, 2:3], a=sm[:, 1:2]: e.reciprocal(out=o, in_=a), [sm], [sm])
                P.op("vector", lambda e, o=ex[:], a=ex[:], s=sm[:, 2:3]: e.tensor_scalar(out=o, in0=a, scalar1=s, scalar2=None, op0=ALU.mult), [ex, sm], [ex])
                pt = psn("t", [0, 1])
                P.op("tensor", lambda e, o=pt[0:16, 0:128], a=ex[:], idn=ident[:]: e.transpose(out=o, in_=a, identity=idn), [ex, ident], [pt])
                P.op("scalar", lambda e, o=affTt[:, rr0:rr0 + 128], a=pt[0:16, 0:128]: e.copy(out=o, in_=a), [pt], [affTt])
        P.dma("sync", lambda e, o=affdst.ap, i=affTt[:]: e.dma_start(out=o, in_=i), affdst, [affTt])
        P.barrier(); es.close()


    def combine(l, hsrc, nrows, cbound, slots_in, ridx_in, slotsc_in, ridxc_in, acc, hdst, post=None):
        es = contextlib.ExitStack()
        nw = 2 if cbound > 0 else 1
        g2 = [P.sb("cg2_%d" % w, [128, D], F32, es) for w in range(nw)]
        for w in range(nw):
            bcast_vec(g2[w], l, 5, w)
        zt = P.sb("czt", [128, D], F32, es)
        P.op("vector", lambda e, o=zt[:]: e.memset(o, 0.0), [], [zt])
        for r0 in range(0, nrows, 128):
            P.dma("sync", lambda e, o=acc.ap[r0:r0 + 128, :], i=zt[:]: e.dma_start(out=o, in_=i), acc, [zt], disjoint=True)
        sts = [P.sb("cst%d" % i, [128, D], F32, es) for i in range(2)]
        its = [P.sb("cit%d" % i, [128, 1], I32, es) for i in range(2)]
        n = 0
        NT_ = c.cap // 128
        for e_ in range(16):
            tl = [(slots_in, ridx_in, j * 128, 128) for j in range(NT_)]
            if slotsc_in is not None:
                tl.append((slotsc_in, ridxc_in, 0, c.capc))
            for ti_, (sl, ri, j0, ns) in enumerate(tl):
                st = sts[n % 2]; it = its[n % 2]; n += 1
                P.dma("sync", lambda e, o=st[0:ns, :], i=sl.ap[e_, j0:j0 + ns, :]: e.dma_start(out=o, in_=i), st)
                P.dma("sync", lambda e, o=it[0:ns, :], i=ri.ap[e_:e_ + 1, j0:j0 + ns].rearrange("o n -> n o"): e.dma_start(out=o, in_=i), it)
                P.dma("gpsimd", lambda e, ix=it[0:ns, 0:1], i=st[0:ns, :]: e.indirect_dma_start(
                    out=acc.ap, out_offset=bass.IndirectOffsetOnAxis(ap=ix, axis=0), in_=i, in_offset=None, compute_op=ALU.add),
                    acc, [st, it])
        hts = [P.sb("cht%d" % i, [128, D], F32, es) for i in range(2)]
        ats_ = [P.sb("cat%d" % i, [128, D], F32, es) for i in range(2)]
        n = 0
        for r0 in range(0, nrows, 128):
            w = 1 if r0 < cbound else 0
            ht = hts[n % 2]; at = ats_[n % 2]; n += 1
            P.dma("sync", lambda e, o=ht[:], i=hsrc.ap[r0:r0 + 128, :]: e.dma_start(out=o, in_=i), ht, [hsrc])
            P.dma("sync", lambda e, o=at[:], i=acc.ap[r0:r0 + 128, :]: e.dma_start(out=o, in_=i), at, [acc])
            P.op("vector", lambda e, o=at[:], a=at[:], b=g2[w][:]: e.tensor_tensor(out=o, in0=a, in1=b, op=ALU.mult), [at, g2[w]], [at])
            P.op("gpsimd", lambda e, o=ht[:], a=ht[:], b=at[:]: e.tensor_tensor(out=o, in0=a, in1=b, op=ALU.add), [ht, at], [ht])
            if post is None:
                P.dma("sync", lambda e, o=hdst.ap[r0:r0 + 128, :], i=ht[:]: e.dma_start(out=o, in_=i), hdst, [ht], disjoint=True)
            else:
                post(ht, r0, at)
        P.barrier(); es.close()

    if which == "final":
        h2 = din("h2", [S, D]); slots_in = din("slots_b", [16, c.cap, D]); ridx_in = din("ridx_b", [16, c.cap], I32)
        fng = din("fng", [1, D])
        out = dout("out", [S, D]); outs.append("out")
        acc = P.dram("acc", [S, D], F32)
        fgb = P.sb("fgb", [128, D], F32)
        P.dma("sync", lambda e, o=fgb[:], i=fng.ap[0:1, :].partition_broadcast(128): e.dma_start(out=o, in_=i), fgb)
        smf = [P.sb("smf%d" % i, [128, 4], F32) for i in range(2)]
        cnt = [0]

        def post(ht, r0, scratch):
            sm = smf[cnt[0] % 2]; cnt[0] += 1
            norm_tile(ht, fgb, None, scratch, sm)
            P.dma("sync", lambda e, o=out.ap[r0:r0 + 128, :], i=scratch[:]: e.dma_start(out=o, in_=i), out, [scratch], disjoint=True)
        combine(1, h2, S, 0, slots_in, ridx_in, None, None, acc, None, post)
        P.wait_all("sync", [out])
        return P.build(), outs

    if which in ("mix1", "mix1a"):
        LW, LC, NLB = c.LW, c.LC, c.NLB
        h1 = din("h1", [NK, D]); slots_in = din("slots_b", [16, c.cap, D]); ridx_in = din("ridx_b", [16, c.cap], I32)
        slotsc_in = din("slotsc_b", [16, c.capc, D]); ridxc_in = din("ridxc_b", [16, c.capc], I32)
        od_w_in = din("od_w_in", [D, 2 * LW]); od_w_out = din("od_w_out", [LW, D])
        od_cw = din("od_cw", [4, LW]); od_cb = din("od_cb", [1, LW])
        od_wa = din("od_wa", [2, NLB, 256, 256]); od_wx = din("od_wx", [2, NLB, 256, 256])
        od_vec = din("od_vec", [6, LW])
        router = din("router", [D, 16])
        if which == "mix1":
            h2 = dout("h2", [S, D]); blat1 = dout("blat1", [S, D], BF16); affT1 = dout("affT1", [16, S]); outs += ["h2", "blat1", "affT1"]
            hl0 = P.dram("hl0", [NK, D], F32)
        else:
            hl0 = dout("hl0", [NK, D]); outs += ["hl0"]
        acc = P.dram("acc", [NK, D], F32)
        aT1 = P.dram("aT1", [128, DC, NK], BF16)
        xT_d = P.dram("xT_d", [LC, 128, NK], F32); gT_d = P.dram("gT_d", [LC, 128, S], BF16)
        if which == "mix1":
            ygT = P.dram("ygT", [128, LC, S], BF16)
        else:
            ygT = dout("ygT", [128, LC, S], BF16); outs += ["ygT"]
        combine(0, h1, NK, C, slots_in, ridx_in, slotsc_in, ridxc_in, acc, hl0)
        es = contextlib.ExitStack()
        gs1 = [P.sb("gs1_%d" % w, [128, D], F32, es) for w in range(2)]
        sh1 = [P.sb("sh1_%d" % w, [128, D], F32, es) for w in range(2)]
        for w in range(2):
            bcast_vec(gs1[w], 1, 1, w); bcast_vec(sh1[w], 1, 0, w)
        xts = [P.sb("xt%d" % i, [128, D], F32, es) for i in range(2)]
        ats = [P.sb("at%d" % i, [128, D], F32, es) for i in range(2)]
        smalls = [P.sb("sm%d" % i, [128, 4], F32, es) for i in range(2)]
        aTt = [P.sb("aTt%d" % i, [128, DC, 512], BF16, es) for i in range(2)]
        nt = 0; ti = 0
        for r0 in range(0, NK, 512):
            nrow = min(512, NK - r0)
            T = aTt[ti % 2]; ti += 1
            for sub in range(0, nrow, 128):
                xt = xts[nt % 2]; at = ats[nt % 2]; sm = smalls[nt % 2]; nt += 1
                rr0 = r0 + sub
                P.dma("sync", lambda e, o=xt[:], i=hl0.ap[rr0:rr0 + 128, :]: e.dma_start(out=o, in_=i), xt, [hl0])
                w = 1 if rr0 < C else 0
                norm_tile(xt, gs1[w], sh1[w], at, sm)
                transpose_to(at, T, sub)
            P.dma("sync", lambda e, o=aT1.ap[:, :, r0:r0 + nrow], i=T[:, :, 0:nrow]: e.dma_start(out=o, in_=i), aT1, [T], disjoint=True)
        P.barrier(); es.close()
        es = contextlib.ExitStack()
        Wx_ = [P.sb("pWx%d" % i, [128, DC, 256], BF16, es) for i in range(2)]
        Wg_ = [P.sb("pWg%d" % i, [128, DC, 256], BF16, es) for i in range(2)]
        aTt = [P.sb("aTp%d" % i, [128, DC, 512], BF16, es) for i in range(2)]
        xfull = [P.sb("xfull%d" % i, [128, NK], F32, es) for i in range(2)]
        gfull = [P.sb("gfull%d" % i, [128, NK], BF16, es) for i in range(2)]
        ti = 0
        for cc in range(LC // 2):
            Wx = Wx_[cc % 2]; Wg = Wg_[cc % 2]
            wload(Wx, od_w_in.ap[:, cc * 256:(cc + 1) * 256]); wload(Wg, od_w_in.ap[:, LW + cc * 256:LW + (cc + 1) * 256])
            for r0 in range(0, NK, 512):
                nrow = min(512, NK - r0)
                T = aTt[ti % 2]; ti += 1
                P.dma("sync", lambda e, o=T[:, :, 0:nrow], i=aT1.ap[:, :, r0:r0 + nrow]: e.dma_start(out=o, in_=i), T, [aT1])
                for half in range(2):
                    px = psn("x", [2, 3]); pg = psn("g", [4, 5])
                    for k in range(DC):
                        P.op("tensor", lambda e, o=px[:, 0:nrow], a=Wx[:, k, half * 128:(half + 1) * 128], b=T[:, k, 0:nrow], k=k:
                             e.matmul(o, lhsT=a, rhs=b, start=(k == 0), stop=(k == DC - 1)), [Wx, T], [px])
                    for k in range(DC):
                        P.op("tensor", lambda e, o=pg[:, 0:nrow], a=Wg[:, k, half * 128:(half + 1) * 128], b=T[:, k, 0:nrow], k=k:
                             e.matmul(o, lhsT=a, rhs=b, start=(k == 0), stop=(k == DC - 1)), [Wg, T], [pg])
                    P.op("vector", lambda e, o=xfull[half][:, r0:r0 + nrow], a=px[:, 0:nrow]: e.tensor_copy(out=o, in_=a), [px], [xfull[half]])
                    P.op("scalar", lambda e, o=gfull[half][:, r0:r0 + nrow], a=pg[:, 0:nrow]: e.activation(out=o, in_=a, func=AF.Gelu_apprx_tanh),
                         [pg], [gfull[half]])
            for half in range(2):
                ch = cc * 2 + half
                P.dma("sync", lambda e, o=xT_d.ap[ch], i=xfull[half][:]: e.dma_start(out=o, in_=i), xT_d, [xfull[half]], disjoint=True)
                P.dma("sync", lambda e, o=gT_d.ap[ch], i=gfull[half][:, C:NK]: e.dma_start(out=o, in_=i), gT_d, [gfull[half]], disjoint=True)
        P.barrier(); es.close()
        es = contextlib.ExitStack()
        vr = P.sb("vr", [11, LW], F32, es); colv = P.sb("colv", [128, LC, 11], F32, es)
        P.dma("sync", lambda e, o=vr[0:4, :], i=od_cw.ap: e.dma_start(out=o, in_=i), vr)
        P.dma("sync", lambda e, o=vr[4:5, :], i=od_cb.ap: e.dma_start(out=o, in_=i), vr)
        P.dma("sync", lambda e, o=vr[5:11, :], i=od_vec.ap: e.dma_start(out=o, in_=i), vr)
        for k in range(LC):
            pt = psn("t", [0, 1])
            P.op("tensor", lambda e, o=pt[:, 0:11], a=vr[:, k * 128:(k + 1) * 128], idn=ident[0:11, 0:11]:
                 e.transpose(out=o, in_=a, identity=idn), [vr, ident], [pt])
            P.op("vector", lambda e, o=colv[:, k, :], a=pt[:, 0:11]: e.tensor_copy(out=o, in_=a), [pt], [colv])
        sc8 = P.sb("sc8", [128, LC, 2], F32, es)
        P.op("scalar", lambda e, o=sc8[:], a=colv[:, :, 9:11]: e.activation(out=o, in_=a, func=AF.Exp, scale=-1.0), [colv], [sc8])
        P.op("scalar", lambda e, o=sc8[:], a=sc8[:]: e.activation(out=o, in_=a, func=AF.Ln, bias=1.0), [sc8], [sc8])
        P.op("vector", lambda e, o=sc8[:], a=sc8[:]: e.tensor_scalar(out=o, in0=a, scalar1=-8.0, scalar2=None, op0=ALU.mult), [sc8], [sc8])
        xb = P.sb("xb", [128, NK], F32, es)
        ub = [P.sb("ub%d" % i, [128, NK], F32, es) for i in range(2)]
        ubf = P.sb("ubf", [128, 2, NK], BF16, es)
        afull = P.sb("afull", [128, NK], F32, es); bfull = P.sb("bfull", [128, NK], F32, es); ysc = P.sb("ysc", [128, NK], F32, es)
        ysum = P.sb("ysum", [128, S], F32, es); gt = P.sb("gt", [128, S], BF16, es); ygb = P.sb("ygb", [128, S], BF16, es)
        Wa_t = [P.sb("Wa%d" % i, [128, 2, 256], BF16, es) for i in range(2)]
        Wx_t = [P.sb("Wxx%d" % i, [128, 2, 256], BF16, es) for i in range(2)]
        rt_ = [P.sb("rt_%d" % i, [128, 512], F32, es) for i in range(2)]
        it_ = [P.sb("it_%d" % i, [128, 512], F32, es) for i in range(2)]
        t2_ = [P.sb("t2_%d" % i, [128, 512], F32, es) for i in range(2)]
        wi = 0; ri_ = 0
        SCH = 1024

        def scan_cols(dst, cols_fwd, init_ap, init_buf):
            prev = init_ap; pb = init_buf
            for (a0, n, rev) in cols_fwd:
                if rev:
                    sl = slice(a0 + n - 1, (a0 - 1) if a0 > 0 else None, -1)
                    last = slice(a0, a0 + 1)
                else:
                    sl = slice(a0, a0 + n); last = slice(a0 + n - 1, a0 + n)
                rd = [afull, bfull] + ([pb] if pb is not None else [])
                P.op("vector", lambda e, o=dst[:, sl], d0=afull[:, sl], d1=bfull[:, sl], ini=prev:
                     e.tensor_tensor_scan(out=o, data0=d0, data1=d1, initial=ini, op0=ALU.mult, op1=ALU.add), rd, [dst])
                prev = dst[:, last]; pb = dst
        for hb in range(NLB):
            for jc in range(2):
                ch = 2 * hb + jc
                P.dma("sync", lambda e, o=xb[:], i=xT_d.ap[ch]: e.dma_start(out=o, in_=i), xb, [xT_d])
                u = ub[jc]
                P.op("vector", lambda e, o=u[:], a=xb[:], s1=colv[:, ch, 1:2], s2=colv[:, ch, 4:5]:
                     e.tensor_scalar(out=o, in0=a, scalar1=s1, scalar2=s2, op0=ALU.mult, op1=ALU.add), [xb, colv], [u])
                for (a0, a1) in ((0, C), (C, NK)):
                    for (tap, do, di, ln) in ((0, a0 + 1, a0, a1 - a0 - 1), (2, a0, a0 + 1, a1 - a0 - 1), (3, a0, a0 + 2, a1 - a0 - 2)):
                        P.op("vector", lambda e, o=u[:, do:do + ln], a=xb[:, di:di + ln], s=colv[:, ch, tap:tap + 1], b=u[:, do:do + ln]:
                             e.scalar_tensor_tensor(out=o, in0=a, scalar=s, in1=b, op0=ALU.mult, op1=ALU.add), [xb, colv, u], [u])
                P.op("gpsimd", lambda e, o=ubf[:, jc, :], a=u[:]: e.tensor_copy(out=o, in_=a), [u], [ubf])
            for jc in range(2):
                ch = 2 * hb + jc
                for d in range(2):
                    Wa = Wa_t[wi % 2]; Wx = Wx_t[wi % 2]; wi += 1
                    wload(Wa, od_wa.ap[d, hb]); wload(Wx, od_wx.ap[d, hb])
                    for r0 in range(0, NK, 512):
                        nrow = min(512, NK - r0)
                        pa = psn("x", [2, 3]); px = psn("g", [4, 5])
                        for ic in range(2):
                            P.op("tensor", lambda e, o=pa[:, 0:nrow], a=Wa[:, ic, jc * 128:(jc + 1) * 128], b=ubf[:, ic, r0:r0 + nrow], ic=ic:
                                 e.matmul(o, lhsT=a, rhs=b, start=(ic == 0), stop=(ic == 1)), [Wa, ubf], [pa])
                        for ic in range(2):
                            P.op("tensor", lambda e, o=px[:, 0:nrow], a=Wx[:, ic, jc * 128:(jc + 1) * 128], b=ubf[:, ic, r0:r0 + nrow], ic=ic:
                                 e.matmul(o, lhsT=a, rhs=b, start=(ic == 0), stop=(ic == 1)), [Wx, ubf], [px])
                        rt = rt_[ri_ % 2]; itt = it_[ri_ % 2]; t2 = t2_[ri_ % 2]; ri_ += 1
                        P.op("scalar", lambda e, o=rt[:, 0:nrow], a=pa[:, 0:nrow], b=colv[:, ch, 5 + d:6 + d]:
                             e.activation(out=o, in_=a, func=AF.Sigmoid, bias=b), [pa, colv], [rt])
                        P.op("scalar", lambda e, o=afull[:, r0:r0 + nrow], a=rt[:, 0:nrow], s=sc8[:, ch, d:d + 1]:
                             e.activation(out=o, in_=a, func=AF.Exp, scale=s), [rt, sc8], [afull])
                        P.op("scalar", lambda e, o=itt[:, 0:nrow], a=px[:, 0:nrow], b=colv[:, ch, 7 + d:8 + d]:
                             e.activation(out=o, in_=a, func=AF.Sigmoid, bias=b), [px, colv], [itt])
                        P.op("gpsimd", lambda e, o=t2[:, 0:nrow], a=afull[:, r0:r0 + nrow]: e.tensor_tensor(out=o, in0=a, in1=a, op=ALU.mult), [afull], [t2])
                        P.op("scalar", lambda e, o=t2[:, 0:nrow], a=t2[:, 0:nrow]: e.activation(out=o, in_=a, func=AF.Sqrt, scale=-1.0, bias=1.0), [t2], [t2])
                        P.op("gpsimd", lambda e, o=t2[:, 0:nrow], a=t2[:, 0:nrow], b=itt[:, 0:nrow]: e.tensor_tensor(out=o, in0=a, in1=b, op=ALU.mult), [t2, itt], [t2])
                        P.op("vector", lambda e, o=bfull[:, r0:r0 + nrow], a=t2[:, 0:nrow], b=ub[jc][:, r0:r0 + nrow]:
                             e.tensor_tensor(out=o, in0=a, in1=b, op=ALU.mult), [t2, ub[jc]], [bfull])
                    if d == 0:
                        pieces = [(a0, min(SCH, NK - a0), False) for a0 in range(0, NK, SCH)]
                        scan_cols(ysc, pieces, 0.0, None)
                        P.op("gpsimd", lambda e, o=ysum[:], a=ysc[:, C:NK]: e.tensor_copy(out=o, in_=a), [ysc], [ysum])
                    else:
                        pieces = [(0, C, True)] + [(a0, min(SCH, NK - a0), True) for a0 in range(C + ((NK - C - 1) // SCH) * SCH, C - 1, -SCH)]
                        scan_cols(ysc, pieces, 0.0, None)
                        P.op("gpsimd", lambda e, o=ysum[:], a=ysum[:], b=ysc[:, C:NK]: e.tensor_tensor(out=o, in0=a, in1=b, op=ALU.add), [ysum, ysc], [ysum])
                P.dma("sync", lambda e, o=gt[:], i=gT_d.ap[ch]: e.dma_start(out=o, in_=i), gt, [gT_d])
                P.op("vector", lambda e, o=ygb[:], a=ysum[:], b=gt[:]: e.tensor_tensor(out=o, in0=a, in1=b, op=ALU.mult), [ysum, gt], [ygb])
                P.dma("sync", lambda e, o=ygT.ap[:, ch, :], i=ygb[:]: e.dma_start(out=o, in_=i), ygT, [ygb], disjoint=True)
        P.barrier(); es.close()
        if which == "mix1a":
            P.wait_all("sync", [hl0, ygT])
            return P.build(), outs
        pass_out(1, ygT, LC, od_w_out, hl0, C, S, 0, h2, blat1, affT1)
        P.wait_all("sync", [h2, blat1, affT1])
        return P.build(), outs

    if which == "out0":
        NOWN = c.CH + c.SH
        catT_in = din("catT_in", [128, DC, NOWN], BF16); xin = din("xin_own", [NOWN, D])
        w_out_all = din("ev_w_out", [D, D]); router = din("router", [D, 16])
        h1 = dout("h1", [NOWN, D]); blat = dout("blat", [NOWN, D], BF16); affT = dout("affT", [16, NOWN])
        outs += ["h1", "blat", "affT"]
        pass_out(0, catT_in, DC, w_out_all, xin, 0, NOWN, c.CH, h1, blat, affT)
        P.wait_all("sync", [h1, blat, affT])
        return P.build(), outs

    if which == "out1":
        ygT_in = din("ygT_in", [128, c.LC, c.SH], BF16); hl0o = din("hl0_own", [c.SH, D])
        od_w_out = din("od_w_out", [c.LW, D]); router = din("router", [D, 16])
        h2 = dout("h2", [c.SH, D]); blat1 = dout("blat1", [c.SH, D], BF16); affT1 = dout("affT1", [16, c.SH]); outs += ["h2", "blat1", "affT1"]
        pass_out(1, ygT_in, c.LC, od_w_out, hl0o, 0, c.SH, 0, h2, blat1, affT1)
        P.wait_all("sync", [h2, blat1, affT1])
        return P.build(), outs

    if which in ("mix0", "mix0a"):
        xin = din("xin", [NK, D])
        ropet = din("ropet", [NK, 128])
        w_in_all = din("ev_w_in", [D, c.EIN]); w_out_all = din("ev_w_out", [D, D])
        evq = din("evq", [1, 128]); evk = din("evk", [1, 128])
        ev_cw = din("ev_cw", [3, c.SW]); ev_cb = din("ev_cb", [1, c.SW])
        router = din("router", [D, 16])
        aT0 = P.dram("aT0", [128, DC, NK], BF16)
        if which == "mix0a":
            catT = dout("catT", [128, c.HQ + c.SC, NK], BF16); outs += ["catT"]
        else:
            catT = P.dram("catT", [128, DC, NK], BF16)
            h1 = dout("h1", [NK, D]); blat = dout("blat", [NK, D], BF16); affT = dout("affT", [16, NK])
            outs += ["h1", "blat", "affT"]
        def wload(dst, src_ap):
            P.dma("gpsimd", lambda e, o=dst[:], i=src_ap.rearrange("(k p) n -> p k n", p=128): e.dma_start(out=o, in_=i), dst)

        def bc4(ap2, H):
            return bass.AP(ap2.tensor, ap2.offset, [list(ap2.ap[0]), [0, H], list(ap2.ap[1])])

        es = contextlib.ExitStack()
        gs1 = [P.sb("gs1_%d" % w, [128, D], F32, es) for w in range(2)]
        sh1 = [P.sb("sh1_%d" % w, [128, D], F32, es) for w in range(2)]
        for w in range(2):
            bcast_vec(gs1[w], 0, 1, w); bcast_vec(sh1[w], 0, 0, w)
        xts = [P.sb("xt%d" % i, [128, D], F32, es) for i in range(2)]
        ats = [P.sb("at%d" % i, [128, D], F32, es) for i in range(2)]
        smalls = [P.sb("sm%d" % i, [128, 4], F32, es) for i in range(2)]
        aTt = [P.sb("aTt%d" % i, [128, DC, 512], BF16, es) for i in range(2)]

        def pass_a(src, gsl, shl, dstT, nrows, cbound):
            nt = 0; ti = 0
            for r0 in range(0, nrows, 512):
                nrow = min(512, nrows - r0)
                T = aTt[ti % 2]; ti += 1
                for sub in range(0, nrow, 128):
                    xt = xts[nt % 2]; at = ats[nt % 2]; sm = smalls[nt % 2]; nt += 1
                    rr0 = r0 + sub
                    P.dma("sync", lambda e, o=xt[:], i=src.ap[rr0:rr0 + 128, :]: e.dma_start(out=o, in_=i), xt)
                    w = 1 if rr0 < cbound else 0
                    norm_tile(xt, gsl[w], shl[w], at, sm)
                    transpose_to(at, T, sub)
                P.dma("sync", lambda e, o=dstT.ap[:, :, r0:r0 + nrow], i=T[:, :, 0:nrow]: e.dma_start(out=o, in_=i), dstT, [T])
        if upto <= -3:
            d0 = dout("d0", [128, D]); outs.append("d0")
            P.dma("sync", lambda e, o=d0.ap, i=gs1[0][:]: e.dma_start(out=o, in_=i), d0, [gs1[0]])
            P.wait_all("sync", [d0]); es.close(); return P.build(), outs
        pass_a(xin, gs1, sh1, aT0, NK, C)
        P.barrier(); es.close()
        if upto <= -2:
            d0 = dout("d0", [128, DC, NK], BF16); outs.append("d0")
            P.dma("gpsimd", lambda e, o=d0.ap, i=aT0.ap: e.dma_start(out=o, in_=i), d0, [aT0])
            P.wait_all("sync", [d0]); return P.build(), outs

        es = contextlib.ExitStack()
        HQ, HKV, AW, KVW = c.HQ, c.HKV, c.AW, c.KVW
        NKT = NK // 128
        kT = P.sb("kT", [128, HKV, NK], BF16, es)
        qT = P.sb("qT", [128, HQ, NK], BF16, es)
        Vaug = P.sb("Vaug", [128, NKT, HKV, 132], BF16, es)
        import os
        if not os.environ.get("SKV"):
            onesf = P.sb("onesf", [128, 132], F32, es)
            P.op("vector", lambda e, o=onesf[:]: e.memset(o, 1.0), [], [onesf])
            oa = onesf[:]
            P.op("vector", lambda e, o=Vaug[:].rearrange("p a b c -> p (a b) c"),
                 i=bass.AP(oa.tensor, oa.offset, [list(oa.ap[0]), [0, NKT * HKV], [1, 132]]): e.tensor_copy(out=o, in_=i), [onesf], [Vaug])
        gkb = P.sb("gkb", [128, 128], F32, es); gqb = P.sb("gqb", [128, 128], F32, es)
        if not os.environ.get("SKG"):
            P.dma("sync", lambda e, o=gkb[:], i=evk.ap[0:1, :].partition_broadcast(128): e.dma_start(out=o, in_=i), gkb)
            P.dma("sync", lambda e, o=gqb[:], i=evq.ap[0:1, :].partition_broadcast(128): e.dma_start(out=o, in_=i), gqb)
        es2 = contextlib.ExitStack()
        Wkv = P.sb("Wkv", [128, DC, 2 * KVW], BF16, es2); Wq = P.sb("Wq", [128, DC, AW], BF16, es2)
        wload(Wkv, w_in_all.ap[:, AW:AW + 2 * KVW]); wload(Wq, w_in_all.ap[:, 0:AW])
        NBQ = 1 if D > 1024 else 2
        aTt = [P.sb("aTq%d" % i, [128, DC, 512], BF16, es2) for i in range(NBQ)]
        kf = [P.sb("kf%d" % i, [128, 4 * 128], F32, es2) for i in range(2)]
        sq = [P.sb("sq%d" % i, [128, 4 * 128], F32, es2) for i in range(2)]
        kr = [P.sb("kr%d" % i, [128, 4 * 128], F32, es2) for i in range(2)]
        hs = [P.sb("hs%d" % i, [128, 8], F32, es2) for i in range(2)]
        rts = [P.sb("rt%d" % i, [128, 128], F32, es2) for i in range(2)]
        hn = [0]

        import os
        SKF = os.environ.get("SKF", "")
        _realop = P.op
        def headnorm_rope(ps_ap, H, gb, rt, dstT, h0, tok0):
            i = hn[0] % 2; hn[0] += 1
            cnt = [0]
            class PX:
                def op(self, eng, fn, r=(), w=()):
                    cnt[0] += 1
                    if ("%02d" % cnt[0]) in SKF.split(","):
                        return
                    return _realop(eng, fn, r, w)
            P_ = PX()
            f, s_, r_, h_ = kf[i], sq[i], kr[i], hs[i]
            n = H * 128
            P_.op("vector", lambda e, o=f[:, 0:n], a=ps_ap: e.tensor_copy(out=o, in_=a), [ps_ap_buf[0]], [f])
            P_.op("vector", lambda e, o=s_[:, 0:n], a=f[:, 0:n]: e.tensor_tensor(out=o, in0=a, in1=a, op=ALU.mult), [f], [s_])
            P_.op("vector", lambda e, o=h_[:, 0:H], a=s_[:, 0:n].rearrange("p (h d) -> p h d", h=H):
                 e.tensor_reduce(out=o, in_=a, axis=AX.X, op=ALU.add), [s_], [h_])
            P_.op("scalar", lambda e, o=h_[:, 0:H], a=h_[:, 0:H]: e.activation(out=o, in_=a, func=AF.Sqrt, scale=1.0 / 128, bias=EPS), [h_], [h_])
            P_.op("vector", lambda e, o=h_[:, 0:H], a=h_[:, 0:H]: e.reciprocal(out=o, in_=a), [h_], [h_])
            f3 = f[:, 0:n].rearrange("p (h d) -> p h d", h=H)
            hb_ = h_[:, 0:H]
            rb = bass.AP(hb_.tensor, hb_.offset, [list(hb_.ap[0]), list(hb_.ap[1]), [0, 128]])
            P_.op("vector", lambda e, o=f3, a=f3, b=rb: e.tensor_tensor(out=o, in0=a, in1=b, op=ALU.mult), [f, h_], [f])
            P_.op("vector", lambda e, o=f3, a=f3, b=bc4(gb[:], H): e.tensor_tensor(out=o, in0=a, in1=b, op=ALU.mult), [f, gb], [f])
            f5 = f[:, 0:n].rearrange("p (g t j) -> p g t j", t=2, j=32)
            r5 = r_[:, 0:n].rearrange("p (g t j) -> p g t j", t=2, j=32)
            s5 = s_[:, 0:n].rearrange("p (g t j) -> p g t j", t=2, j=32)
            u1, u2 = f5[:, :, 0, :], f5[:, :, 1, :]
            cs = rt[:, 0:64]; sn = rt[:, 64:128]
            def tb(a2):
                return bass.AP(a2.tensor, a2.offset, [list(a2.ap[0]), [0, H], [32, 2], [1, 32]])
            u1v = bass.AP(u1.tensor, u1.offset, [list(u1.ap[0]), [128, H], [64, 2], [1, 32]])
            u2v = bass.AP(u2.tensor, u2.offset, [list(u2.ap[0]), [128, H], [64, 2], [1, 32]])
            o1 = r5[:, :, 0, :]; o2 = r5[:, :, 1, :]
            o1v = bass.AP(o1.tensor, o1.offset, [list(o1.ap[0]), [128, H], [64, 2], [1, 32]])
            o2v = bass.AP(o2.tensor, o2.offset, [list(o2.ap[0]), [128, H], [64, 2], [1, 32]])
            t1 = s5[:, :, 0, :]; t2 = s5[:, :, 1, :]
            t1v = bass.AP(t1.tensor, t1.offset, [list(t1.ap[0]), [128, H], [64, 2], [1, 32]])
            t2v = bass.AP(t2.tensor, t2.offset, [list(t2.ap[0]), [128, H], [64, 2], [1, 32]])
            P_.op("vector", lambda e, o=o1v, a=u1v, b=tb(cs): e.tensor_tensor(out=o, in0=a, in1=b, op=ALU.mult), [f, rt], [r_])
            P_.op("vector", lambda e, o=t1v, a=u2v, b=tb(sn): e.tensor_tensor(out=o, in0=a, in1=b, op=ALU.mult), [f, rt], [s_])
            P_.op("vector", lambda e, o=o1v, a=o1v, b=t1v: e.tensor_tensor(out=o, in0=a, in1=b, op=ALU.subtract), [r_, s_], [r_])
            P_.op("vector", lambda e, o=o2v, a=u1v, b=tb(sn): e.tensor_tensor(out=o, in0=a, in1=b, op=ALU.mult), [f, rt], [r_])
            P_.op("vector", lambda e, o=t2v, a=u2v, b=tb(cs): e.tensor_tensor(out=o, in0=a, in1=b, op=ALU.mult), [f, rt], [s_])
            P_.op("vector", lambda e, o=o2v, a=o2v, b=t2v: e.tensor_tensor(out=o, in0=a, in1=b, op=ALU.add), [r_, s_], [r_])
            pt = psn("t", [0, 1])
            for h in range(H):
                P_.op("tensor", lambda e, o=pt[:, h * 128:(h + 1) * 128], a=r_[:, h * 128:(h + 1) * 128], idn=ident[:]:
                     e.transpose(out=o, in_=a, identity=idn), [r_, ident], [pt])
            P_.op("scalar", lambda e, o=dstT[:, h0:h0 + H, tok0:tok0 + 128], a=pt[:, 0:n].rearrange("p (h t) -> p h t", h=H):
                 e.copy(out=o, in_=a), [pt], [dstT])

        ps_ap_buf = [None]
        ti = 0; ri = 0
        for r0 in range(0, NK, 512):
            nrow = min(512, NK - r0)
            T = aTt[ti % NBQ]; ti += 1
            P.dma("sync", lambda e, o=T[:, :, 0:nrow], i=aT0.ap[:, :, r0:r0 + nrow]: e.dma_start(out=o, in_=i), T, [aT0])
            for sub in range(0, nrow, 128):
                tok0 = r0 + sub; kt = tok0 // 128
                rt = rts[ri % 2]; ri += 1
                P.dma("sync", lambda e, o=rt[:], i=ropet.ap[tok0:tok0 + 128, :]: e.dma_start(out=o, in_=i), rt)
                pk = psn("p", [2, 3])
                for k in range(DC):
                    P.op("tensor", lambda e, o=pk[:, 0:2 * KVW], a=T[:, k, sub:sub + 128], b=Wkv[:, k, :], k=k:
                         e.matmul(o, lhsT=a, rhs=b, start=(k == 0), stop=(k == DC - 1)), [T, Wkv], [pk])
                for hh in range(HKV):
                    P.op("vector", lambda e, o=Vaug[:, kt, hh, 0:128], a=pk[:, KVW + hh * 128:KVW + (hh + 1) * 128]:
                         e.tensor_copy(out=o, in_=a), [pk], [Vaug])
                ps_ap_buf[0] = pk
                headnorm_rope(pk[:, 0:KVW], HKV, gkb, rt, kT, 0, tok0)
                for q0 in range(0, AW, 512):
                    nq = min(512, AW - q0); Hh = nq // 128
                    pq = psn("p", [2, 3])
                    for k in range(DC):
                        P.op("tensor", lambda e, o=pq[:, 0:nq], a=T[:, k, sub:sub + 128], b=Wq[:, k, q0:q0 + nq], k=k:
                             e.matmul(o, lhsT=a, rhs=b, start=(k == 0), stop=(k == DC - 1)), [T, Wq], [pq])
                    ps_ap_buf[0] = pq
                    headnorm_rope(pq[:, 0:nq], Hh, gqb, rt, qT, q0 // 128, tok0)
        P.barrier(); es2.close()
        if upto <= -1:
            d0 = dout("d0", [128, HQ, NK], BF16); outs.append("d0")
            P.dma("sync", lambda e, o=d0.ap, i=qT[:]: e.dma_start(out=o, in_=i), d0, [qT])
            P.wait_all("sync", [d0]); es.close(); return P.build(), outs

        es2 = contextlib.ExitStack()
        ptb = [P.sb("ptb%d" % i, [128, 512], BF16, es2) for i in range(3)]
        catg = [P.sb("catg%d" % i, [128, HQ, 512], BF16, es2) for i in range(2)]
        ofs = [P.sb("of%d" % i, [128, 128], F32, es2) for i in range(2)]
        rsm = [P.sb("rsm%d" % i, [128, 1], F32, es2) for i in range(2)]
        groups = [(0, C, C)] + [(C + g * 512, 512, NK) for g in range(S // 512)]
        sc = 128 ** -0.5
        pi = 0; oi = 0
        for gi, (q0, nq, nkeys) in enumerate(groups):
            cg = catg[gi % 2]
            nsub = nq // 128
            for hq in range(HQ):
                hk = hq // (HQ // HKV)
                nkt = nkeys // 128
                for kt in range(nkt):
                    sp = psn("s", [2, 3])
                    P.op("tensor", lambda e, o=sp[:, 0:nq], a=kT[:, hk, kt * 128:(kt + 1) * 128], b=qT[:, hq, q0:q0 + nq]:
                         e.matmul(o, lhsT=a, rhs=b, start=True, stop=True), [kT, qT], [sp])
                    pb = ptb[pi % 3]; pi += 1
                    P.op("scalar", lambda e, o=pb[:, 0:nq], a=sp[:, 0:nq]: e.activation(out=o, in_=a, func=AF.Exp, scale=sc), [sp], [pb])
                    for qs in range(nsub):
                        P.op("tensor", lambda e, o=PS[4 + qs][:, 0:129], a=pb[:, qs * 128:(qs + 1) * 128], b=Vaug[:, kt, hk, 0:129], kt=kt:
                             e.matmul(o, lhsT=a, rhs=b, start=(kt == 0), stop=(kt == nkt - 1)), [pb, Vaug], [PS[4 + qs]])
                pt = psn("t", [0, 1])
                for qs in range(nsub):
                    of = ofs[oi % 2]; rs = rsm[oi % 2]; oi += 1
                    P.op("vector", lambda e, o=rs[:], a=PS[4 + qs][:, 128:129]: e.reciprocal(out=o, in_=a), [PS[4 + qs]], [rs])
                    P.op("vector", lambda e, o=of[:], a=PS[4 + qs][:, 0:128], s=rs[:, 0:1]:
                         e.tensor_scalar(out=o, in0=a, scalar1=s, scalar2=None, op0=ALU.mult), [PS[4 + qs], rs], [of])
                    P.op("tensor", lambda e, o=pt[:, qs * 128:(qs + 1) * 128], a=of[:], idn=ident[:]:
                         e.transpose(out=o, in_=a, identity=idn), [of, ident], [pt])
                P.op("scalar", lambda e, o=cg[:, hq, 0:nq], a=pt[:, 0:nq]: e.copy(out=o, in_=a), [pt], [cg])
            P.dma("sync", lambda e, o=catT.ap[:, 0:HQ, q0:q0 + nq], i=cg[:, :, 0:nq]: e.dma_start(out=o, in_=i), catT, [cg])
        P.barrier(); es2.close(); es.close()
        if upto <= 0:
            d0 = dout("d0", [128, DC, NK], BF16); outs.append("d0")
            P.dma("gpsimd", lambda e, o=d0.ap, i=catT.ap: e.dma_start(out=o, in_=i), d0, [catT])
            P.wait_all("sync", [d0]); return P.build(), outs

        es = contextlib.ExitStack()
        SC, SW = c.SC, c.SW
        cwr = P.sb("cwr", [4, SW], F32, es); cwt = P.sb("cwt", [128, SC, 4], F32, es)
        P.dma("sync", lambda e, o=cwr[0:3, :], i=ev_cw.ap: e.dma_start(out=o, in_=i), cwr)
        P.dma("sync", lambda e, o=cwr[3:4, :], i=ev_cb.ap: e.dma_start(out=o, in_=i), cwr)
        for k in range(SC):
            pt = psn("t", [0, 1])
            P.op("tensor", lambda e, o=pt[:, 0:4], a=cwr[:, k * 128:(k + 1) * 128], idn=ident[0:4, 0:4]:
                 e.transpose(out=o, in_=a, identity=idn), [cwr, ident], [pt])
            P.op("vector", lambda e, o=cwt[:, k, :], a=pt[:, 0:4]: e.tensor_copy(out=o, in_=a), [pt], [cwt])
        Wb3 = [[P.sb("Wb%d_%d" % (i, j), [128, DC, 128], BF16, es) for j in range(3)] for i in range(2)]
        aTt = [P.sb("aTc%d" % i, [128, DC, 512], BF16, es) for i in range(2)]
        cuf = [P.sb("cuf%d" % i, [128, NK], F32, es) for i in range(2)]
        Bf = [P.sb("Bf%d" % i, [128, NK], F32, es) for i in range(2)]
        accf = [P.sb("accf%d" % i, [128, NK], F32, es) for i in range(2)]
        outb = [P.sb("outb%d" % i, [128, NK], BF16, es) for i in range(2)]
        tmpc = [P.sb("tmpc%d" % i, [128, 512], F32, es) for i in range(2)]
        base = AW + 2 * KVW
        ti = 0; tci = 0
        for ch in range(SC):
            W3 = Wb3[ch % 2]
            for j in range(3):
                wload(W3[j], w_in_all.ap[:, base + j * SW + ch * 128: base + j * SW + (ch + 1) * 128])
            cu, Bt, ac, ob = cuf[ch % 2], Bf[ch % 2], accf[ch % 2], outb[ch % 2]
            for r0 in range(0, NK, 512):
                nrow = min(512, NK - r0)
                T = aTt[ti % 2]; ti += 1
                P.dma("sync", lambda e, o=T[:, :, 0:nrow], i=aT0.ap[:, :, r0:r0 + nrow]: e.dma_start(out=o, in_=i), T, [aT0])
                pp = []
                for j in range(3):
                    pj = psn("c", [2, 3, 4, 5, 6, 7])
                    for k in range(DC):
                        P.op("tensor", lambda e, o=pj[:, 0:nrow], a=W3[j][:, k, :], b=T[:, k, 0:nrow], k=k:
                             e.matmul(o, lhsT=a, rhs=b, start=(k == 0), stop=(k == DC - 1)), [W3[j], T], [pj])
                    pp.append(pj)
                tc_ = tmpc[tci % 2]; tci += 1
                P.op("scalar", lambda e, o=Bt[:, r0:r0 + nrow], a=pp[0][:, 0:nrow]: e.copy(out=o, in_=a), [pp[0]], [Bt])
                P.op("scalar", lambda e, o=tc_[:, 0:nrow], a=pp[1][:, 0:nrow]: e.copy(out=o, in_=a), [pp[1]], [tc_])
                P.op("vector", lambda e, o=cu[:, r0:r0 + nrow], a=tc_[:, 0:nrow], b=pp[2][:, 0:nrow]:
                     e.tensor_tensor(out=o, in0=a, in1=b, op=ALU.mult), [tc_, pp[2]], [cu])
            P.op("vector", lambda e, o=ac[:], a=cu[:], s1=cwt[:, ch, 1:2], s2=cwt[:, ch, 3:4]:
                 e.tensor_scalar(out=o, in0=a, scalar1=s1, scalar2=s2, op0=ALU.mult, op1=ALU.add), [cu, cwt], [ac])
            for (a0, a1) in ((0, C), (C, NK)):
                P.op("vector", lambda e, o=ac[:, a0 + 1:a1], a=cu[:, a0:a1 - 1], s=cwt[:, ch, 0:1], b=ac[:, a0 + 1:a1]:
                     e.scalar_tensor_tensor(out=o, in0=a, scalar=s, in1=b, op0=ALU.mult, op1=ALU.add), [cu, cwt, ac], [ac])
                P.op("vector", lambda e, o=ac[:, a0:a1 - 1], a=cu[:, a0 + 1:a1], s=cwt[:, ch, 2:3], b=ac[:, a0:a1 - 1]:
                     e.scalar_tensor_tensor(out=o, in0=a, scalar=s, in1=b, op0=ALU.mult, op1=ALU.add), [cu, cwt, ac], [ac])
            P.op("gpsimd", lambda e, o=ob[:], a=ac[:], b=Bt[:]: e.tensor_tensor(out=o, in0=a, in1=b, op=ALU.mult), [ac, Bt], [ob])
            P.dma("sync", lambda e, o=catT.ap[:, HQ + ch, :], i=ob[:]: e.dma_start(out=o, in_=i), catT, [ob])
        P.barrier(); es.close()

        if which == "mix0a":
            P.wait_all("sync", [catT])
            return P.build(), outs
        pass_out(0, catT, DC, w_out_all, xin, 0, NK, C, h1, blat, affT)
        P.wait_all("sync", [h1, blat, affT])
        return P.build(), outs


def build_moe(cfg, has_ctx):
    c = cfg
    D, S, C, DC, FF, FC = c.D, c.S, c.C, c.DC, c.FF, c.FC
    C0 = C if has_ctx else 0
    NKl = S + C0
    cap, capc = c.cap, c.capc
    NT = cap // 128
    P = Prog()
    din = lambda n, s, dt=F32: P.dram(n, s, dt, kind="ExternalInput")
    dout = lambda n, s, dt=F32: P.dram(n, s, dt, kind="ExternalOutput")
    affrows = din("affrows", [8, NKl]); blat_all = din("blat_all", [4 * NKl, D], BF16)
    wg = din("wg", [2, D, FF]); wu = din("wu", [2, D, FF]); wd = din("wd", [2, FF, D])
    rowoff = din("rowoff", [8, 1]); ident_in = din("ident", [128, 128])
    slots = dout("slots", [8, cap, D]); ridx = dout("ridx", [8, cap], I32)
    outs = ["slots", "ridx"]
    if has_ctx:
        slotsc = dout("slotsc", [8, capc, D]); ridxc = dout("ridxc", [8, capc], I32); outs += ["slotsc", "ridxc"]
    PS = [P.ps("ps%d" % i, [128, 512], F32) for i in range(8)]
    rr = {}

    def psn(group, banks):
        i = rr.get(group, 0); rr[group] = i + 1
        return PS[banks[i % len(banks)]]
    ident = P.sb("identf", [128, 128], F32)
    P.dma("sync", lambda e, o=ident[:], i=ident_in[:]: e.dma_start(out=o, in_=i), ident)
    identb = P.sb("identb", [128, 128], BF16)
    P.op("vector", lambda e, o=identb[:], i=ident[:]: e.tensor_copy(out=o, in_=i), [ident], [identb])
    rof = P.sb("rof", [8, 1], F32)
    P.dma("sync", lambda e, o=rof[:], i=rowoff.ap: e.dma_start(out=o, in_=i), rof)
    gT = P.sb("gT", [128, NT, 8], F32); gixT = P.sb("gixT", [128, NT, 8], I32)
    gcT = P.sb("gcT", [32, 8], F32); gixcT = P.sb("gixcT", [32, 8], I32)
    es = contextlib.ExitStack()
    at = P.sb("at", [8, NKl], F32, es); wk = P.sb("wk", [8, S], F32, es)
    P.dma("sync", lambda e, o=at[:], i=affrows.ap: e.dma_start(out=o, in_=i), at)

    def topk(src_ap, srcbuf, n, k, work, off, tagv):
        vals = P.sb("vals" + tagv, [8, k], F32, es); idxu = P.sb("idxu" + tagv, [8, k], U32, es)
        idxf = P.sb("idxf" + tagv, [8, k], F32, es); gi = P.sb("gi" + tagv, [8, k], F32, es); ri = P.sb("ri" + tagv, [8, k], I32, es)
        cur = src_ap; curb = srcbuf
        for it in range(k // 8):
            v8 = vals[:, it * 8:(it + 1) * 8]
            P.op("vector", lambda e, o=v8, a=cur: e.max(out=o, in_=a), [curb], [vals])
            P.op("vector", lambda e, o=idxu[:, it * 8:(it + 1) * 8], m=v8, a=cur: e.max_index(out=o, in_max=m, in_values=a), [curb, vals], [idxu])
            if it < k // 8 - 1:
                P.op("vector", lambda e, o=work[:, 0:n], m=v8, a=cur: e.match_replace(out=o, in_to_replace=m, in_values=a, imm_value=-1.0),
                     [curb, vals], [work])
                cur = work[:, 0:n]; curb = work
        P.op("vector", lambda e, o=idxf[:], a=idxu[:]: e.tensor_copy(out=o, in_=a), [idxu], [idxf])
        P.op("vector", lambda e, o=gi[:], a=idxf[:], s=rof[:, 0:1]: e.tensor_scalar(out=o, in0=a, scalar1=s, scalar2=float(off), op0=ALU.add, op1=ALU.add),
             [idxf, rof], [gi])
        P.op("vector", lambda e, o=idxf[:], a=idxf[:]: e.tensor_scalar(out=o, in0=a, scalar1=float(off), scalar2=None, op0=ALU.add), [idxf], [idxf])
        P.op("vector", lambda e, o=ri[:], a=idxf[:]: e.tensor_copy(out=o, in_=a), [idxf], [ri])
        return vals, gi, ri
    vals, gi, ri = topk(at[:, C0:NKl], at, S, cap, wk, C0, "l")
    P.dma("sync", lambda e, o=ridx.ap, i=ri[:]: e.dma_start(out=o, in_=i), ridx, [ri])
    for j in range(NT):
        pt = psn("t", [0, 1])
        P.op("tensor", lambda e, o=pt[:, 0:8], a=gi[:, j * 128:(j + 1) * 128], idn=ident[0:8, 0:8]: e.transpose(out=o, in_=a, identity=idn), [gi, ident], [pt])
        P.op("tensor", lambda e, o=pt[:, 8:16], a=vals[:, j * 128:(j + 1) * 128], idn=ident[0:8, 0:8]: e.transpose(out=o, in_=a, identity=idn), [vals, ident], [pt])
        P.op("vector", lambda e, o=gixT[:, j, :], a=pt[:, 0:8]: e.tensor_copy(out=o, in_=a), [pt], [gixT])
        P.op("vector", lambda e, o=gT[:, j, :], a=pt[:, 8:16]: e.tensor_copy(out=o, in_=a), [pt], [gT])
    if has_ctx:
        wkc = P.sb("wkc", [8, C], F32, es)
        valsc, gic, ric = topk(at[:, 0:C], at, C, capc, wkc, 0, "c")
        P.dma("sync", lambda e, o=ridxc.ap, i=ric[:]: e.dma_start(out=o, in_=i), ridxc, [ric])
        pt = psn("t", [0, 1])
        P.op("tensor", lambda e, o=pt[0:capc, 0:8], a=gic[:, :], idn=ident[0:8, 0:8]: e.transpose(out=o, in_=a, identity=idn), [gic, ident], [pt])
        P.op("tensor", lambda e, o=pt[0:capc, 8:16], a=valsc[:, :], idn=ident[0:8, 0:8]: e.transpose(out=o, in_=a, identity=idn), [valsc, ident], [pt])
        P.op("vector", lambda e, o=gixcT[0:capc, :], a=pt[0:capc, 0:8]: e.tensor_copy(out=o, in_=a), [pt], [gixcT])
        P.op("vector", lambda e, o=gcT[0:capc, :], a=pt[0:capc, 8:16]: e.tensor_copy(out=o, in_=a), [pt], [gcT])
    P.barrier(); es.close()
    NSL = 2 * cap + (2 * capc if has_ctx else 0)
    xsT = P.sb("xsT", [128, DC, NSL], BF16); hidT = P.sb("hidT", [128, FC, NSL], BF16)
    xg = [P.sb("xg%d" % i, [128, D], BF16) for i in range(2)]
    Wt = [[P.sb("W%d_%d" % (m, i), [128, DC, 256], BF16) for i in range(2)] for m in range(3)]
    sg = [P.sb("sg%d" % i, [128, 512], F32) for i in range(2)]
    ot = [P.sb("ot%d" % i, [128, 256], F32) for i in range(4)]
    wi = [0, 0, 0]; xi = 0; si = 0; oi = 0

    def wl(m, src_ap):
        t = Wt[m][wi[m] % 2]; wi[m] += 1
        P.dma("gpsimd", lambda e, o=t[:], i=src_ap.rearrange("(k p) n -> p k n", p=128): e.dma_start(out=o, in_=i), t)
        return t
    for el in range(2):
        for bg in range(2):
            tiles = []
            s0 = 0
            for b in (2 * bg, 2 * bg + 1):
                r8 = el * 4 + b
                for j in range(NT):
                    tiles.append((s0, 128, r8, 0, j)); s0 += 128
                if has_ctx:
                    tiles.append((s0, capc, r8, 1, 0)); s0 += capc
            nsl = s0
            for (t0, ns, r8, kind, j) in tiles:
                g = xg[xi % 2]; xi += 1
                ixap = gixcT[0:ns, r8:r8 + 1] if kind else gixT[:, j, r8:r8 + 1]
                ixb = gixcT if kind else gixT
                P.dma("gpsimd", lambda e, o=g[0:ns, :], ix=ixap: e.indirect_dma_start(
                    out=o, out_offset=None, in_=blat_all.ap, in_offset=bass.IndirectOffsetOnAxis(ap=ix, axis=0)), g, [ixb, blat_all])
                for k4 in range(0, DC, 4):
                    pt = psn("t", [0, 1])
                    ptb = pt[:].bitcast(BF16)
                    for kk in range(4):
                        k = k4 + kk
                        P.op("tensor", lambda e, o=ptb[:, kk * 128:kk * 128 + ns], a=g[0:ns, k * 128:(k + 1) * 128], idn=identb[0:ns, 0:ns]:
                             e.transpose(out=o, in_=a, identity=idn), [g, identb], [pt])
                    P.op("scalar", lambda e, o=xsT[:, k4:k4 + 4, t0:t0 + ns], a=ptb[:, 0:512].rearrange("p (k t) -> p k t", k=4)[:, :, 0:ns]:
                         e.copy(out=o, in_=a), [pt], [xsT])
            for f2 in range(FF // 256):
                Wg_ = wl(0, wg.ap[el, :, f2 * 256:(f2 + 1) * 256]); Wu_ = wl(1, wu.ap[el, :, f2 * 256:(f2 + 1) * 256])
                for half in range(2):
                    fc = f2 * 2 + half
                    for c0 in range(0, nsl, 512):
                        ns = min(512, nsl - c0)
                        pg = psn("g", [2, 3]); pu = psn("u", [4, 5])
                        for k in range(DC):
                            P.op("tensor", lambda e, o=pg[:, 0:ns], a=Wg_[:, k, half * 128:(half + 1) * 128], b=xsT[:, k, c0:c0 + ns], k=k:
                                 e.matmul(o, lhsT=a, rhs=b, start=(k == 0), stop=(k == DC - 1)), [Wg_, xsT], [pg])
                        for k in range(DC):
                            P.op("tensor", lambda e, o=pu[:, 0:ns], a=Wu_[:, k, half * 128:(half + 1) * 128], b=xsT[:, k, c0:c0 + ns], k=k:
                                 e.matmul(o, lhsT=a, rhs=b, start=(k == 0), stop=(k == DC - 1)), [Wu_, xsT], [pu])
                        s_ = sg[si % 2]; si += 1
                        P.op("scalar", lambda e, o=s_[:, 0:ns], a=pg[:, 0:ns]: e.activation(out=o, in_=a, func=AF.Silu), [pg], [s_])
                        P.op("vector", lambda e, o=hidT[:, fc, c0:c0 + ns], a=s_[:, 0:ns], b=pu[:, 0:ns]:
                             e.tensor_tensor(out=o, in0=a, in1=b, op=ALU.mult), [s_, pu], [hidT])
            for n0 in range(0, D, 256):
                Wd_ = wl(2, wd.ap[el, :, n0:n0 + 256])
                for (t0, ns, r8, kind, j) in tiles:
                    po = psn("o", [6, 7])
                    for f in range(FC):
                        P.op("tensor", lambda e, o=po[0:ns, 0:256], a=hidT[:, f, t0:t0 + ns], b=Wd_[:, f, :], f=f:
                             e.matmul(o, lhsT=a, rhs=b, start=(f == 0), stop=(f == FC - 1)), [hidT, Wd_], [po])
                    o_ = ot[oi % 4]; oi += 1
                    gap = gcT[0:ns, r8:r8 + 1] if kind else gT[:, j, r8:r8 + 1]
                    P.op("vector", lambda e, o=o_[0:ns, :], a=po[0:ns, 0:256], s=gap: e.tensor_scalar(out=o, in0=a, scalar1=s, scalar2=None, op0=ALU.mult),
                         [po, gcT if kind else gT], [o_])
                    if kind:
                        P.dma("sync", lambda e, o=slotsc.ap[r8, 0:ns, n0:n0 + 256], i=o_[0:ns, :]: e.dma_start(out=o, in_=i), slotsc, [o_], disjoint=True)
                    else:
                        P.dma("sync", lambda e, o=slots.ap[r8, j * 128:(j + 1) * 128, n0:n0 + 256], i=o_[0:ns, :]: e.dma_start(out=o, in_=i), slots, [o_], disjoint=True)
    P.wait_all("sync", [slots, ridx] + ([slotsc, ridxc] if has_ctx else []))
    return P.build(), outs


def rope_table(cfg):
    S, C = cfg.S, cfg.C
    t = np.arange(S); row = t // 64; col = t % 64
    freqs = (10000.0 ** (-np.arange(0, 64, 2, dtype=np.float32) / 64)).astype(np.float32)
    ar = row[:, None].astype(np.float32) * freqs[None]; ac = col[:, None].astype(np.float32) * freqs[None]
    tab = np.zeros((cfg.NK, 128), np.float32)
    tab[:C, 0:64] = 1.0
    tab[C:, 0:32] = np.cos(ar); tab[C:, 32:64] = np.cos(ac); tab[C:, 64:96] = np.sin(ar); tab[C:, 96:128] = np.sin(ac)
    return tab


def _run(nc, maps, tag):
    t0 = time.time()
    maps = [{k: np.ascontiguousarray(v) for k, v in m.items()} for m in maps]
    res = _bu.run_bass_kernel_spmd(nc, maps, core_ids=list(range(len(maps))))
    print("launch", tag, "%.1fs" % (time.time() - t0), "exec_ns", getattr(res, "exec_time_ns", None), flush=True)
    return res.results


def pipeline(kb, cfg, I, progs=None):
    kb = _KB
    D, S, C, NK, D8 = cfg.D, cfg.S, cfg.C, cfg.NK, cfg.D8
    ident = np.eye(128, dtype=np.float32)
    sel = np.zeros((2, 256), np.float32); sel[0, :128] = 1; sel[1, 128:] = 1
    nc, _ = kb.build_mod(cfg)
    maps = []
    for r in range(8):
        maps.append(dict(c5=np.concatenate([I["c"], I["c_ctx"][None]], 0),
                         modw=I["mod_w"].reshape(2, D, 6, 8, D8)[:, :, :, r, :].reshape(2, D, 6 * D8),
                         modb=I["mod_b"].reshape(2, 6, 8, D8)[:, :, r, :].reshape(2, 6 * D8), ident=ident))
    res = _run(nc, maps, "mod")
    mod = np.stack([res[r]["mloc_out"].reshape(5, 2, 6, D8) for r in range(8)], 3).reshape(5, 2, 6, D)

    def common(b):
        return dict(modrows=np.stack([mod[b].reshape(-1), mod[4].reshape(-1)], 0), n1g=I["norm1_g"], n2g=I["norm2_g"],
                    ident=ident, selin=sel)
    hc = kb.half_cfg(cfg)
    CH, SH = cfg.CH, cfg.SH
    nc, _ = kb.build(hc, "mix0a")
    rt = rope_table(cfg)
    Wi = I["ev_w_in"][0]
    AW, KVW, SW = cfg.AW, cfg.KVW, cfg.SW
    maps = []
    for r in range(8):
        b, s = r // 2, r % 2
        q0 = s * hc.AW
        kvh0 = (s * cfg.HKV // 2) * 128 if cfg.HKV >= 2 else 0
        cols = [Wi[:, q0:q0 + hc.AW], Wi[:, AW + kvh0:AW + kvh0 + hc.KVW], Wi[:, AW + KVW + kvh0:AW + KVW + kvh0 + hc.KVW]]
        for j in range(3):
            c0 = AW + 2 * KVW + j * SW + s * hc.SW
            cols.append(Wi[:, c0:c0 + hc.SW])
        m = common(b)
        m.update(xin=np.concatenate([I["ctx"][b], I["x"][b]], 0), ropet=rt, ev_w_in=np.concatenate(cols, 1), ev_w_out=I["ev_w_out"][0],
                 evq=I["ev_q_norm"], evk=I["ev_k_norm"], ev_cw=I["ev_conv_w"][0][:, s * hc.SW:(s + 1) * hc.SW],
                 ev_cb=I["ev_conv_b"][:, s * hc.SW:(s + 1) * hc.SW], router=I["moe_router"][0])
        maps.append(m)
    ra = _run(nc, maps, "mix0a")
    nc, _ = kb.build(cfg, "out0")
    maps = []
    HQh, SCh = hc.HQ, hc.SC
    for r in range(8):
        b, s = r // 2, r % 2
        c0, c1 = ra[2 * b]["catT"], ra[2 * b + 1]["catT"]
        full = np.concatenate([c0[:, :HQh], c1[:, :HQh], c0[:, HQh:], c1[:, HQh:]], 1)
        colsel = np.concatenate([np.arange(s * CH, (s + 1) * CH), C + np.arange(s * SH, (s + 1) * SH)])
        m = common(b)
        m.update(catT_in=full[:, :, colsel], xin_own=np.concatenate([I["ctx"][b][s * CH:(s + 1) * CH], I["x"][b][s * SH:(s + 1) * SH]], 0),
                 ev_w_out=I["ev_w_out"][0], router=I["moe_router"][0])
        maps.append(m)
    rb = _run(nc, maps, "out0")
    r0 = []
    for b in range(4):
        a0, a1 = rb[2 * b], rb[2 * b + 1]
        r0.append(dict(h1=np.concatenate([a0["h1"][:CH], a1["h1"][:CH], a0["h1"][CH:], a1["h1"][CH:]], 0),
                       blat=np.concatenate([a0["blat"][:CH], a1["blat"][:CH], a0["blat"][CH:], a1["blat"][CH:]], 0),
                       affT=np.concatenate([a0["affT"][:, :CH], a1["affT"][:, :CH], a0["affT"][:, CH:], a1["affT"][:, CH:]], 1)))

    def moe(layer, blat_list, aff_list, has_ctx):
        NKl = NK if has_ctx else S
        nc, _ = kb.build_moe(cfg, has_ctx)
        blat_all = np.concatenate(blat_list, 0)
        maps = []
        for k in range(8):
            maps.append(dict(blat_all=blat_all, ident=ident,
                             affrows=np.stack([aff_list[b][2 * k + el] for el in range(2) for b in range(4)], 0),
                             rowoff=np.array([[b * NKl] for el in range(2) for b in range(4)], np.float32),
                             wg=I["moe_w_gate"][layer, 2 * k:2 * k + 2], wu=I["moe_w_up"][layer, 2 * k:2 * k + 2],
                             wd=I["moe_w_down"][layer, 2 * k:2 * k + 2]))
        rs = _run(nc, maps, "moe%d" % layer)
        outb = []
        for b in range(4):
            d = dict(slots_b=np.stack([rs[e // 2]["slots"][(e % 2) * 4 + b] for e in range(16)], 0),
                     ridx_b=np.stack([rs[e // 2]["ridx"][(e % 2) * 4 + b] for e in range(16)], 0))
            if has_ctx:
                d.update(slotsc_b=np.stack([rs[e // 2]["slotsc"][(e % 2) * 4 + b] for e in range(16)], 0),
                         ridxc_b=np.stack([rs[e // 2]["ridxc"][(e % 2) * 4 + b] for e in range(16)], 0))
            outb.append(d)
        return outb
    m0 = moe(0, [r0[b]["blat"] for b in range(4)], [r0[b]["affT"] for b in range(4)], True)
    nc, _ = kb.build(cfg, "mix1")
    maps = []
    od_vec = np.concatenate([I["od_ba"][0], I["od_bx"][0], I["od_lam"][0]], 0)
    for b in range(4):
        m = common(b)
        m.update(h1=r0[b]["h1"], od_w_in=I["od_w_in"][0], od_w_out=I["od_w_out"][0], od_cw=I["od_conv_w"][0], od_cb=I["od_conv_b"],
                 od_wa=I["od_wa"][0], od_wx=I["od_wx"][0], od_vec=od_vec, router=I["moe_router"][1])
        m.update(m0[b])
        maps.append(m)
    r1 = _run(nc, maps, "mix1")
    m1 = moe(1, [r1[b]["blat1"] for b in range(4)], [r1[b]["affT1"] for b in range(4)], False)
    nc, _ = kb.build(cfg, "final")
    maps = []
    for b in range(4):
        m = common(b)
        m.update(h2=r1[b]["h2"], fng=I["final_norm_g"][None])
        m.update(m1[b])
        maps.append(m)
    r2 = _run(nc, maps, "final")
    dbg = dict(r0=r0, r1=r1)
    return np.stack([r2[b]["out"] for b in range(4)], 0), dbg


class _KBNS:
    pass


_KB = _KBNS()
_KB.build_mod = build_mod
_KB.build = build
_KB.build_moe = build_moe
_KB.half_cfg = half_cfg


def kernel(**inputs):
    I = {k: np.asarray(v) for k, v in inputs.items()}
    cfg = Cfg()
    out, _ = pipeline(None, cfg, I)
    return np.ascontiguousarray(out.astype(np.float32))
```

```python
import time
import concourse.bass_utils as _bu
import contextlib
import numpy as np
import concourse.bass as bass
import concourse.mybir as mybir

F32 = mybir.dt.float32
BF16 = mybir.dt.bfloat16
I32 = mybir.dt.int32
U32 = mybir.dt.uint32
ALU = mybir.AluOpType
AF = mybir.ActivationFunctionType
AX = mybir.AxisListType

ENGS = ("tensor", "vector", "scalar", "gpsimd", "sync")


class Buf:
    def __init__(self, P, name, ap_fn):
        self.P = P
        self.name = name
        self._ap = ap_fn
        self.last_w = None
        self.reads = []
        self.sem = None
        self.cnt = 0

    def __getitem__(self, idx):
        return self._ap()[idx]

    @property
    def ap(self):
        return self._ap()


class Prog:
    def __init__(self):
        self.nc = bass.Bass("TRN2", target_bir_lowering=False)
        self.es = contextlib.ExitStack()
        self.q = {e: [] for e in ENGS}
        self.esem = {}
        self.ecnt = {e: 0 for e in ENGS}
        for e in ENGS:
            self.esem[e] = self.es.enter_context(self.nc.semaphore("S_" + e))
        self.waited = {e: {} for e in ENGS}
        self.semobjs = {}
        self.nbuf = 0
        self.dma_bufs = []

    def sb(self, name, shape, dt, es=None):
        t = (es or self.es).enter_context(self.nc.sbuf_tensor(name, list(shape), dt))
        return Buf(self, name, lambda: t)

    def ps(self, name, shape, dt, es=None):
        t = (es or self.es).enter_context(self.nc.psum_tensor(name, list(shape), dt))
        return Buf(self, name, lambda: t)

    def dram(self, name, shape, dt, kind=None):
        if kind is None:
            t = self.nc.dram_tensor(name, list(shape), dt)
        else:
            t = self.nc.dram_tensor(name, list(shape), dt, kind=kind)
        return Buf(self, name, lambda: t.ap())

    def _bufsem(self, b):
        if b.sem is None:
            b.sem = self.es.enter_context(self.nc.semaphore("D_%d" % self.nbuf))
            self.nbuf += 1
            self.dma_bufs.append(b)
        return b.sem

    def _deps(self, eng, reads, writes):
        evs = []
        for b in reads:
            if b.last_w is not None:
                evs.append(b.last_w)
        for b in writes:
            if b.last_w is not None:
                evs.append(b.last_w)
            evs.extend(b.reads)
        need = {}
        for (sem, val) in evs:
            if eng == "tensor" and sem is self.esem["tensor"]:
                continue
            k = id(sem)
            if self.waited[eng].get(k, 0) >= val:
                continue
            if k not in need or need[k][1] < val:
                need[k] = (sem, val)
        for k, (sem, val) in need.items():
            self.waited[eng][k] = val
        return list(need.values())

    def _mark(self, ev, reads, writes):
        for b in reads:
            b.reads.append(ev)
            if len(b.reads) > 64:
                best = {}
                for (s, v) in b.reads:
                    if id(s) not in best or best[id(s)][1] < v:
                        best[id(s)] = (s, v)
                b.reads = list(best.values())
        for b in writes:
            b.last_w = ev
            b.reads = []

    def op(self, eng, fn, reads=(), writes=()):
        waits = self._deps(eng, reads, writes)
        self.ecnt[eng] += 1
        ev = (self.esem[eng], self.ecnt[eng])
        self.q[eng].append((waits, fn, ev[0], 1))
        self._mark(ev, reads, writes)
        return ev

    def dma(self, eng, fn, dst, reads=(), extra_writes=(), disjoint=False):
        sem = self._bufsem(dst)
        waits = self._deps(eng, reads, ([] if disjoint else [dst]) + list(extra_writes))
        dst.cnt += 16
        ev = (sem, dst.cnt)
        self.q[eng].append((waits, fn, sem, 16))
        self._mark(ev, reads, [dst] + list(extra_writes))
        return ev

    def cc(self, fn, dst, reads=()):
        sem = self._bufsem(dst)
        waits = self._deps("gpsimd", reads, [dst])
        dst.cnt += 1
        ev = (sem, dst.cnt)
        self.q["gpsimd"].append((waits, fn, sem, None))
        self._mark(ev, reads, [dst])
        return ev

    def barrier(self):
        evs = [(self.esem[e], self.ecnt[e]) for e in ENGS if self.ecnt[e] > 0]
        evs += [(b.sem, b.cnt) for b in self.dma_bufs if b.cnt > 0]
        for eng in ENGS:
            waits = []
            for (sem, val) in evs:
                if sem is self.esem[eng]:
                    continue
                k = id(sem)
                if self.waited[eng].get(k, 0) >= val:
                    continue
                self.waited[eng][k] = val
                waits.append((sem, val))
            if waits:
                self.q[eng].append((waits, None, None, 0))

    def wait_all(self, eng, bufs):
        waits = []
        for b in bufs:
            if b.last_w is not None:
                waits.append(b.last_w)
        self.q[eng].append((waits, None, None, 0))

    def build(self):
        nc = self.nc
        with nc.Block() as block:
            def mk(engname):
                items = self.q[engname]

                def body(e):
                    for (waits, fn, sem, inc) in items:
                        for (s, v) in waits:
                            e.wait_ge(s, v)
                        if fn is None:
                            continue
                        ins = fn(e)
                        if inc is None:
                            ins.then_inc(sem)
                        else:
                            ins.then_inc(sem, inc)
                return body
            block.tensor(mk("tensor"))
            block.vector(mk("vector"))
            block.scalar(mk("scalar"))
            block.gpsimd(mk("gpsimd"))
            block.sync(mk("sync"))
        self.es.close()
        return nc


import contextlib, math
import numpy as np

EPS = 1e-6


class Cfg:
    def __init__(self, D=2048, S=4096, C=256):
        self.D, self.S, self.C = D, S, C
        self.B, self.NE, self.L = 4, 16, 2
        self.DC = D // 128
        self.AW = D // 2; self.HQ = self.AW // 128; self.HKV = self.HQ // 4; self.KVW = self.HKV * 128
        self.SW = D // 2; self.SC = self.SW // 128
        self.EIN = self.AW + 2 * self.KVW + 3 * self.SW
        self.LW = 5 * D // 4; self.LC = self.LW // 128; self.NLB = self.LW // 256
        self.FF = D; self.FC = self.FF // 128
        self.SH, self.CH = S // 2, C // 2
        self.NK = S + C
        self.cap = 2 * S // 16; self.capc = 2 * C // 16
        self.D8 = D // 8
        self.NOWN = self.SH + self.CH


def half_cfg(c):
    import copy
    h = copy.copy(c)
    h.AW = c.AW // 2; h.HQ = c.HQ // 2; h.HKV = max(1, c.HKV // 2); h.KVW = h.HKV * 128
    h.SW = c.SW // 2; h.SC = c.SC // 2; h.EIN = h.AW + 2 * h.KVW + 3 * h.SW
    h.LW = c.LW // 2; h.LC = c.LC // 2; h.NLB = c.NLB // 2
    return h


def build_mod(cfg):
    c = cfg; D, DC = c.D, c.DC
    P = Prog()
    din = lambda n, s, dt=F32: P.dram(n, s, dt, kind="ExternalInput")
    c5 = din("c5", [5, D]); modw = din("modw", [2, D, 6 * c.D8]); modb = din("modb", [2, 6 * c.D8])
    ident_in = din("ident", [128, 128])
    mout = P.dram("mloc_out", [5, 2 * 6 * c.D8], F32, kind="ExternalOutput")
    PS = [P.ps("ps%d" % i, [128, 512], F32) for i in range(4)]
    ident = P.sb("identf", [128, 128], F32)
    P.dma("sync", lambda e, o=ident[:], i=ident_in[:]: e.dma_start(out=o, in_=i), ident)
    c5t = P.sb("c5t", [5, D], F32); cT = P.sb("cT", [128, DC, 5], F32)
    P.dma("sync", lambda e, o=c5t[:], i=c5[:]: e.dma_start(out=o, in_=i), c5t)
    P.op("scalar", lambda e, o=c5t[:], i=c5t[:]: e.activation(out=o, in_=i, func=AF.Silu), [c5t], [c5t])
    for k in range(DC):
        pt = PS[k % 2]
        P.op("tensor", lambda e, o=pt[:, 0:5], i=c5t[:, k * 128:(k + 1) * 128], idn=ident[0:5, 0:5]:
             e.transpose(out=o, in_=i, identity=idn), [c5t, ident], [pt])
        P.op("vector", lambda e, o=cT[:, k, :], i=pt[:, 0:5]: e.tensor_copy(out=o, in_=i), [pt], [cT])
    ncol = 6 * c.D8
    mloc = P.sb("mloc", [5, 2 * ncol], F32); mbt = P.sb("mbt", [5, 2 * ncol], F32)
    P.dma("sync", lambda e, o=mbt[:], i=modb.ap.rearrange("l n -> (l n)").partition_broadcast(5): e.dma_start(out=o, in_=i), mbt)
    wst = [P.sb("mw%d" % i, [128, DC, 256], F32) for i in range(2)]
    ci = 0
    for l in range(2):
        for n0 in range(0, ncol, 256):
            w = wst[ci % 2]; pm = PS[2 + ci % 2]; ci += 1
            P.dma("sync", lambda e, o=w[:], i=modw.ap[l, :, n0:n0 + 256].rearrange("(k p) n -> p k n", p=128): e.dma_start(out=o, in_=i), w)
            for k in range(DC):
                P.op("tensor", lambda e, o=pm[0:5, 0:256], a=cT[:, k, :], b=w[:, k, :], k=k:
                     e.matmul(o, lhsT=a, rhs=b, start=(k == 0), stop=(k == DC - 1)), [cT, w], [pm])
            P.op("vector", lambda e, o=mloc[:, l * ncol + n0:l * ncol + n0 + 256], a=pm[0:5, 0:256],
                 b=mbt[:, l * ncol + n0:l * ncol + n0 + 256]: e.tensor_tensor(out=o, in0=a, in1=b, op=ALU.add), [pm, mbt], [mloc])
    P.dma("sync", lambda e, o=mout.ap, i=mloc[:]: e.dma_start(out=o, in_=i), mout, [mloc])
    P.wait_all("sync", [mout])
    return P.build(), ["mloc_out"]


def build(cfg, which, layer=0, dbg=False, upto=99):
    c = cfg
    D, S, C, DC, SH, CH, NK = c.D, c.S, c.C, c.DC, c.SH, c.CH, c.NK
    P = Prog(); nc = P.nc
    din = lambda n, s, dt=F32: P.dram(n, s, dt, kind="ExternalInput")
    dout = lambda n, s, dt=F32: P.dram(n, s, dt, kind="ExternalOutput")
    outs = []
    modrows = din("modrows", [2, 2 * 6 * D])
    n1g = din("n1g", [2, D]); n2g = din("n2g", [2, D])
    ident_in = din("ident", [128, 128])
    PS = [P.ps("ps%d" % i, [128, 512], F32) for i in range(8)]
    rr = {}

    def psn(group, banks):
        i = rr.get(group, 0); rr[group] = i + 1
        return PS[banks[i % len(banks)]]

    ident = P.sb("identf", [128, 128], F32)
    P.dma("sync", lambda e, o=ident[:], i=ident_in[:]: e.dma_start(out=o, in_=i), ident)
    identb = P.sb("identb", [128, 128], BF16)
    P.op("vector", lambda e, o=identb[:], i=ident[:]: e.tensor_copy(out=o, in_=i), [ident], [identb])
    selt = P.sb("selt", [2, 256], F32)
    P.op("vector", lambda e, o=selt[:]: e.memset(o, 0.0), [], [selt])
    selin = din("selin", [2, 256])
    P.dma("sync", lambda e, o=selt[:], i=selin.ap: e.dma_start(out=o, in_=i), selt)
    bvn = [0]

    def bcast_vec(dst, l, j, which):
        tes = contextlib.ExitStack()
        bvn[0] += 1
        mv = P.sb("mv%d" % bvn[0], [2, D], F32, tes)
        P.dma("sync", lambda e, o=mv[:], i=modrows.ap[:, (l * 6 + j) * D:(l * 6 + j + 1) * D]: e.dma_start(out=o, in_=i), mv)
        if j in (1, 4):
            gv = P.sb("gv%d" % bvn[0], [2, D], F32, tes)
            gsrc = n1g if j == 1 else n2g
            P.dma("sync", lambda e, o=gv[:], i=gsrc.ap[l:l + 1, :].partition_broadcast(2): e.dma_start(out=o, in_=i), gv)
            P.op("vector", lambda e, o=mv[:], a=mv[:], b=gv[:]:
                 e.scalar_tensor_tensor(out=o, in0=a, scalar=1.0, in1=b, op0=ALU.add, op1=ALU.mult), [mv, gv], [mv])
        for n0 in range(0, D, 512):
            pb = psn("m", [2, 3])
            P.op("tensor", lambda e, o=pb[:, :], a=selt[:, which * 128:(which + 1) * 128], b=mv[:, n0:n0 + 512]:
                 e.matmul(o, lhsT=a, rhs=b, start=True, stop=True), [selt, mv], [pb])
            P.op("scalar", lambda e, o=dst[:, n0:n0 + 512], i=pb[:, :]: e.copy(out=o, in_=i), [pb], [dst])
        P.barrier(); tes.close()

    def norm_tile(xt, gs, sh, at, small):
        P.op("scalar", lambda e, o=at[:], i=xt[:], a=small[:, 0:1]: e.activation(out=o, in_=i, func=AF.Square, accum_out=a),
             [xt], [at, small])
        P.op("scalar", lambda e, o=small[:, 1:2], i=small[:, 0:1]: e.activation(out=o, in_=i, func=AF.Sqrt, scale=1.0 / D, bias=EPS),
             [small], [small])
        P.op("vector", lambda e, o=small[:, 2:3], i=small[:, 1:2]: e.reciprocal(out=o, in_=i), [small], [small])
        P.op("vector", lambda e, o=at[:], a=xt[:], s=small[:, 2:3], b=gs[:]:
             e.scalar_tensor_tensor(out=o, in0=a, scalar=s, in1=b, op0=ALU.mult, op1=ALU.mult), [xt, small, gs], [at])
        if sh is not None:
            P.op("gpsimd", lambda e, o=at[:], a=at[:], b=sh[:]: e.tensor_tensor(out=o, in0=a, in1=b, op=ALU.add), [at, sh], [at])

    def transpose_to(at, dstT, col0, ncols_part=128, dt_ident=None):
        for k4 in range(0, DC, 4):
            pt = psn("t", [0, 1])
            for kk in range(4):
                k = k4 + kk
                P.op("tensor", lambda e, o=pt[:, kk * 128:(kk + 1) * 128], i=at[:, k * 128:(k + 1) * 128], idn=ident[:]:
                     e.transpose(out=o, in_=i, identity=idn), [at, ident], [pt])
            P.op("scalar", lambda e, o=dstT[:, k4:k4 + 4, col0:col0 + 128], i=pt[:].rearrange("p (k t) -> p k t", k=4):
                 e.copy(out=o, in_=i), [pt], [dstT])


    def wload(dst, src_ap):
        import os
        if os.environ.get("NOWL"):
            P.op("vector", lambda e, o=dst[:]: e.memset(o, 0.01), [], [dst]); return
        P.dma("gpsimd", lambda e, o=dst[:], i=src_ap.rearrange("(k p) n -> p k n", p=128): e.dma_start(out=o, in_=i), dst)

    def pass_out(l, srcT, KC, w_all, hsrc, hrow0, nrows, cbound, hdst, bdst, affdst, dbgname=None):
        es = contextlib.ExitStack()
        Wo = P.sb("Wo", [128, KC, D], BF16, es)
        wload(Wo, w_all.ap)
        nw = 2 if cbound > 0 else 1
        g1 = [P.sb("g1_%d" % w, [128, D], F32, es) for w in range(nw)]
        gs2 = [P.sb("gs2_%d" % w, [128, D], F32, es) for w in range(nw)]
        sh2 = [P.sb("sh2_%d" % w, [128, D], F32, es) for w in range(nw)]
        for w in range(nw):
            bcast_vec(g1[w], l, 2, w); bcast_vec(gs2[w], l, 4, w); bcast_vec(sh2[w], l, 3, w)
        wr = P.sb("wr", [128, DC, 16], F32, es)
        P.dma("sync", lambda e, o=wr[:], i=router.ap.rearrange("(k p) n -> p k n", p=128): e.dma_start(out=o, in_=i), wr)
        NBF = 1 if D > 1024 else 2
        cts = [P.sb("ct%d" % i, [128, KC, 512], BF16, es) for i in range(NBF)]
        xts = [P.sb("oxt%d" % i, [128, D], F32, es) for i in range(NBF)]
        hts = [P.sb("oht%d" % i, [128, D], F32, es) for i in range(NBF)]
        bts = [P.sb("obt%d" % i, [128, D], F32, es) for i in range(NBF)]
        bbs = [P.sb("obb%d" % i, [128, D], BF16, es) for i in range(NBF)]
        bTs = [P.sb("obT%d" % i, [128, DC, 128], F32, es) for i in range(NBF)]
        sms = [P.sb("osm%d" % i, [128, 4], F32, es) for i in range(2)]
        exs = [P.sb("oex%d" % i, [128, 16], F32, es) for i in range(2)]
        affTt = P.sb("affTt", [16, nrows], F32, es)
        ti = 0; ni = 0
        for r0 in range(0, nrows, 512):
            nrow = min(512, nrows - r0)
            T = cts[ti % NBF]; ti += 1
            P.dma("sync", lambda e, o=T[:, :, 0:nrow], i=srcT.ap[:, :, r0:r0 + nrow]: e.dma_start(out=o, in_=i), T, [srcT])
            for sub in range(0, nrow, 128):
                rr0 = r0 + sub
                w = 1 if rr0 < cbound else 0
                xt, ht, bt, bb, bT, sm, ex = xts[ni % NBF], hts[ni % NBF], bts[ni % NBF], bbs[ni % NBF], bTs[ni % NBF], sms[ni % 2], exs[ni % 2]; ni += 1
                P.dma("sync", lambda e, o=xt[:], i=hsrc.ap[hrow0 + rr0:hrow0 + rr0 + 128, :]: e.dma_start(out=o, in_=i), xt, [hsrc])
                for n0 in range(0, D, 512):
                    po = psn("o", [2, 3, 4])
                    for k in range(KC):
                        P.op("tensor", lambda e, o=po[:, :], a=T[:, k, sub:sub + 128], b=Wo[:, k, n0:n0 + 512], k=k:
                             e.matmul(o, lhsT=a, rhs=b, start=(k == 0), stop=(k == KC - 1)), [T, Wo], [po])
                    P.op("vector", lambda e, o=ht[:, n0:n0 + 512], a=po[:, :], b=g1[w][:, n0:n0 + 512]:
                         e.tensor_tensor(out=o, in0=a, in1=b, op=ALU.mult), [po, g1[w]], [ht])
                P.op("gpsimd", lambda e, o=ht[:], a=ht[:], b=xt[:]: e.tensor_tensor(out=o, in0=a, in1=b, op=ALU.add), [ht, xt], [ht])
                P.dma("sync", lambda e, o=hdst.ap[rr0:rr0 + 128, :], i=ht[:]: e.dma_start(out=o, in_=i), hdst, [ht])
                norm_tile(ht, gs2[w], sh2[w], bt, sm)
                P.op("scalar", lambda e, o=bb[:], a=bt[:]: e.copy(out=o, in_=a), [bt], [bb])
                P.dma("sync", lambda e, o=bdst.ap[rr0:rr0 + 128, :], i=bb[:]: e.dma_start(out=o, in_=i), bdst, [bb])
                transpose_to(bt, bT, 0)
                pr = psn("r", [5, 6])
                for k in range(DC):
                    P.op("tensor", lambda e, o=pr[:, 0:16], a=bT[:, k, :], b=wr[:, k, :], k=k:
                         e.matmul(o, lhsT=a, rhs=b, start=(k == 0), stop=(k == DC - 1)), [bT, wr], [pr])
                P.op("vector", lambda e, o=sm[:, 0:1], a=pr[:, 0:16]: e.tensor_reduce(out=o, in_=a, axis=AX.X, op=ALU.max, negate=True), [pr], [sm])
                P.op("scalar", lambda e, o=ex[:], a=pr[:, 0:16], b=sm[:, 0:1], s=sm[:, 1:2]:
                     e.activation(out=o, in_=a, func=AF.Exp, bias=b, accum_out=s), [pr, sm], [ex, sm])
                P.op("vector", lambda e, o=sm[:, 2:3], a=sm[:, 1:2]: e.reciprocal(out=o, in_=a), [sm], [sm])
                P.op("vector", lambda e, o=ex[:], a=ex[:], s=sm[:, 2:3]: e.tensor_scalar(out=o, in0=a, scalar1=s, scalar2=None, op0=ALU.mult), [ex, sm], [ex])
                pt = psn("t", [0, 1])
                P.op("tensor", lambda e, o=pt[0:16, 0:128], a=ex[:], idn=ident[:]: e.transpose(out=o, in_=a, identity=idn), [ex, ident], [pt])
                P.op("scalar", lambda e, o=affTt[:, rr0:rr0 + 128], a=pt[0:16, 0:128]: e.copy(out=o, in_=a), [pt], [affTt])
        P.dma("sync", lambda e, o=affdst.ap, i=affTt[:]: e.dma_start(out=o, in_=i), affdst, [affTt])
        P.barrier(); es.close()


    def combine(l, hsrc, nrows, cbound, slots_in, ridx_in, slotsc_in, ridxc_in, acc, hdst, post=None):
        es = contextlib.ExitStack()
        nw = 2 if cbound > 0 else 1
        g2 = [P.sb("cg2_%d" % w, [128, D], F32, es) for w in range(nw)]
        for w in range(nw):
            bcast_vec(g2[w], l, 5, w)
        zt = P.sb("czt", [128, D], F32, es)
        P.op("vector", lambda e, o=zt[:]: e.memset(o, 0.0), [], [zt])
        for r0 in range(0, nrows, 128):
            P.dma("sync", lambda e, o=acc.ap[r0:r0 + 128, :], i=zt[:]: e.dma_start(out=o, in_=i), acc, [zt], disjoint=True)
        sts = [P.sb("cst%d" % i, [128, D], F32, es) for i in range(2)]
        its = [P.sb("cit%d" % i, [128, 1], I32, es) for i in range(2)]
        n = 0
        NT_ = c.cap // 128
        for e_ in range(16):
            tl = [(slots_in, ridx_in, j * 128, 128) for j in range(NT_)]
            if slotsc_in is not None:
                tl.append((slotsc_in, ridxc_in, 0, c.capc))
            for ti_, (sl, ri, j0, ns) in enumerate(tl):
                st = sts[n % 2]; it = its[n % 2]; n += 1
                P.dma("sync", lambda e, o=st[0:ns, :], i=sl.ap[e_, j0:j0 + ns, :]: e.dma_start(out=o, in_=i), st)
                P.dma("sync", lambda e, o=it[0:ns, :], i=ri.ap[e_:e_ + 1, j0:j0 + ns].rearrange("o n -> n o"): e.dma_start(out=o, in_=i), it)
                P.dma("gpsimd", lambda e, ix=it[0:ns, 0:1], i=st[0:ns, :]: e.indirect_dma_start(
                    out=acc.ap, out_offset=bass.IndirectOffsetOnAxis(ap=ix, axis=0), in_=i, in_offset=None, compute_op=ALU.add),
                    acc, [st, it])
        hts = [P.sb("cht%d" % i, [128, D], F32, es) for i in range(2)]
        ats_ = [P.sb("cat%d" % i, [128, D], F32, es) for i in range(2)]
        n = 0
        for r0 in range(0, nrows, 128):
            w = 1 if r0 < cbound else 0
            ht = hts[n % 2]; at = ats_[n % 2]; n += 1
            P.dma("sync", lambda e, o=ht[:], i=hsrc.ap[r0:r0 + 128, :]: e.dma_start(out=o, in_=i), ht, [hsrc])
            P.dma("sync", lambda e, o=at[:], i=acc.ap[r0:r0 + 128, :]: e.dma_start(out=o, in_=i), at, [acc])
            P.op("vector", lambda e, o=at[:], a=at[:], b=g2[w][:]: e.tensor_tensor(out=o, in0=a, in1=b, op=ALU.mult), [at, g2[w]], [at])
            P.op("gpsimd", lambda e, o=ht[:], a=ht[:], b=at[:]: e.tensor_tensor(out=o, in0=a, in1=b, op=ALU.add), [ht, at], [ht])
            if post is None:
                P.dma("sync", lambda e, o=hdst.ap[r0:r0 + 128, :], i=ht[:]: e.dma_start(out=o, in_=i), hdst, [ht], disjoint=True)
            else:
                post(ht, r0, at)
        P.barrier(); es.close()

    if which == "final":
        h2 = din("h2", [S, D]); slots_in = din("slots_b", [16, c.cap, D]); ridx_in = din("ridx_b", [16, c.cap], I32)
        fng = din("fng", [1, D])
        out = dout("out", [S, D]); outs.append("out")
        acc = P.dram("acc", [S, D], F32)
        fgb = P.sb("fgb", [128, D], F32)
        P.dma("sync", lambda e, o=fgb[:], i=fng.ap[0:1, :].partition_broadcast(128): e.dma_start(out=o, in_=i), fgb)
        smf = [P.sb("smf%d" % i, [128, 4], F32) for i in range(2)]
        cnt = [0]

        def post(ht, r0, scratch):
            sm = smf[cnt[0] % 2]; cnt[0] += 1
            norm_tile(ht, fgb, None, scratch, sm)
            P.dma("sync", lambda e, o=out.ap[r0:r0 + 128, :], i=scratch[:]: e.dma_start(out=o, in_=i), out, [scratch], disjoint=True)
        combine(1, h2, S, 0, slots_in, ridx_in, None, None, acc, None, post)
        P.wait_all("sync", [out])
        return P.build(), outs

    if which in ("mix1", "mix1a"):
        LW, LC, NLB = c.LW, c.LC, c.NLB
        h1 = din("h1", [NK, D]); slots_in = din("slots_b", [16, c.cap, D]); ridx_in = din("ridx_b", [16, c.cap], I32)
        slotsc_in = din("slotsc_b", [16, c.capc, D]); ridxc_in = din("ridxc_b", [16, c.capc], I32)
        od_w_in = din("od_w_in", [D, 2 * LW]); od_w_out = din("od_w_out", [LW, D])
        od_cw = din("od_cw", [4, LW]); od_cb = din("od_cb", [1, LW])
        od_wa = din("od_wa", [2, NLB, 256, 256]); od_wx = din("od_wx", [2, NLB, 256, 256])
        od_vec = din("od_vec", [6, LW])
        router = din("router", [D, 16])
        if which == "mix1":
            h2 = dout("h2", [S, D]); blat1 = dout("blat1", [S, D], BF16); affT1 = dout("affT1", [16, S]); outs += ["h2", "blat1", "affT1"]
            hl0 = P.dram("hl0", [NK, D], F32)
        else:
            hl0 = dout("hl0", [NK, D]); outs += ["hl0"]
        acc = P.dram("acc", [NK, D], F32)
        aT1 = P.dram("aT1", [128, DC, NK], BF16)
        xT_d = P.dram("xT_d", [LC, 128, NK], F32); gT_d = P.dram("gT_d", [LC, 128, S], BF16)
        if which == "mix1":
            ygT = P.dram("ygT", [128, LC, S], BF16)
        else:
            ygT = dout("ygT", [128, LC, S], BF16); outs += ["ygT"]
        combine(0, h1, NK, C, slots_in, ridx_in, slotsc_in, ridxc_in, acc, hl0)
        es = contextlib.ExitStack()
        gs1 = [P.sb("gs1_%d" % w, [128, D], F32, es) for w in range(2)]
        sh1 = [P.sb("sh1_%d" % w, [128, D], F32, es) for w in range(2)]
        for w in range(2):
            bcast_vec(gs1[w], 1, 1, w); bcast_vec(sh1[w], 1, 0, w)
        xts = [P.sb("xt%d" % i, [128, D], F32, es) for i in range(2)]
        ats = [P.sb("at%d" % i, [128, D], F32, es) for i in range(2)]
        smalls = [P.sb("sm%d" % i, [128, 4], F32, es) for i in range(2)]
        aTt = [P.sb("aTt%d" % i, [128, DC, 512], BF16, es) for i in range(2)]
        nt = 0; ti = 0
        for r0 in range(0, NK, 512):
            nrow = min(512, NK - r0)
            T = aTt[ti % 2]; ti += 1
            for sub in range(0, nrow, 128):
                xt = xts[nt % 2]; at = ats[nt % 2]; sm = smalls[nt % 2]; nt += 1
                rr0 = r0 + sub
                P.dma("sync", lambda e, o=xt[:], i=hl0.ap[rr0:rr0 + 128, :]: e.dma_start(out=o, in_=i), xt, [hl0])
                w = 1 if rr0 < C else 0
                norm_tile(xt, gs1[w], sh1[w], at, sm)
                transpose_to(at, T, sub)
            P.dma("sync", lambda e, o=aT1.ap[:, :, r0:r0 + nrow], i=T[:, :, 0:nrow]: e.dma_start(out=o, in_=i), aT1, [T], disjoint=True)
        P.barrier(); es.close()
        es = contextlib.ExitStack()
        Wx_ = [P.sb("pWx%d" % i, [128, DC, 256], BF16, es) for i in range(2)]
        Wg_ = [P.sb("pWg%d" % i, [128, DC, 256], BF16, es) for i in range(2)]
        aTt = [P.sb("aTp%d" % i, [128, DC, 512], BF16, es) for i in range(2)]
        xfull = [P.sb("xfull%d" % i, [128, NK], F32, es) for i in range(2)]
        gfull = [P.sb("gfull%d" % i, [128, NK], BF16, es) for i in range(2)]
        ti = 0
        for cc in range(LC // 2):
            Wx = Wx_[cc % 2]; Wg = Wg_[cc % 2]
            wload(Wx, od_w_in.ap[:, cc * 256:(cc + 1) * 256]); wload(Wg, od_w_in.ap[:, LW + cc * 256:LW + (cc + 1) * 256])
            for r0 in range(0, NK, 512):
                nrow = min(512, NK - r0)
                T = aTt[ti % 2]; ti += 1
                P.dma("sync", lambda e, o=T[:, :, 0:nrow], i=aT1.ap[:, :, r0:r0 + nrow]: e.dma_start(out=o, in_=i), T, [aT1])
                for half in range(2):
                    px = psn("x", [2, 3]); pg = psn("g", [4, 5])
                    for k in range(DC):
                        P.op("tensor", lambda e, o=px[:, 0:nrow], a=Wx[:, k, half * 128:(half + 1) * 128], b=T[:, k, 0:nrow], k=k:
                             e.matmul(o, lhsT=a, rhs=b, start=(k == 0), stop=(k == DC - 1)), [Wx, T], [px])
                    for k in range(DC):
                        P.op("tensor", lambda e, o=pg[:, 0:nrow], a=Wg[:, k, half * 128:(half + 1) * 128], b=T[:, k, 0:nrow], k=k:
                             e.matmul(o, lhsT=a, rhs=b, start=(k == 0), stop=(k == DC - 1)), [Wg, T], [pg])
                    P.op("vector", lambda e, o=xfull[half][:, r0:r0 + nrow], a=px[:, 0:nrow]: e.tensor_copy(out=o, in_=a), [px], [xfull[half]])
                    P.op("scalar", lambda e, o=gfull[half][:, r0:r0 + nrow], a=pg[:, 0:nrow]: e.activation(out=o, in_=a, func=AF.Gelu_apprx_tanh),
                         [pg], [gfull[half]])
            for half in range(2):
                ch = cc * 2 + half
                P.dma("sync", lambda e, o=xT_d.ap[ch], i=xfull[half][:]: e.dma_start(out=o, in_=i), xT_d, [xfull[half]], disjoint=True)
                P.dma("sync", lambda e, o=gT_d.ap[ch], i=gfull[half][:, C:NK]: e.dma_start(out=o, in_=i), gT_d, [gfull[half]], disjoint=True)
        P.barrier(); es.close()
        es = contextlib.ExitStack()
        vr = P.sb("vr", [11, LW], F32, es); colv = P.sb("colv", [128, LC, 11], F32, es)
        P.dma("sync", lambda e, o=vr[0:4, :], i=od_cw.ap: e.dma_start(out=o, in_=i), vr)
        P.dma("sync", lambda e, o=vr[4:5, :], i=od_cb.ap: e.dma_start(out=o, in_=i), vr)
        P.dma("sync", lambda e, o=vr[5:11, :], i=od_vec.ap: e.dma_start(out=o, in_=i), vr)
        for k in range(LC):
            pt = psn("t", [0, 1])
            P.op("tensor", lambda e, o=pt[:, 0:11], a=vr[:, k * 128:(k + 1) * 128], idn=ident[0:11, 0:11]:
                 e.transpose(out=o, in_=a, identity=idn), [vr, ident], [pt])
            P.op("vector", lambda e, o=colv[:, k, :], a=pt[:, 0:11]: e.tensor_copy(out=o, in_=a), [pt], [colv])
        sc8 = P.sb("sc8", [128, LC, 2], F32, es)
        P.op("scalar", lambda e, o=sc8[:], a=colv[:, :, 9:11]: e.activation(out=o, in_=a, func=AF.Exp, scale=-1.0), [colv], [sc8])
        P.op("scalar", lambda e, o=sc8[:], a=sc8[:]: e.activation(out=o, in_=a, func=AF.Ln, bias=1.0), [sc8], [sc8])
        P.op("vector", lambda e, o=sc8[:], a=sc8[:]: e.tensor_scalar(out=o, in0=a, scalar1=-8.0, scalar2=None, op0=ALU.mult), [sc8], [sc8])
        xb = P.sb("xb", [128, NK], F32, es)
        ub = [P.sb("ub%d" % i, [128, NK], F32, es) for i in range(2)]
        ubf = P.sb("ubf", [128, 2, NK], BF16, es)
        afull = P.sb("afull", [128, NK], F32, es); bfull = P.sb("bfull", [128, NK], F32, es); ysc = P.sb("ysc", [128, NK], F32, es)
        ysum = P.sb("ysum", [128, S], F32, es); gt = P.sb("gt", [128, S], BF16, es); ygb = P.sb("ygb", [128, S], BF16, es)
        Wa_t = [P.sb("Wa%d" % i, [128, 2, 256], BF16, es) for i in range(2)]
        Wx_t = [P.sb("Wxx%d" % i, [128, 2, 256], BF16, es) for i in range(2)]
        rt_ = [P.sb("rt_%d" % i, [128, 512], F32, es) for i in range(2)]
        it_ = [P.sb("it_%d" % i, [128, 512], F32, es) for i in range(2)]
        t2_ = [P.sb("t2_%d" % i, [128, 512], F32, es) for i in range(2)]
        wi = 0; ri_ = 0
        SCH = 1024

        def scan_cols(dst, cols_fwd, init_ap, init_buf):
            prev = init_ap; pb = init_buf
            for (a0, n, rev) in cols_fwd:
                if rev:
                    sl = slice(a0 + n - 1, (a0 - 1) if a0 > 0 else None, -1)
                    last = slice(a0, a0 + 1)
                else:
                    sl = slice(a0, a0 + n); last = slice(a0 + n - 1, a0 + n)
                rd = [afull, bfull] + ([pb] if pb is not None else [])
                P.op("vector", lambda e, o=dst[:, sl], d0=afull[:, sl], d1=bfull[:, sl], ini=prev:
                     e.tensor_tensor_scan(out=o, data0=d0, data1=d1, initial=ini, op0=ALU.mult, op1=ALU.add), rd, [dst])
                prev = dst[:, last]; pb = dst
        for hb in range(NLB):
            for jc in range(2):
                ch = 2 * hb + jc
                P.dma("sync", lambda e, o=xb[:], i=xT_d.ap[ch]: e.dma_start(out=o, in_=i), xb, [xT_d])
                u = ub[jc]
                P.op("vector", lambda e, o=u[:], a=xb[:], s1=colv[:, ch, 1:2], s2=colv[:, ch, 4:5]:
                     e.tensor_scalar(out=o, in0=a, scalar1=s1, scalar2=s2, op0=ALU.mult, op1=ALU.add), [xb, colv], [u])
                for (a0, a1) in ((0, C), (C, NK)):
                    for (tap, do, di, ln) in ((0, a0 + 1, a0, a1 - a0 - 1), (2, a0, a0 + 1, a1 - a0 - 1), (3, a0, a0 + 2, a1 - a0 - 2)):
                        P.op("vector", lambda e, o=u[:, do:do + ln], a=xb[:, di:di + ln], s=colv[:, ch, tap:tap + 1], b=u[:, do:do + ln]:
                             e.scalar_tensor_tensor(out=o, in0=a, scalar=s, in1=b, op0=ALU.mult, op1=ALU.add), [xb, colv, u], [u])
                P.op("gpsimd", lambda e, o=ubf[:, jc, :], a=u[:]: e.tensor_copy(out=o, in_=a), [u], [ubf])
            for jc in range(2):
                ch = 2 * hb + jc
                for d in range(2):
                    Wa = Wa_t[wi % 2]; Wx = Wx_t[wi % 2]; wi += 1
                    wload(Wa, od_wa.ap[d, hb]); wload(Wx, od_wx.ap[d, hb])
                    for r0 in range(0, NK, 512):
                        nrow = min(512, NK - r0)
                        pa = psn("x", [2, 3]); px = psn("g", [4, 5])
                        for ic in range(2):
                            P.op("tensor", lambda e, o=pa[:, 0:nrow], a=Wa[:, ic, jc * 128:(jc + 1) * 128], b=ubf[:, ic, r0:r0 + nrow], ic=ic:
                                 e.matmul(o, lhsT=a, rhs=b, start=(ic == 0), stop=(ic == 1)), [Wa, ubf], [pa])
                        for ic in range(2):
                            P.op("tensor", lambda e, o=px[:, 0:nrow], a=Wx[:, ic, jc * 128:(jc + 1) * 128], b=ubf[:, ic, r0:r0 + nrow], ic=ic:
                                 e.matmul(o, lhsT=a, rhs=b, start=(ic == 0), stop=(ic == 1)), [Wx, ubf], [px])
                        rt = rt_[ri_ % 2]; itt = it_[ri_ % 2]; t2 = t2_[ri_ % 2]; ri_ += 1
                        P.op("scalar", lambda e, o=rt[:, 0:nrow], a=pa[:, 0:nrow], b=colv[:, ch, 5 + d:6 + d]:
                             e.activation(out=o, in_=a, func=AF.Sigmoid, bias=b), [pa, colv], [rt])
                        P.op("scalar", lambda e, o=afull[:, r0:r0 + nrow], a=rt[:, 0:nrow], s=sc8[:, ch, d:d + 1]:
                             e.activation(out=o, in_=a, func=AF.Exp, scale=s), [rt, sc8], [afull])
                        P.op("scalar", lambda e, o=itt[:, 0:nrow], a=px[:, 0:nrow], b=colv[:, ch, 7 + d:8 + d]:
                             e.activation(out=o, in_=a, func=AF.Sigmoid, bias=b), [px, colv], [itt])
                        P.op("gpsimd", lambda e, o=t2[:, 0:nrow], a=afull[:, r0:r0 + nrow]: e.tensor_tensor(out=o, in0=a, in1=a, op=ALU.mult), [afull], [t2])
                        P.op("scalar", lambda e, o=t2[:, 0:nrow], a=t2[:, 0:nrow]: e.activation(out=o, in_=a, func=AF.Sqrt, scale=-1.0, bias=1.0), [t2], [t2])
                        P.op("gpsimd", lambda e, o=t2[:, 0:nrow], a=t2[:, 0:nrow], b=itt[:, 0:nrow]: e.tensor_tensor(out=o, in0=a, in1=b, op=ALU.mult), [t2, itt], [t2])
                        P.op("vector", lambda e, o=bfull[:, r0:r0 + nrow], a=t2[:, 0:nrow], b=ub[jc][:, r0:r0 + nrow]:
                             e.tensor_tensor(out=o, in0=a, in1=b, op=ALU.mult), [t2, ub[jc]], [bfull])
                    if d == 0:
                        pieces = [(a0, min(SCH, NK - a0), False) for a0 in range(0, NK, SCH)]
                        scan_cols(ysc, pieces, 0.0, None)
                        P.op("gpsimd", lambda e, o=ysum[:], a=ysc[:, C:NK]: e.tensor_copy(out=o, in_=a), [ysc], [ysum])
                    else:
                        pieces = [(0, C, True)] + [(a0, min(SCH, NK - a0), True) for a0 in range(C + ((NK - C - 1) // SCH) * SCH, C - 1, -SCH)]
                        scan_cols(ysc, pieces, 0.0, None)
                        P.op("gpsimd", lambda e, o=ysum[:], a=ysum[:], b=ysc[:, C:NK]: e.tensor_tensor(out=o, in0=a, in1=b, op=ALU.add), [ysum, ysc], [ysum])
                P.dma("sync", lambda e, o=gt[:], i=gT_d.ap[ch]: e.dma_start(out=o, in_=i), gt, [gT_d])
                P.op("vector", lambda e, o=ygb[:], a=ysum[:], b=gt[:]: e.tensor_tensor(out=o, in0=a, in1=b, op=ALU.mult), [ysum, gt], [ygb])
                P.dma("sync", lambda e, o=ygT.ap[:, ch, :], i=ygb[:]: e.dma_start(out=o, in_=i), ygT, [ygb], disjoint=True)
        P.barrier(); es.close()
        if which == "mix1a":
            P.wait_all("sync", [hl0, ygT])
            return P.build(), outs
        pass_out(1, ygT, LC, od_w_out, hl0, C, S, 0, h2, blat1, affT1)
        P.wait_all("sync", [h2, blat1, affT1])
        return P.build(), outs

    if which == "out0":
        NOWN = c.CH + c.SH
        catT_in = din("catT_in", [128, DC, NOWN], BF16); xin = din("xin_own", [NOWN, D])
        w_out_all = din("ev_w_out", [D, D]); router = din("router", [D, 16])
        h1 = dout("h1", [NOWN, D]); blat = dout("blat", [NOWN, D], BF16); affT = dout("affT", [16, NOWN])
        outs += ["h1", "blat", "affT"]
        pass_out(0, catT_in, DC, w_out_all, xin, 0, NOWN, c.CH, h1, blat, affT)
        P.wait_all("sync", [h1, blat, affT])
        return P.build(), outs

    if which == "out1":
        ygT_in = din("ygT_in", [128, c.LC, c.SH], BF16); hl0o = din("hl0_own", [c.SH, D])
        od_w_out = din("od_w_out", [c.LW, D]); router = din("router", [D, 16])
        h2 = dout("h2", [c.SH, D]); blat1 = dout("blat1", [c.SH, D], BF16); affT1 = dout("affT1", [16, c.SH]); outs += ["h2", "blat1", "affT1"]
        pass_out(1, ygT_in, c.LC, od_w_out, hl0o, 0, c.SH, 0, h2, blat1, affT1)
        P.wait_all("sync", [h2, blat1, affT1])
        return P.build(), outs

    if which in ("mix0", "mix0a"):
        xin = din("xin", [NK, D])
        ropet = din("ropet", [NK, 128])
        w_in_all = din("ev_w_in", [D, c.EIN]); w_out_all = din("ev_w_out", [D, D])
        evq = din("evq", [1, 128]); evk = din("evk", [1, 128])
        ev_cw = din("ev_cw", [3, c.SW]); ev_cb = din("ev_cb", [1, c.SW])
        router = din("router", [D, 16])
        aT0 = P.dram("aT0", [128, DC, NK], BF16)
        if which == "mix0a":
            catT = dout("catT", [128, c.HQ + c.SC, NK], BF16); outs += ["catT"]
        else:
            catT = P.dram("catT", [128, DC, NK], BF16)
            h1 = dout("h1", [NK, D]); blat = dout("blat", [NK, D], BF16); affT = dout("affT", [16, NK])
            outs += ["h1", "blat", "affT"]
        def wload(dst, src_ap):
            P.dma("gpsimd", lambda e, o=dst[:], i=src_ap.rearrange("(k p) n -> p k n", p=128): e.dma_start(out=o, in_=i), dst)

        def bc4(ap2, H):
            return bass.AP(ap2.tensor, ap2.offset, [list(ap2.ap[0]), [0, H], list(ap2.ap[1])])

        es = contextlib.ExitStack()
        gs1 = [P.sb("gs1_%d" % w, [128, D], F32, es) for w in range(2)]
        sh1 = [P.sb("sh1_%d" % w, [128, D], F32, es) for w in range(2)]
        for w in range(2):
            bcast_vec(gs1[w], 0, 1, w); bcast_vec(sh1[w], 0, 0, w)
        xts = [P.sb("xt%d" % i, [128, D], F32, es) for i in range(2)]
        ats = [P.sb("at%d" % i, [128, D], F32, es) for i in range(2)]
        smalls = [P.sb("sm%d" % i, [128, 4], F32, es) for i in range(2)]
        aTt = [P.sb("aTt%d" % i, [128, DC, 512], BF16, es) for i in range(2)]

        def pass_a(src, gsl, shl, dstT, nrows, cbound):
            nt = 0; ti = 0
            for r0 in range(0, nrows, 512):
                nrow = min(512, nrows - r0)
                T = aTt[ti % 2]; ti += 1
                for sub in range(0, nrow, 128):
                    xt = xts[nt % 2]; at = ats[nt % 2]; sm = smalls[nt % 2]; nt += 1
                    rr0 = r0 + sub
                    P.dma("sync", lambda e, o=xt[:], i=src.ap[rr0:rr0 + 128, :]: e.dma_start(out=o, in_=i), xt)
                    w = 1 if rr0 < cbound else 0
                    norm_tile(xt, gsl[w], shl[w], at, sm)
                    transpose_to(at, T, sub)
                P.dma("sync", lambda e, o=dstT.ap[:, :, r0:r0 + nrow], i=T[:, :, 0:nrow]: e.dma_start(out=o, in_=i), dstT, [T])
        if upto <= -3:
            d0 = dout("d0", [128, D]); outs.append("d0")
            P.dma("sync", lambda e, o=d0.ap, i=gs1[0][:]: e.dma_start(out=o, in_=i), d0, [gs1[0]])
            P.wait_all("sync", [d0]); es.close(); return P.build(), outs
        pass_a(xin, gs1, sh1, aT0, NK, C)
        P.barrier(); es.close()
        if upto <= -2:
            d0 = dout("d0", [128, DC, NK], BF16); outs.append("d0")
            P.dma("gpsimd", lambda e, o=d0.ap, i=aT0.ap: e.dma_start(out=o, in_=i), d0, [aT0])
            P.wait_all("sync", [d0]); return P.build(), outs

        es = contextlib.ExitStack()
        HQ, HKV, AW, KVW = c.HQ, c.HKV, c.AW, c.KVW
        NKT = NK // 128
        kT = P.sb("kT", [128, HKV, NK], BF16, es)
        qT = P.sb("qT", [128, HQ, NK], BF16, es)
        Vaug = P.sb("Vaug", [128, NKT, HKV, 132], BF16, es)
        import os
        if not os.environ.get("SKV"):
            onesf = P.sb("onesf", [128, 132], F32, es)
            P.op("vector", lambda e, o=onesf[:]: e.memset(o, 1.0), [], [onesf])
            oa = onesf[:]
            P.op("vector", lambda e, o=Vaug[:].rearrange("p a b c -> p (a b) c"),
                 i=bass.AP(oa.tensor, oa.offset, [list(oa.ap[0]), [0, NKT * HKV], [1, 132]]): e.tensor_copy(out=o, in_=i), [onesf], [Vaug])
        gkb = P.sb("gkb", [128, 128], F32, es); gqb = P.sb("gqb", [128, 128], F32, es)
        if not os.environ.get("SKG"):
            P.dma("sync", lambda e, o=gkb[:], i=evk.ap[0:1, :].partition_broadcast(128): e.dma_start(out=o, in_=i), gkb)
            P.dma("sync", lambda e, o=gqb[:], i=evq.ap[0:1, :].partition_broadcast(128): e.dma_start(out=o, in_=i), gqb)
        es2 = contextlib.ExitStack()
        Wkv = P.sb("Wkv", [128, DC, 2 * KVW], BF16, es2); Wq = P.sb("Wq", [128, DC, AW], BF16, es2)
        wload(Wkv, w_in_all.ap[:, AW:AW + 2 * KVW]); wload(Wq, w_in_all.ap[:, 0:AW])
        NBQ = 1 if D > 1024 else 2
        aTt = [P.sb("aTq%d" % i, [128, DC, 512], BF16, es2) for i in range(NBQ)]
        kf = [P.sb("kf%d" % i, [128, 4 * 128], F32, es2) for i in range(2)]
        sq = [P.sb("sq%d" % i, [128, 4 * 128], F32, es2) for i in range(2)]
        kr = [P.sb("kr%d" % i, [128, 4 * 128], F32, es2) for i in range(2)]
        hs = [P.sb("hs%d" % i, [128, 8], F32, es2) for i in range(2)]
        rts = [P.sb("rt%d" % i, [128, 128], F32, es2) for i in range(2)]
        hn = [0]

        import os
        SKF = os.environ.get("SKF", "")
        _realop = P.op
        def headnorm_rope(ps_ap, H, gb, rt, dstT, h0, tok0):
            i = hn[0] % 2; hn[0] += 1
            cnt = [0]
            class PX:
                def op(self, eng, fn, r=(), w=()):
                    cnt[0] += 1
                    if ("%02d" % cnt[0]) in SKF.split(","):
                        return
                    return _realop(eng, fn, r, w)
            P_ = PX()
            f, s_, r_, h_ = kf[i], sq[i], kr[i], hs[i]
            n = H * 128
            P_.op("vector", lambda e, o=f[:, 0:n], a=ps_ap: e.tensor_copy(out=o, in_=a), [ps_ap_buf[0]], [f])
            P_.op("vector", lambda e, o=s_[:, 0:n], a=f[:, 0:n]: e.tensor_tensor(out=o, in0=a, in1=a, op=ALU.mult), [f], [s_])
            P_.op("vector", lambda e, o=h_[:, 0:H], a=s_[:, 0:n].rearrange("p (h d) -> p h d", h=H):
                 e.tensor_reduce(out=o, in_=a, axis=AX.X, op=ALU.add), [s_], [h_])
            P_.op("scalar", lambda e, o=h_[:, 0:H], a=h_[:, 0:H]: e.activation(out=o, in_=a, func=AF.Sqrt, scale=1.0 / 128, bias=EPS), [h_], [h_])
            P_.op("vector", lambda e, o=h_[:, 0:H], a=h_[:, 0:H]: e.reciprocal(out=o, in_=a), [h_], [h_])
            f3 = f[:, 0:n].rearrange("p (h d) -> p h d", h=H)
            hb_ = h_[:, 0:H]
            rb = bass.AP(hb_.tensor, hb_.offset, [list(hb_.ap[0]), list(hb_.ap[1]), [0, 128]])
            P_.op("vector", lambda e, o=f3, a=f3, b=rb: e.tensor_tensor(out=o, in0=a, in1=b, op=ALU.mult), [f, h_], [f])
            P_.op("vector", lambda e, o=f3, a=f3, b=bc4(gb[:], H): e.tensor_tensor(out=o, in0=a, in1=b, op=ALU.mult), [f, gb], [f])
            f5 = f[:, 0:n].rearrange("p (g t j) -> p g t j", t=2, j=32)
            r5 = r_[:, 0:n].rearrange("p (g t j) -> p g t j", t=2, j=32)
            s5 = s_[:, 0:n].rearrange("p (g t j) -> p g t j", t=2, j=32)
            u1, u2 = f5[:, :, 0, :], f5[:, :, 1, :]
            cs = rt[:, 0:64]; sn = rt[:, 64:128]
            def tb(a2):
                return bass.AP(a2.tensor, a2.offset, [list(a2.ap[0]), [0, H], [32, 2], [1, 32]])
            u1v = bass.AP(u1.tensor, u1.offset, [list(u1.ap[0]), [128, H], [64, 2], [1, 32]])
            u2v = bass.AP(u2.tensor, u2.offset, [list(u2.ap[0]), [128, H], [64, 2], [1, 32]])
            o1 = r5[:, :, 0, :]; o2 = r5[:, :, 1, :]
            o1v = bass.AP(o1.tensor, o1.offset, [list(o1.ap[0]), [128, H], [64, 2], [1, 32]])
            o2v = bass.AP(o2.tensor, o2.offset, [list(o2.ap[0]), [128, H], [64, 2], [1, 32]])
            t1 = s5[:, :, 0, :]; t2 = s5[:, :, 1, :]
            t1v = bass.AP(t1.tensor, t1.offset, [list(t1.ap[0]), [128, H], [64, 2], [1, 32]])
            t2v = bass.AP(t2.tensor, t2.offset, [list(t2.ap[0]), [128, H], [64, 2], [1, 32]])
            P_.op("vector", lambda e, o=o1v, a=u1v, b=tb(cs): e.tensor_tensor(out=o, in0=a, in1=b, op=ALU.mult), [f, rt], [r_])
            P_.op("vector", lambda e, o=t1v, a=u2v, b=tb(sn): e.tensor_tensor(out=o, in0=a, in1=b, op=ALU.mult), [f, rt], [s_])
            P_.op("vector", lambda e, o=o1v, a=o1v, b=t1v: e.tensor_tensor(out=o, in0=a, in1=b, op=ALU.subtract), [r_, s_], [r_])
            P_.op("vector", lambda e, o=o2v, a=u1v, b=tb(sn): e.tensor_tensor(out=o, in0=a, in1=b, op=ALU.mult), [f, rt], [r_])
            P_.op("vector", lambda e, o=t2v, a=u2v, b=tb(cs): e.tensor_tensor(out=o, in0=a, in1=b, op=ALU.mult), [f, rt], [s_])
            P_.op("vector", lambda e, o=o2v, a=o2v, b=t2v: e.tensor_tensor(out=o, in0=a, in1=b, op=ALU.add), [r_, s_], [r_])
            pt = psn("t", [0, 1])
            for h in range(H):
                P_.op("tensor", lambda e, o=pt[:, h * 128:(h + 1) * 128], a=r_[:, h * 128:(h + 1) * 128], idn=ident[:]:
                     e.transpose(out=o, in_=a, identity=idn), [r_, ident], [pt])
            P_.op("scalar", lambda e, o=dstT[:, h0:h0 + H, tok0:tok0 + 128], a=pt[:, 0:n].rearrange("p (h t) -> p h t", h=H):
                 e.copy(out=o, in_=a), [pt], [dstT])

        ps_ap_buf = [None]
        ti = 0; ri = 0
        for r0 in range(0, NK, 512):
            nrow = min(512, NK - r0)
            T = aTt[ti % NBQ]; ti += 1
            P.dma("sync", lambda e, o=T[:, :, 0:nrow], i=aT0.ap[:, :, r0:r0 + nrow]: e.dma_start(out=o, in_=i), T, [aT0])
            for sub in range(0, nrow, 128):
                tok0 = r0 + sub; kt = tok0 // 128
                rt = rts[ri % 2]; ri += 1
                P.dma("sync", lambda e, o=rt[:], i=ropet.ap[tok0:tok0 + 128, :]: e.dma_start(out=o, in_=i), rt)
                pk = psn("p", [2, 3])
                for k in range(DC):
                    P.op("tensor", lambda e, o=pk[:, 0:2 * KVW], a=T[:, k, sub:sub + 128], b=Wkv[:, k, :], k=k:
                         e.matmul(o, lhsT=a, rhs=b, start=(k == 0), stop=(k == DC - 1)), [T, Wkv], [pk])
                for hh in range(HKV):
                    P.op("vector", lambda e, o=Vaug[:, kt, hh, 0:128], a=pk[:, KVW + hh * 128:KVW + (hh + 1) * 128]:
                         e.tensor_copy(out=o, in_=a), [pk], [Vaug])
                ps_ap_buf[0] = pk
                headnorm_rope(pk[:, 0:KVW], HKV, gkb, rt, kT, 0, tok0)
                for q0 in range(0, AW, 512):
                    nq = min(512, AW - q0); Hh = nq // 128
                    pq = psn("p", [2, 3])
                    for k in range(DC):
                        P.op("tensor", lambda e, o=pq[:, 0:nq], a=T[:, k, sub:sub + 128], b=Wq[:, k, q0:q0 + nq], k=k:
                             e.matmul(o, lhsT=a, rhs=b, start=(k == 0), stop=(k == DC - 1)), [T, Wq], [pq])
                    ps_ap_buf[0] = pq
                    headnorm_rope(pq[:, 0:nq], Hh, gqb, rt, qT, q0 // 128, tok0)
        P.barrier(); es2.close()
        if upto <= -1:
            d0 = dout("d0", [128, HQ, NK], BF16); outs.append("d0")
            P.dma("sync", lambda e, o=d0.ap, i=qT[:]: e.dma_start(out=o, in_=i), d0, [qT])
            P.wait_all("sync", [d0]); es.close(); return P.build(), outs

        es2 = contextlib.ExitStack()
        ptb = [P.sb("ptb%d" % i, [128, 512], BF16, es2) for i in range(3)]
        catg = [P.sb("catg%d" % i, [128, HQ, 512], BF16, es2) for i in range(2)]
        ofs = [P.sb("of%d" % i, [128, 128], F32, es2) for i in range(2)]
        rsm = [P.sb("rsm%d" % i, [128, 1], F32, es2) for i in range(2)]
        groups = [(0, C, C)] + [(C + g * 512, 512, NK) for g in range(S // 512)]
        sc = 128 ** -0.5
        pi = 0; oi = 0
        for gi, (q0, nq, nkeys) in enumerate(groups):
            cg = catg[gi % 2]
            nsub = nq // 128
            for hq in range(HQ):
                hk = hq // (HQ // HKV)
                nkt = nkeys // 128
                for kt in range(nkt):
                    sp = psn("s", [2, 3])
                    P.op("tensor", lambda e, o=sp[:, 0:nq], a=kT[:, hk, kt * 128:(kt + 1) * 128], b=qT[:, hq, q0:q0 + nq]:
                         e.matmul(o, lhsT=a, rhs=b, start=True, stop=True), [kT, qT], [sp])
                    pb = ptb[pi % 3]; pi += 1
                    P.op("scalar", lambda e, o=pb[:, 0:nq], a=sp[:, 0:nq]: e.activation(out=o, in_=a, func=AF.Exp, scale=sc), [sp], [pb])
                    for qs in range(nsub):
                        P.op("tensor", lambda e, o=PS[4 + qs][:, 0:129], a=pb[:, qs * 128:(qs + 1) * 128], b=Vaug[:, kt, hk, 0:129], kt=kt:
                             e.matmul(o, lhsT=a, rhs=b, start=(kt == 0), stop=(kt == nkt - 1)), [pb, Vaug], [PS[4 + qs]])
                pt = psn("t", [0, 1])
                for qs in range(nsub):
                    of = ofs[oi % 2]; rs = rsm[oi % 2]; oi += 1
                    P.op("vector", lambda e, o=rs[:], a=PS[4 + qs][:, 128:129]: e.reciprocal(out=o, in_=a), [PS[4 + qs]], [rs])
                    P.op("vector", lambda e, o=of[:], a=PS[4 + qs][:, 0:128], s=rs[:, 0:1]:
                         e.tensor_scalar(out=o, in0=a, scalar1=s, scalar2=None, op0=ALU.mult), [PS[4 + qs], rs], [of])
                    P.op("tensor", lambda e, o=pt[:, qs * 128:(qs + 1) * 128], a=of[:], idn=ident[:]:
                         e.transpose(out=o, in_=a, identity=idn), [of, ident], [pt])
                P.op("scalar", lambda e, o=cg[:, hq, 0:nq], a=pt[:, 0:nq]: e.copy(out=o, in_=a), [pt], [cg])
            P.dma("sync", lambda e, o=catT.ap[:, 0:HQ, q0:q0 + nq], i=cg[:, :, 0:nq]: e.dma_start(out=o, in_=i), catT, [cg])
        P.barrier(); es2.close(); es.close()
        if upto <= 0:
            d0 = dout("d0", [128, DC, NK], BF16); outs.append("d0")
            P.dma("gpsimd", lambda e, o=d0.ap, i=catT.ap: e.dma_start(out=o, in_=i), d0, [catT])
            P.wait_all("sync", [d0]); return P.build(), outs

        es = contextlib.ExitStack()
        SC, SW = c.SC, c.SW
        cwr = P.sb("cwr", [4, SW], F32, es); cwt = P.sb("cwt", [128, SC, 4], F32, es)
        P.dma("sync", lambda e, o=cwr[0:3, :], i=ev_cw.ap: e.dma_start(out=o, in_=i), cwr)
        P.dma("sync", lambda e, o=cwr[3:4, :], i=ev_cb.ap: e.dma_start(out=o, in_=i), cwr)
        for k in range(SC):
            pt = psn("t", [0, 1])
            P.op("tensor", lambda e, o=pt[:, 0:4], a=cwr[:, k * 128:(k + 1) * 128], idn=ident[0:4, 0:4]:
                 e.transpose(out=o, in_=a, identity=idn), [cwr, ident], [pt])
            P.op("vector", lambda e, o=cwt[:, k, :], a=pt[:, 0:4]: e.tensor_copy(out=o, in_=a), [pt], [cwt])
        Wb3 = [[P.sb("Wb%d_%d" % (i, j), [128, DC, 128], BF16, es) for j in range(3)] for i in range(2)]
        aTt = [P.sb("aTc%d" % i, [128, DC, 512], BF16, es) for i in range(2)]
        cuf = [P.sb("cuf%d" % i, [128, NK], F32, es) for i in range(2)]
        Bf = [P.sb("Bf%d" % i, [128, NK], F32, es) for i in range(2)]
        accf = [P.sb("accf%d" % i, [128, NK], F32, es) for i in range(2)]
        outb = [P.sb("outb%d" % i, [128, NK], BF16, es) for i in range(2)]
        tmpc = [P.sb("tmpc%d" % i, [128, 512], F32, es) for i in range(2)]
        base = AW + 2 * KVW
        ti = 0; tci = 0
        for ch in range(SC):
            W3 = Wb3[ch % 2]
            for j in range(3):
                wload(W3[j], w_in_all.ap[:, base + j * SW + ch * 128: base + j * SW + (ch + 1) * 128])
            cu, Bt, ac, ob = cuf[ch % 2], Bf[ch % 2], accf[ch % 2], outb[ch % 2]
            for r0 in range(0, NK, 512):
                nrow = min(512, NK - r0)
                T = aTt[ti % 2]; ti += 1
                P.dma("sync", lambda e, o=T[:, :, 0:nrow], i=aT0.ap[:, :, r0:r0 + nrow]: e.dma_start(out=o, in_=i), T, [aT0])
                pp = []
                for j in range(3):
                    pj = psn("c", [2, 3, 4, 5, 6, 7])
                    for k in range(DC):
                        P.op("tensor", lambda e, o=pj[:, 0:nrow], a=W3[j][:, k, :], b=T[:, k, 0:nrow], k=k:
                             e.matmul(o, lhsT=a, rhs=b, start=(k == 0), stop=(k == DC - 1)), [W3[j], T], [pj])
                    pp.append(pj)
                tc_ = tmpc[tci % 2]; tci += 1
                P.op("scalar", lambda e, o=Bt[:, r0:r0 + nrow], a=pp[0][:, 0:nrow]: e.copy(out=o, in_=a), [pp[0]], [Bt])
                P.op("scalar", lambda e, o=tc_[:, 0:nrow], a=pp[1][:, 0:nrow]: e.copy(out=o, in_=a), [pp[1]], [tc_])
                P.op("vector", lambda e, o=cu[:, r0:r0 + nrow], a=tc_[:, 0:nrow], b=pp[2][:, 0:nrow]:
                     e.tensor_tensor(out=o, in0=a, in1=b, op=ALU.mult), [tc_, pp[2]], [cu])
            P.op("vector", lambda e, o=ac[:], a=cu[:], s1=cwt[:, ch, 1:2], s2=cwt[:, ch, 3:4]:
                 e.tensor_scalar(out=o, in0=a, scalar1=s1, scalar2=s2, op0=ALU.mult, op1=ALU.add), [cu, cwt], [ac])
            for (a0, a1) in ((0, C), (C, NK)):
                P.op("vector", lambda e, o=ac[:, a0 + 1:a1], a=cu[:, a0:a1 - 1], s=cwt[:, ch, 0:1], b=ac[:, a0 + 1:a1]:
                     e.scalar_tensor_tensor(out=o, in0=a, scalar=s, in1=b, op0=ALU.mult, op1=ALU.add), [cu, cwt, ac], [ac])
                P.op("vector", lambda e, o=ac[:, a0:a1 - 1], a=cu[:, a0 + 1:a1], s=cwt[:, ch, 2:3], b=ac[:, a0:a1 - 1]:
                     e.scalar_tensor_tensor(out=o, in0=a, scalar=s, in1=b, op0=ALU.mult, op1=ALU.add), [cu, cwt, ac], [ac])
            P.op("gpsimd", lambda e, o=ob[:], a=ac[:], b=Bt[:]: e.tensor_tensor(out=o, in0=a, in1=b, op=ALU.mult), [ac, Bt], [ob])
            P.dma("sync", lambda e, o=catT.ap[:, HQ + ch, :], i=ob[:]: e.dma_start(out=o, in_=i), catT, [ob])
        P.barrier(); es.close()

        if which == "mix0a":
            P.wait_all("sync", [catT])
            return P.build(), outs
        pass_out(0, catT, DC, w_out_all, xin, 0, NK, C, h1, blat, affT)
        P.wait_all("sync", [h1, blat, affT])
        return P.build(), outs


def build_moe(cfg, has_ctx):
    c = cfg
    D, S, C, DC, FF, FC = c.D, c.S, c.C, c.DC, c.FF, c.FC
    C0 = C if has_ctx else 0
    NKl = S + C0
    cap, capc = c.cap, c.capc
    NT = cap // 128
    P = Prog()
    din = lambda n, s, dt=F32: P.dram(n, s, dt, kind="ExternalInput")
    dout = lambda n, s, dt=F32: P.dram(n, s, dt, kind="ExternalOutput")
    affrows = din("affrows", [8, NKl]); blat_all = din("blat_all", [4 * NKl, D], BF16)
    wg = din("wg", [2, D, FF]); wu = din("wu", [2, D, FF]); wd = din("wd", [2, FF, D])
    rowoff = din("rowoff", [8, 1]); ident_in = din("ident", [128, 128])
    slots = dout("slots", [8, cap, D]); ridx = dout("ridx", [8, cap], I32)
    outs = ["slots", "ridx"]
    if has_ctx:
        slotsc = dout("slotsc", [8, capc, D]); ridxc = dout("ridxc", [8, capc], I32); outs += ["slotsc", "ridxc"]
    PS = [P.ps("ps%d" % i, [128, 512], F32) for i in range(8)]
    rr = {}

    def psn(group, banks):
        i = rr.get(group, 0); rr[group] = i + 1
        return PS[banks[i % len(banks)]]
    ident = P.sb("identf", [128, 128], F32)
    P.dma("sync", lambda e, o=ident[:], i=ident_in[:]: e.dma_start(out=o, in_=i), ident)
    identb = P.sb("identb", [128, 128], BF16)
    P.op("vector", lambda e, o=identb[:], i=ident[:]: e.tensor_copy(out=o, in_=i), [ident], [identb])
    rof = P.sb("rof", [8, 1], F32)
    P.dma("sync", lambda e, o=rof[:], i=rowoff.ap: e.dma_start(out=o, in_=i), rof)
    gT = P.sb("gT", [128, NT, 8], F32); gixT = P.sb("gixT", [128, NT, 8], I32)
    gcT = P.sb("gcT", [32, 8], F32); gixcT = P.sb("gixcT", [32, 8], I32)
    es = contextlib.ExitStack()
    at = P.sb("at", [8, NKl], F32, es); wk = P.sb("wk", [8, S], F32, es)
    P.dma("sync", lambda e, o=at[:], i=affrows.ap: e.dma_start(out=o, in_=i), at)

    def topk(src_ap, srcbuf, n, k, work, off, tagv):
        vals = P.sb("vals" + tagv, [8, k], F32, es); idxu = P.sb("idxu" + tagv, [8, k], U32, es)
        idxf = P.sb("idxf" + tagv, [8, k], F32, es); gi = P.sb("gi" + tagv, [8, k], F32, es); ri = P.sb("ri" + tagv, [8, k], I32, es)
        cur = src_ap; curb = srcbuf
        for it in range(k // 8):
            v8 = vals[:, it * 8:(it + 1) * 8]
            P.op("vector", lambda e, o=v8, a=cur: e.max(out=o, in_=a), [curb], [vals])
            P.op("vector", lambda e, o=idxu[:, it * 8:(it + 1) * 8], m=v8, a=cur: e.max_index(out=o, in_max=m, in_values=a), [curb, vals], [idxu])
            if it < k // 8 - 1:
                P.op("vector", lambda e, o=work[:, 0:n], m=v8, a=cur: e.match_replace(out=o, in_to_replace=m, in_values=a, imm_value=-1.0),
                     [curb, vals], [work])
                cur = work[:, 0:n]; curb = work
        P.op("vector", lambda e, o=idxf[:], a=idxu[:]: e.tensor_copy(out=o, in_=a), [idxu], [idxf])
        P.op("vector", lambda e, o=gi[:], a=idxf[:], s=rof[:, 0:1]: e.tensor_scalar(out=o, in0=a, scalar1=s, scalar2=float(off), op0=ALU.add, op1=ALU.add),
             [idxf, rof], [gi])
        P.op("vector", lambda e, o=idxf[:], a=idxf[:]: e.tensor_scalar(out=o, in0=a, scalar1=float(off), scalar2=None, op0=ALU.add), [idxf], [idxf])
        P.op("vector", lambda e, o=ri[:], a=idxf[:]: e.tensor_copy(out=o, in_=a), [idxf], [ri])
        return vals, gi, ri
    vals, gi, ri = topk(at[:, C0:NKl], at, S, cap, wk, C0, "l")
    P.dma("sync", lambda e, o=ridx.ap, i=ri[:]: e.dma_start(out=o, in_=i), ridx, [ri])
    for j in range(NT):
        pt = psn("t", [0, 1])
        P.op("tensor", lambda e, o=pt[:, 0:8], a=gi[:, j * 128:(j + 1) * 128], idn=ident[0:8, 0:8]: e.transpose(out=o, in_=a, identity=idn), [gi, ident], [pt])
        P.op("tensor", lambda e, o=pt[:, 8:16], a=vals[:, j * 128:(j + 1) * 128], idn=ident[0:8, 0:8]: e.transpose(out=o, in_=a, identity=idn), [vals, ident], [pt])
        P.op("vector", lambda e, o=gixT[:, j, :], a=pt[:, 0:8]: e.tensor_copy(out=o, in_=a), [pt], [gixT])
        P.op("vector", lambda e, o=gT[:, j, :], a=pt[:, 8:16]: e.tensor_copy(out=o, in_=a), [pt], [gT])
    if has_ctx:
        wkc = P.sb("wkc", [8, C], F32, es)
        valsc, gic, ric = topk(at[:, 0:C], at, C, capc, wkc, 0, "c")
        P.dma("sync", lambda e, o=ridxc.ap, i=ric[:]: e.dma_start(out=o, in_=i), ridxc, [ric])
        pt = psn("t", [0, 1])
        P.op("tensor", lambda e, o=pt[0:capc, 0:8], a=gic[:, :], idn=ident[0:8, 0:8]: e.transpose(out=o, in_=a, identity=idn), [gic, ident], [pt])
        P.op("tensor", lambda e, o=pt[0:capc, 8:16], a=valsc[:, :], idn=ident[0:8, 0:8]: e.transpose(out=o, in_=a, identity=idn), [valsc, ident], [pt])
        P.op("vector", lambda e, o=gixcT[0:capc, :], a=pt[0:capc, 0:8]: e.tensor_copy(out=o, in_=a), [pt], [gixcT])
        P.op("vector", lambda e, o=gcT[0:capc, :], a=pt[0:capc, 8:16]: e.tensor_copy(out=o, in_=a), [pt], [gcT])
    P.barrier(); es.close()
    NSL = 2 * cap + (2 * capc if has_ctx else 0)
    xsT = P.sb("xsT", [128, DC, NSL], BF16); hidT = P.sb("hidT", [128, FC, NSL], BF16)
    xg = [P.sb("xg%d" % i, [128, D], BF16) for i in range(2)]
    Wt = [[P.sb("W%d_%d" % (m, i), [128, DC, 256], BF16) for i in range(2)] for m in range(3)]
    sg = [P.sb("sg%d" % i, [128, 512], F32) for i in range(2)]
    ot = [P.sb("ot%d" % i, [128, 256], F32) for i in range(4)]
    wi = [0, 0, 0]; xi = 0; si = 0; oi = 0

    def wl(m, src_ap):
        t = Wt[m][wi[m] % 2]; wi[m] += 1
        P.dma("gpsimd", lambda e, o=t[:], i=src_ap.rearrange("(k p) n -> p k n", p=128): e.dma_start(out=o, in_=i), t)
        return t
    for el in range(2):
        for bg in range(2):
            tiles = []
            s0 = 0
            for b in (2 * bg, 2 * bg + 1):
                r8 = el * 4 + b
                for j in range(NT):
                    tiles.append((s0, 128, r8, 0, j)); s0 += 128
                if has_ctx:
                    tiles.append((s0, capc, r8, 1, 0)); s0 += capc
            nsl = s0
            for (t0, ns, r8, kind, j) in tiles:
                g = xg[xi % 2]; xi += 1
                ixap = gixcT[0:ns, r8:r8 + 1] if kind else gixT[:, j, r8:r8 + 1]
                ixb = gixcT if kind else gixT
                P.dma("gpsimd", lambda e, o=g[0:ns, :], ix=ixap: e.indirect_dma_start(
                    out=o, out_offset=None, in_=blat_all.ap, in_offset=bass.IndirectOffsetOnAxis(ap=ix, axis=0)), g, [ixb, blat_all])
                for k4 in range(0, DC, 4):
                    pt = psn("t", [0, 1])
                    ptb = pt[:].bitcast(BF16)
                    for kk in range(4):
                        k = k4 + kk
                        P.op("tensor", lambda e, o=ptb[:, kk * 128:kk * 128 + ns], a=g[0:ns, k * 128:(k + 1) * 128], idn=identb[0:ns, 0:ns]:
                             e.transpose(out=o, in_=a, identity=idn), [g, identb], [pt])
                    P.op("scalar", lambda e, o=xsT[:, k4:k4 + 4, t0:t0 + ns], a=ptb[:, 0:512].rearrange("p (k t) -> p k t", k=4)[:, :, 0:ns]:
                         e.copy(out=o, in_=a), [pt], [xsT])
            for f2 in range(FF // 256):
                Wg_ = wl(0, wg.ap[el, :, f2 * 256:(f2 + 1) * 256]); Wu_ = wl(1, wu.ap[el, :, f2 * 256:(f2 + 1) * 256])
                for half in range(2):
                    fc = f2 * 2 + half
                    for c0 in range(0, nsl, 512):
                        ns = min(512, nsl - c0)
                        pg = psn("g", [2, 3]); pu = psn("u", [4, 5])
                        for k in range(DC):
                            P.op("tensor", lambda e, o=pg[:, 0:ns], a=Wg_[:, k, half * 128:(half + 1) * 128], b=xsT[:, k, c0:c0 + ns], k=k:
                                 e.matmul(o, lhsT=a, rhs=b, start=(k == 0), stop=(k == DC - 1)), [Wg_, xsT], [pg])
                        for k in range(DC):
                            P.op("tensor", lambda e, o=pu[:, 0:ns], a=Wu_[:, k, half * 128:(half + 1) * 128], b=xsT[:, k, c0:c0 + ns], k=k:
                                 e.matmul(o, lhsT=a, rhs=b, start=(k == 0), stop=(k == DC - 1)), [Wu_, xsT], [pu])
                        s_ = sg[si % 2]; si += 1
                        P.op("scalar", lambda e, o=s_[:, 0:ns], a=pg[:, 0:ns]: e.activation(out=o, in_=a, func=AF.Silu), [pg], [s_])
                        P.op("vector", lambda e, o=hidT[:, fc, c0:c0 + ns], a=s_[:, 0:ns], b=pu[:, 0:ns]:
                             e.tensor_tensor(out=o, in0=a, in1=b, op=ALU.mult), [s_, pu], [hidT])
            for n0 in range(0, D, 256):
                Wd_ = wl(2, wd.ap[el, :, n0:n0 + 256])
                for (t0, ns, r8, kind, j) in tiles:
                    po = psn("o", [6, 7])
                    for f in range(FC):
                        P.op("tensor", lambda e, o=po[0:ns, 0:256], a=hidT[:, f, t0:t0 + ns], b=Wd_[:, f, :], f=f:
                             e.matmul(o, lhsT=a, rhs=b, start=(f == 0), stop=(f == FC - 1)), [hidT, Wd_], [po])
                    o_ = ot[oi % 4]; oi += 1
                    gap = gcT[0:ns, r8:r8 + 1] if kind else gT[:, j, r8:r8 + 1]
                    P.op("vector", lambda e, o=o_[0:ns, :], a=po[0:ns, 0:256], s=gap: e.tensor_scalar(out=o, in0=a, scalar1=s, scalar2=None, op0=ALU.mult),
                         [po, gcT if kind else gT], [o_])
                    if kind:
                        P.dma("sync", lambda e, o=slotsc.ap[r8, 0:ns, n0:n0 + 256], i=o_[0:ns, :]: e.dma_start(out=o, in_=i), slotsc, [o_], disjoint=True)
                    else:
                        P.dma("sync", lambda e, o=slots.ap[r8, j * 128:(j + 1) * 128, n0:n0 + 256], i=o_[0:ns, :]: e.dma_start(out=o, in_=i), slots, [o_], disjoint=True)
    P.wait_all("sync", [slots, ridx] + ([slotsc, ridxc] if has_ctx else []))
    return P.build(), outs


def rope_table(cfg):
    S, C = cfg.S, cfg.C
    t = np.arange(S); row = t // 64; col = t % 64
    freqs = (10000.0 ** (-np.arange(0, 64, 2, dtype=np.float32) / 64)).astype(np.float32)
    ar = row[:, None].astype(np.float32) * freqs[None]; ac = col[:, None].astype(np.float32) * freqs[None]
    tab = np.zeros((cfg.NK, 128), np.float32)
    tab[:C, 0:64] = 1.0
    tab[C:, 0:32] = np.cos(ar); tab[C:, 32:64] = np.cos(ac); tab[C:, 64:96] = np.sin(ar); tab[C:, 96:128] = np.sin(ac)
    return tab


def _run(nc, maps, tag):
    t0 = time.time()
    maps = [{k: np.ascontiguousarray(v) for k, v in m.items()} for m in maps]
    res = _bu.run_bass_kernel_spmd(nc, maps, core_ids=list(range(len(maps))))
    print("launch", tag, "%.1fs" % (time.time() - t0), "exec_ns", getattr(res, "exec_time_ns", None), flush=True)
    return res.results


def pipeline(kb, cfg, I, progs=None):
    kb = _KB
    D, S, C, NK, D8 = cfg.D, cfg.S, cfg.C, cfg.NK, cfg.D8
    ident = np.eye(128, dtype=np.float32)
    sel = np.zeros((2, 256), np.float32); sel[0, :128] = 1; sel[1, 128:] = 1
    nc, _ = kb.build_mod(cfg)
    maps = []
    for r in range(8):
        maps.append(dict(c5=np.concatenate([I["c"], I["c_ctx"][None]], 0),
                         modw=I["mod_w"].reshape(2, D, 6, 8, D8)[:, :, :, r, :].reshape(2, D, 6 * D8),
                         modb=I["mod_b"].reshape(2, 6, 8, D8)[:, :, r, :].reshape(2, 6 * D8), ident=ident))
    res = _run(nc, maps, "mod")
    mod = np.stack([res[r]["mloc_out"].reshape(5, 2, 6, D8) for r in range(8)], 3).reshape(5, 2, 6, D)

    def common(b):
        return dict(modrows=np.stack([mod[b].reshape(-1), mod[4].reshape(-1)], 0), n1g=I["norm1_g"], n2g=I["norm2_g"],
                    ident=ident, selin=sel)
    hc = kb.half_cfg(cfg)
    CH, SH = cfg.CH, cfg.SH
    nc, _ = kb.build(hc, "mix0a")
    rt = rope_table(cfg)
    Wi = I["ev_w_in"][0]
    AW, KVW, SW = cfg.AW, cfg.KVW, cfg.SW
    maps = []
    for r in range(8):
        b, s = r // 2, r % 2
        q0 = s * hc.AW
        kvh0 = (s * cfg.HKV // 2) * 128 if cfg.HKV >= 2 else 0
        cols = [Wi[:, q0:q0 + hc.AW], Wi[:, AW + kvh0:AW + kvh0 + hc.KVW], Wi[:, AW + KVW + kvh0:AW + KVW + kvh0 + hc.KVW]]
        for j in range(3):
            c0 = AW + 2 * KVW + j * SW + s * hc.SW
            cols.append(Wi[:, c0:c0 + hc.SW])
        m = common(b)
        m.update(xin=np.concatenate([I["ctx"][b], I["x"][b]], 0), ropet=rt, ev_w_in=np.concatenate(cols, 1), ev_w_out=I["ev_w_out"][0],
                 evq=I["ev_q_norm"], evk=I["ev_k_norm"], ev_cw=I["ev_conv_w"][0][:, s * hc.SW:(s + 1) * hc.SW],
                 ev_cb=I["ev_conv_b"][:, s * hc.SW:(s + 1) * hc.SW], router=I["moe_router"][0])
        maps.append(m)
    ra = _run(nc, maps, "mix0a")
    nc, _ = kb.build(cfg, "out0")
    maps = []
    HQh, SCh = hc.HQ, hc.SC
    for r in range(8):
        b, s = r // 2, r % 2
        c0, c1 = ra[2 * b]["catT"], ra[2 * b + 1]["catT"]
        full = np.concatenate([c0[:, :HQh], c1[:, :HQh], c0[:, HQh:], c1[:, HQh:]], 1)
        colsel = np.concatenate([np.arange(s * CH, (s + 1) * CH), C + np.arange(s * SH, (s + 1) * SH)])
        m = common(b)
        m.update(catT_in=full[:, :, colsel], xin_own=np.concatenate([I["ctx"][b][s * CH:(s + 1) * CH], I["x"][b][s * SH:(s + 1) * SH]], 0),
                 ev_w_out=I["ev_w_out"][0], router=I["moe_router"][0])
        maps.append(m)
    rb = _run(nc, maps, "out0")
    r0 = []
    for b in range(4):
        a0, a1 = rb[2 * b], rb[2 * b + 1]
        r0.append(dict(h1=np.concatenate([a0["h1"][:CH], a1["h1"][:CH], a0["h1"][CH:], a1["h1"][CH:]], 0),
                       blat=np.concatenate([a0["blat"][:CH], a1["blat"][:CH], a0["blat"][CH:], a1["blat"][CH:]], 0),
                       affT=np.concatenate([a0["affT"][:, :CH], a1["affT"][:, :CH], a0["affT"][:, CH:], a1["affT"][:, CH:]], 1)))

    def moe(layer, blat_list, aff_list, has_ctx):
        NKl = NK if has_ctx else S
        nc, _ = kb.build_moe(cfg, has_ctx)
        blat_all = np.concatenate(blat_list, 0)
        maps = []
        for k in range(8):
            maps.append(dict(blat_all=blat_all, ident=ident,
                             affrows=np.stack([aff_list[b][2 * k + el] for el in range(2) for b in range(4)], 0),
                             rowoff=np.array([[b * NKl] for el in range(2) for b in range(4)], np.float32),
                             wg=I["moe_w_gate"][layer, 2 * k:2 * k + 2], wu=I["moe_w_up"][layer, 2 * k:2 * k + 2],
                             wd=I["moe_w_down"][layer, 2 * k:2 * k + 2]))
        rs = _run(nc, maps, "moe%d" % layer)
        outb = []
        for b in range(4):
            d = dict(slots_b=np.stack([rs[e // 2]["slots"][(e % 2) * 4 + b] for e in range(16)], 0),
                     ridx_b=np.stack([rs[e // 2]["ridx"][(e % 2) * 4 + b] for e in range(16)], 0))
            if has_ctx:
                d.update(slotsc_b=np.stack([rs[e // 2]["slotsc"][(e % 2) * 4 + b] for e in range(16)], 0),
                         ridxc_b=np.stack([rs[e // 2]["ridxc"][(e % 2) * 4 + b] for e in range(16)], 0))
            outb.append(d)
        return outb
    m0 = moe(0, [r0[b]["blat"] for b in range(4)], [r0[b]["affT"] for b in range(4)], True)
    nc, _ = kb.build(hc, "mix1a")
    maps = []
    LW, LWh, NLBh = cfg.LW, hc.LW, hc.NLB
    od_vec = np.concatenate([I["od_ba"][0], I["od_bx"][0], I["od_lam"][0]], 0)
    Wi1 = I["od_w_in"][0]
    for r in range(8):
        b, s = r // 2, r % 2
        cs = slice(s * LWh, (s + 1) * LWh)
        m = common(b)
        m.update(h1=r0[b]["h1"], od_w_in=np.concatenate([Wi1[:, s * LWh:(s + 1) * LWh], Wi1[:, LW + s * LWh:LW + (s + 1) * LWh]], 1),
                 od_w_out=I["od_w_out"][0][cs], od_cw=I["od_conv_w"][0][:, cs], od_cb=I["od_conv_b"][:, cs],
                 od_wa=I["od_wa"][0][:, s * NLBh:(s + 1) * NLBh], od_wx=I["od_wx"][0][:, s * NLBh:(s + 1) * NLBh],
                 od_vec=od_vec[:, cs], router=I["moe_router"][1])
        m.update(m0[b])
        maps.append(m)
    rc = _run(nc, maps, "mix1a")
    nc, _ = kb.build(cfg, "out1")
    maps = []
    for r in range(8):
        b, s = r // 2, r % 2
        yg = np.concatenate([rc[2 * b]["ygT"], rc[2 * b + 1]["ygT"]], 1)
        m = common(b)
        m.update(ygT_in=yg[:, :, s * SH:(s + 1) * SH], hl0_own=rc[2 * b]["hl0"][C + s * SH:C + (s + 1) * SH],
                 od_w_out=I["od_w_out"][0], router=I["moe_router"][1])
        maps.append(m)
    rd = _run(nc, maps, "out1")
    r1 = [dict(h2=np.concatenate([rd[2 * b]["h2"], rd[2 * b + 1]["h2"]], 0),
               blat1=np.concatenate([rd[2 * b]["blat1"], rd[2 * b + 1]["blat1"]], 0),
               affT1=np.concatenate([rd[2 * b]["affT1"], rd[2 * b + 1]["affT1"]], 1)) for b in range(4)]
    m1 = moe(1, [r1[b]["blat1"] for b in range(4)], [r1[b]["affT1"] for b in range(4)], False)
    nc, _ = kb.build(cfg, "final")
    maps = []
    for b in range(4):
        m = common(b)
        m.update(h2=r1[b]["h2"], fng=I["final_norm_g"][None])
        m.update(m1[b])
        maps.append(m)
    r2 = _run(nc, maps, "final")
    dbg = dict(r0=r0, r1=r1)
    return np.stack([r2[b]["out"] for b in range(4)], 0), dbg


class _KBNS:
    pass


_KB = _KBNS()
_KB.build_mod = build_mod
_KB.build = build
_KB.build_moe = build_moe
_KB.half_cfg = half_cfg


def kernel(**inputs):
    I = {k: np.asarray(v) for k, v in inputs.items()}
    cfg = Cfg()
    out, _ = pipeline(None, cfg, I)
    return np.ascontiguousarray(out.astype(np.float32))
```
